# Optimizing a Trainium2 kernel written in Bass

```python
import math
import jax, jax.numpy as jnp
from jax import lax
import numpy as np

D_MODEL = 1024
BATCH = 32
SEQ = 2048
DEPTH = 1
DEC_BATCH = 32
DEC_SEQ = 64
PAST_LEN = 2048

CHUNK = 64
HEAD_DIM = 64
A_Q_HEADS = 8
A_KV_HEADS = 2
A_GROUP = A_Q_HEADS // A_KV_HEADS
A_WINDOW = 128
A_BACK_CHUNKS = A_WINDOW // CHUNK
B_HEADS = 8
B_BACK_CHUNKS = 8
B_REACH = B_BACK_CHUNKS * CHUNK
REL_CLIP = 256
T5_BUCKETS = 32
T5_MAX_DIST = 128
N_GROUPS = 4
EXPERTS_PER_GROUP = 8
N_EXPERTS = N_GROUPS * EXPERTS_PER_GROUP
TOP_K_IN_GROUP = 2
D_EXPERT = D_MODEL // 4
EPS = 1e-6

QA_W = A_Q_HEADS * HEAD_DIM
KVA_W = A_KV_HEADS * HEAD_DIM
B_W = B_HEADS * HEAD_DIM
IN_W = QA_W + 2 * KVA_W + 3 * B_W
SPLITS = [QA_W, QA_W + KVA_W, QA_W + 2 * KVA_W, QA_W + 2 * KVA_W + B_W, QA_W + 2 * KVA_W + 2 * B_W]

kernel_name = 'hybrid_streaming_encoder_step'


def rms_norm(x, g):
    xf = x.astype(jnp.float32)
    y = xf * lax.rsqrt(jnp.mean(xf * xf, axis=-1, keepdims=True) + EPS)
    return (y * g.astype(jnp.float32)).astype(x.dtype)


def modulate(h, shift, scale):
    return h * (1 + scale[:, None, :]) + shift[:, None, :]


def ada_params(c, w_ada, b_ada):
    return jnp.split(jax.nn.silu(c) @ w_ada + b_ada, 6, axis=-1)


def t5_bucket(rel):
    half = T5_BUCKETS // 2
    exact = half // 2
    ret = jnp.where(rel > 0, half, 0)
    n = jnp.abs(rel)
    nf = jnp.maximum(n, 1).astype(jnp.float32)
    large = exact + (jnp.log(nf / exact) / math.log(T5_MAX_DIST / exact) * (half - exact)).astype(jnp.int32)
    large = jnp.minimum(large, half - 1)
    return ret + jnp.where(n < exact, n, large)


def t5_window_bias(t5_table, t, hist):
    rel = jnp.arange(hist + t)[None, :] - hist - jnp.arange(t)[:, None]
    bias = t5_table[t5_bucket(rel)]
    return jnp.transpose(bias, (2, 0, 1)).astype(jnp.float32).reshape(A_KV_HEADS, A_GROUP, t, hist + t)


def chunk_rel_bias(rel_table, t, hist):
    d = jnp.arange(t)[:, None] + hist - jnp.arange(hist + t)[None, :]
    idx = jnp.clip(d, -REL_CLIP, REL_CLIP) + REL_CLIP
    return rel_table[:, idx].astype(jnp.float32)[:, None]


def band_attend(q, k, v, bias, valid, sink):
    s = jnp.einsum('btkgd,bmkd->bkgtm', q, k).astype(jnp.float32) * (HEAD_DIM ** -0.5) + bias
    if valid is not None:
        s = jnp.where(valid, s, -1e30)
    if sink is None:
        p = jax.nn.softmax(s, axis=-1)
    else:
        sk = sink.astype(jnp.float32)[None, :, :, None, None]
        mx = jnp.maximum(jnp.max(s, axis=-1, keepdims=True), sk)
        e = jnp.exp(s - mx)
        p = e / (jnp.sum(e, axis=-1, keepdims=True) + jnp.exp(sk - mx))
    return jnp.einsum('bkgtm,bmkd->btkgd', p.astype(v.dtype), v)


def chunked_band_prompt(q, k, v, n_back, bias, sink):
    b, s, hkv, g, dh = q.shape
    nc = s // CHUNK
    band = (n_back + 1) * CHUNK
    pad = ((0, 0), (n_back * CHUNK, 0), (0, 0), (0, 0))
    kp = jnp.pad(k, pad)
    vp = jnp.pad(v, pad)
    qc = jnp.moveaxis(q.reshape(b, nc, CHUNK, hkv, g, dh), 1, 0)
    m_local = jnp.arange(band)

    def one_chunk(args):
        j, qj = args
        kj = lax.dynamic_slice_in_dim(kp, j * CHUNK, band, axis=1)
        vj = lax.dynamic_slice_in_dim(vp, j * CHUNK, band, axis=1)
        valid = m_local >= (n_back - j) * CHUNK
        return band_attend(qj, kj, vj, bias, valid, sink)

    out = lax.map(one_chunk, (jnp.arange(nc), qc))
    return jnp.moveaxis(out, 0, 1).reshape(b, s, hkv * g * dh)


def split_mixer_inputs(p):
    b, t, _ = p.shape
    qa, ka, va, qb, kb, vb = jnp.split(p, SPLITS, axis=-1)
    return (qa.reshape(b, t, A_KV_HEADS, A_GROUP, HEAD_DIM),
            ka.reshape(b, t, A_KV_HEADS, HEAD_DIM),
            va.reshape(b, t, A_KV_HEADS, HEAD_DIM),
            qb.reshape(b, t, B_HEADS, 1, HEAD_DIM),
            kb.reshape(b, t, B_HEADS, HEAD_DIM),
            vb.reshape(b, t, B_HEADS, HEAD_DIM))


def mixer_sublayer(x, shift, scale, gate, g_pre, g_post, w_in, w_proj_a, w_proj_b, w_gate, b_gate, w_o, attend):
    h = modulate(rms_norm(x, g_pre), shift, scale)
    oa, ob, new_state = attend(*split_mixer_inputs(h @ w_in))
    ga, gb = jnp.split(jax.nn.sigmoid(h @ w_gate + b_gate), 2, axis=-1)
    mixed = (ga * (oa @ w_proj_a) + gb * (ob @ w_proj_b)) @ w_o
    return x + gate[:, None, :] * rms_norm(mixed, g_post), new_state


def hier_moe(h, w_route_g, b_route_g, w_route_e, b_route_e, w_e_gate, w_e_up, w_e_down):
    b, s, d = h.shape
    t = h.reshape(-1, d)
    n = t.shape[0]
    g_logits = (t @ w_route_g).astype(jnp.float32) + b_route_g.astype(jnp.float32)
    g_prob = jax.nn.softmax(g_logits, axis=-1)
    g_idx = jnp.argmax(g_logits, axis=-1)
    g_w = jnp.take_along_axis(g_prob, g_idx[:, None], axis=-1)
    e_logits = ((t @ w_route_e).astype(jnp.float32) + b_route_e.astype(jnp.float32)).reshape(n, N_GROUPS, EXPERTS_PER_GROUP)
    e_sel = jnp.take_along_axis(e_logits, g_idx[:, None, None], axis=1)[:, 0]
    top_v, top_i = lax.top_k(e_sel, TOP_K_IN_GROUP)
    w = g_w * jax.nn.softmax(top_v, axis=-1)
    expert_id = g_idx[:, None] * EXPERTS_PER_GROUP + top_i
    combine = jnp.einsum('nk,nke->ne', w, jax.nn.one_hot(expert_id, N_EXPERTS, dtype=jnp.float32)).astype(t.dtype)
    y = jnp.zeros_like(t)
    for e in range(N_EXPERTS):
        he = jax.nn.silu(t @ w_e_gate[e]) * (t @ w_e_up[e])
        y = y + combine[:, e:e + 1] * (he @ w_e_down[e])
    return y.reshape(b, s, d)


def moe_sublayer(x, shift, scale, gate, g_pre, g_post, w_route_g, b_route_g, w_route_e, b_route_e, w_e_gate, w_e_up, w_e_down):
    h = modulate(rms_norm(x, g_pre), shift, scale)
    y = hier_moe(h, w_route_g, b_route_g, w_route_e, b_route_e, w_e_gate, w_e_up, w_e_down)
    return x + gate[:, None, :] * rms_norm(y, g_post)


def setup_inputs(seed: int = 0) -> dict:
    key = jax.random.key(seed)
    ks = iter(jax.random.split(key, 40))

    def nrm(shape, scale):
        return scale * jax.random.normal(next(ks), shape, jnp.float32)

    la = min(A_WINDOW, PAST_LEN)
    lb = min(B_REACH, PAST_LEN)
    return {
        'x_prompt': nrm((BATCH, SEQ, D_MODEL), 1.0),
        'x_sample': nrm((DEC_BATCH, DEC_SEQ, D_MODEL), 1.0),
        'c_prompt': nrm((BATCH, D_MODEL), 1.0),
        'c_sample': nrm((DEC_BATCH, D_MODEL), 1.0),
        'cache_a_k': nrm((DEPTH, DEC_BATCH, la, A_KV_HEADS, HEAD_DIM), 1.0),
        'cache_a_v': nrm((DEPTH, DEC_BATCH, la, A_KV_HEADS, HEAD_DIM), 1.0),
        'cache_b_k': nrm((DEPTH, DEC_BATCH, lb, B_HEADS, HEAD_DIM), 1.0),
        'cache_b_v': nrm((DEPTH, DEC_BATCH, lb, B_HEADS, HEAD_DIM), 1.0),
        'w_ada': nrm((DEPTH, D_MODEL, 6 * D_MODEL), 0.3 * D_MODEL ** -0.5),
        'b_ada': nrm((DEPTH, 6 * D_MODEL), 0.02),
        'g_pre_mix': 1.0 + nrm((DEPTH, D_MODEL), 0.05),
        'g_post_mix': 1.0 + nrm((DEPTH, D_MODEL), 0.05),
        'g_pre_ffn': 1.0 + nrm((DEPTH, D_MODEL), 0.05),
        'g_post_ffn': 1.0 + nrm((DEPTH, D_MODEL), 0.05),
        'w_in': nrm((DEPTH, D_MODEL, IN_W), D_MODEL ** -0.5),
        'a_sinks': nrm((DEPTH, A_Q_HEADS), 0.5),
        't5_table': nrm((T5_BUCKETS, A_Q_HEADS), 0.5),
        'b_rel_table': nrm((DEPTH, B_HEADS, 2 * REL_CLIP + 1), 0.5),
        'w_proj_a': nrm((DEPTH, QA_W, D_MODEL), QA_W ** -0.5),
        'w_proj_b': nrm((DEPTH, B_W, D_MODEL), B_W ** -0.5),
        'w_gate': nrm((DEPTH, D_MODEL, 2 * D_MODEL), D_MODEL ** -0.5),
        'b_gate': nrm((DEPTH, 2 * D_MODEL), 0.02),
        'w_o': nrm((DEPTH, D_MODEL, D_MODEL), D_MODEL ** -0.5),
        'w_route_g': nrm((DEPTH, D_MODEL, N_GROUPS), D_MODEL ** -0.5),
        'b_route_g': nrm((DEPTH, N_GROUPS), 0.01),
        'w_route_e': nrm((DEPTH, D_MODEL, N_EXPERTS), D_MODEL ** -0.5),
        'b_route_e': nrm((DEPTH, N_EXPERTS), 0.01),
        'w_e_gate': nrm((DEPTH, N_EXPERTS, D_MODEL, D_EXPERT), D_MODEL ** -0.5),
        'w_e_up': nrm((DEPTH, N_EXPERTS, D_MODEL, D_EXPERT), D_MODEL ** -0.5),
        'w_e_down': nrm((DEPTH, N_EXPERTS, D_EXPERT, D_MODEL), D_EXPERT ** -0.5),
    }


def reference(x_prompt, x_sample, c_prompt, c_sample, cache_a_k, cache_a_v, cache_b_k, cache_b_v,
              w_ada, b_ada, g_pre_mix, g_post_mix, g_pre_ffn, g_post_ffn, w_in, a_sinks, t5_table,
              b_rel_table, w_proj_a, w_proj_b, w_gate, b_gate, w_o, w_route_g, b_route_g,
              w_route_e, b_route_e, w_e_gate, w_e_up, w_e_down):
    xp, xs = x_prompt, x_sample
    nak_p, nav_p, nbk_p, nbv_p = [], [], [], []
    nak_s, nav_s, nbk_s, nbv_s = [], [], [], []
    for l in range(DEPTH):
        sinks = a_sinks[l].reshape(A_KV_HEADS, A_GROUP)
        bias_a_p = t5_window_bias(t5_table, CHUNK, A_WINDOW)
        bias_b_p = chunk_rel_bias(b_rel_table[l], CHUNK, B_REACH)
        cak, cav, cbk, cbv = cache_a_k[l], cache_a_v[l], cache_b_k[l], cache_b_v[l]
        la, lb = cak.shape[1], cbk.shape[1]

        def attend_prompt(qa, ka, va, qb, kb, vb):
            oa = chunked_band_prompt(qa, ka, va, A_BACK_CHUNKS, bias_a_p, sinks)
            ob = chunked_band_prompt(qb, kb, vb, B_BACK_CHUNKS, bias_b_p, None)
            return oa, ob, (ka[:, -A_WINDOW:], va[:, -A_WINDOW:], kb[:, -B_REACH:], vb[:, -B_REACH:])

        def attend_sample(qa, ka, va, qb, kb, vb):
            b, t = qa.shape[0], qa.shape[1]
            ka_band = jnp.concatenate([cak, ka], axis=1)
            va_band = jnp.concatenate([cav, va], axis=1)
            kb_band = jnp.concatenate([cbk, kb], axis=1)
            vb_band = jnp.concatenate([cbv, vb], axis=1)
            oa = band_attend(qa, ka_band, va_band, t5_window_bias(t5_table, t, la), None, sinks).reshape(b, t, QA_W)
            ob = band_attend(qb, kb_band, vb_band, chunk_rel_bias(b_rel_table[l], t, lb), None, None).reshape(b, t, B_W)
            return oa, ob, (ka_band[:, -la:], va_band[:, -la:], kb_band[:, -lb:], vb_band[:, -lb:])

        p1s, p1c, p1g, p2s, p2c, p2g = ada_params(c_prompt, w_ada[l], b_ada[l])
        s1s, s1c, s1g, s2s, s2c, s2g = ada_params(c_sample, w_ada[l], b_ada[l])

        xp, st_p = mixer_sublayer(xp, p1s, p1c, p1g, g_pre_mix[l], g_post_mix[l], w_in[l], w_proj_a[l],
                                  w_proj_b[l], w_gate[l], b_gate[l], w_o[l], attend_prompt)
        xs, st_s = mixer_sublayer(xs, s1s, s1c, s1g, g_pre_mix[l], g_post_mix[l], w_in[l], w_proj_a[l],
                                  w_proj_b[l], w_gate[l], b_gate[l], w_o[l], attend_sample)
        xp = moe_sublayer(xp, p2s, p2c, p2g, g_pre_ffn[l], g_post_ffn[l], w_route_g[l], b_route_g[l],
                          w_route_e[l], b_route_e[l], w_e_gate[l], w_e_up[l], w_e_down[l])
        xs = moe_sublayer(xs, s2s, s2c, s2g, g_pre_ffn[l], g_post_ffn[l], w_route_g[l], b_route_g[l],
                          w_route_e[l], b_route_e[l], w_e_gate[l], w_e_up[l], w_e_down[l])
        nak_p.append(st_p[0]); nav_p.append(st_p[1]); nbk_p.append(st_p[2]); nbv_p.append(st_p[3])
        nak_s.append(st_s[0]); nav_s.append(st_s[1]); nbk_s.append(st_s[2]); nbv_s.append(st_s[3])

    return (xp, xs,
            jnp.stack(nak_p), jnp.stack(nav_p), jnp.stack(nbk_p), jnp.stack(nbv_p),
            jnp.stack(nak_s), jnp.stack(nav_s), jnp.stack(nbk_s), jnp.stack(nbv_s))
```

```python
import math
from contextlib import ExitStack

import numpy as np
import jax
import jax.numpy as jnp

import concourse.bass as bass
import concourse.mybir as mybir
from concourse.bass_utils import run_bass_kernel_spmd

F32 = mybir.dt.float32
BF16 = mybir.dt.bfloat16
AF = mybir.ActivationFunctionType
ALU = mybir.AluOpType
AX = mybir.AxisListType

ENGS = ("pe", "act", "dve", "pool", "sp")
NDSEM = 8
NCORES = 8
NB = 4
SEQ = 2048
D = 1024
NEG = -30000.0


class SemState:
    def __init__(self, nc, stack):
        self.esem = {e: stack.enter_context(nc.semaphore("s_" + e)) for e in ENGS}
        self.ecnt = {e: 0 for e in ENGS}
        self.dsem = {q: [stack.enter_context(nc.semaphore("d_%s%d" % (q, i))) for i in range(NDSEM)]
                     for q in ("sp", "act", "pool")}
        self.dcnt = {q: 0 for q in ("sp", "act", "pool")}
        self.seen = {e: {} for e in ENGS}


class Op:
    __slots__ = ("eng", "fn", "dma", "idx", "deps", "sig", "sigval", "dsem", "dval", "dprev")

    def __init__(self, eng, fn, dma):
        self.eng = eng
        self.fn = fn
        self.dma = dma
        self.deps = []
        self.sig = False
        self.sigval = None
        self.dsem = None


class Sched:
    def __init__(self, nc, st):
        self.nc = nc
        self.st = st
        self.ops = {e: [] for e in ENGS}
        self.last_w = {}
        self.readers = {}

    def add(self, eng, fn, reads=(), writes=(), dma=False):
        op = Op(eng, fn, dma)
        op.idx = len(self.ops[eng])
        deps = {}
        for k in reads:
            w = self.last_w.get(k)
            if w is not None:
                deps[id(w)] = w
        for k in writes:
            w = self.last_w.get(k)
            if w is not None:
                deps[id(w)] = w
            for r in self.readers.get(k, ()):
                deps[id(r)] = r
        op.deps = list(deps.values())
        for k in writes:
            self.last_w[k] = op
            self.readers[k] = []
        for k in reads:
            if k not in writes:
                self.readers.setdefault(k, []).append(op)
        self.ops[eng].append(op)
        return op

    def pe(self, fn, reads=(), writes=()):
        return self.add("pe", fn, reads, writes)

    def act(self, fn, reads=(), writes=()):
        return self.add("act", fn, reads, writes)

    def dve(self, fn, reads=(), writes=()):
        return self.add("dve", fn, reads, writes)

    def pool(self, fn, reads=(), writes=()):
        return self.add("pool", fn, reads, writes)

    def dma(self, q, out, in_, reads=(), writes=(), **kw):
        return self.add(q, lambda e: e.dma_start(out=out, in_=in_, **kw), reads, writes, dma=True)

    @staticmethod
    def _needs_sync(p, c):
        if p.dma or c.dma:
            return True
        if p.eng != c.eng:
            return True
        if p.eng == "pe":
            return False
        return (c.idx - p.idx) <= 2

    def emit(self):
        nc, st = self.nc, self.st
        for e in ENGS:
            for o in reversed(self.ops[e]):
                if not o.dma:
                    o.sig = True
                    break
        for e in ENGS:
            for c in self.ops[e]:
                for p in c.deps:
                    if (not p.dma) and self._needs_sync(p, c):
                        p.sig = True
        for e in ENGS:
            for o in self.ops[e]:
                if o.dma:
                    n = st.dcnt[e]
                    st.dcnt[e] += 1
                    o.dsem = st.dsem[e][n % NDSEM]
                    o.dval = 16 * (n // NDSEM + 1)
                    o.dprev = 16 * (n // NDSEM)
                elif o.sig:
                    st.ecnt[e] += 1
                    o.sigval = st.ecnt[e]
        end_e = dict(st.ecnt)
        end_d = dict(st.dcnt)

        def emit_engine(e, eng):
            seen = st.seen[e]

            def wait(sem, val):
                k = id(sem)
                if val <= 0 or seen.get(k, 0) >= val:
                    return
                eng.wait_ge(sem, val)
                seen[k] = val

            for o in self.ops[e]:
                for p in o.deps:
                    if p.dma:
                        wait(p.dsem, p.dval)
                    elif self._needs_sync(p, o):
                        wait(st.esem[p.eng], p.sigval)
                if o.dma:
                    wait(o.dsem, o.dprev)
                    o.fn(eng).then_inc(o.dsem, 16)
                else:
                    ins = o.fn(eng)
                    if o.sig:
                        ins.then_inc(st.esem[e], 1)
            for e2 in ENGS:
                if e2 != e:
                    wait(st.esem[e2], end_e[e2])
            for q in st.dsem:
                n = end_d[q]
                for i in range(NDSEM):
                    if n > i:
                        last_n = ((n - 1 - i) // NDSEM) * NDSEM + i
                        wait(st.dsem[q][i], 16 * (last_n // NDSEM + 1))

        with nc.Block() as block:
            @block.tensor
            def _(eng):
                emit_engine("pe", eng)

            @block.scalar
            def _(eng):
                emit_engine("act", eng)

            @block.vector
            def _(eng):
                emit_engine("dve", eng)

            @block.gpsimd
            def _(eng):
                emit_engine("pool", eng)

            @block.sync
            def _(eng):
                emit_engine("sp", eng)


def AP(t, off, ap):
    return bass.AP(t.tensor, off, ap)


def bcl(ap, n):
    return bass.AP(ap.tensor, ap.offset, [list(x) for x in ap.ap] + [[0, n]])


def t5_runs():
    cpu = jax.devices("cpu")[0]
    with jax.default_device(cpu):
        rel = -(jnp.arange(383, dtype=jnp.int32) - 127)
        half, exact = 16, 8
        ret = jnp.where(rel > 0, half, 0)
        n = jnp.abs(rel)
        nf = jnp.maximum(n, 1).astype(jnp.float32)
        large = exact + (jnp.log(nf / exact) / math.log(128 / exact) * (half - exact)).astype(jnp.int32)
        large = jnp.minimum(large, half - 1)
        bucket = np.asarray(ret + jnp.where(n < exact, n, large))
    runs = []
    j0 = 0
    for j in range(1, 384):
        if j == 383 or bucket[j] != bucket[j0]:
            runs.append((j0, j, int(bucket[j0])))
            j0 = j
    return runs


def build_program():
    nc = bass.Bass("TRN2", target_bir_lowering=False)

    def din(name, shape):
        return nc.dram_tensor(name, list(shape), F32, kind="ExternalInput").ap()

    def dout(name, shape):
        return nc.dram_tensor(name, list(shape), F32, kind="ExternalOutput").ap()

    xp = din("xp", [NB, SEQ, D]); xsm = din("xs", [NB, 64, D])
    cpr = din("cp", [NB, D]); csm = din("cs", [NB, D])
    cak = din("cak", [NB, 128, 128]); cav = din("cav", [NB, 128, 128])
    cbk = din("cbk", [NB, 512, 512]); cbv = din("cbv", [NB, 512, 512])
    w_ada = din("w_ada", [D, 6 * D]); b_ada = din("b_ada", [6 * D])
    g_pre_mix = din("g_pre_mix", [D]); g_post_mix = din("g_post_mix", [D])
    g_pre_ffn = din("g_pre_ffn", [D]); g_post_ffn = din("g_post_ffn", [D])
    w_in = din("w_in", [D, 2304]); a_sinks = din("a_sinks", [8]); t5_table = din("t5_table", [32, 8])
    b_rel = din("b_rel", [8, 513]); w_pa = din("w_pa", [512, D]); w_pb = din("w_pb", [512, D])
    w_gate = din("w_gate", [D, 2 * D]); b_gate = din("b_gate", [2 * D]); w_o = din("w_o", [D, D])
    w_rg = din("w_rg", [D, 4]); b_rg = din("b_rg", [4]); w_re = din("w_re", [D, 32]); b_re = din("b_re", [32])
    w_eg = din("w_eg", [32, D, 256]); w_eu = din("w_eu", [32, D, 256]); w_ed = din("w_ed", [32, 256, D])

    yp = dout("yp", [NB, SEQ, D]); ys = dout("ys", [NB, 64, D])
    nakp = dout("nakp", [NB, 128, 128]); navp = dout("navp", [NB, 128, 128])
    nbkp = dout("nbkp", [NB, 512, 512]); nbvp = dout("nbvp", [NB, 512, 512])
    naks = dout("naks", [NB, 128, 128]); navs = dout("navs", [NB, 128, 128])
    nbks = dout("nbks", [NB, 512, 512]); nbvs = dout("nbvs", [NB, 512, 512])

    NT = NB * 16 + NB
    X1d = nc.dram_tensor("X1d", [NT * 128, D], F32).ap()
    Gd = nc.dram_tensor("Gd", [8, 2, D], F32).ap()
    extB = nc.dram_tensor("extB", [8, 768], F32).ap()
    SB_ = nc.dram_tensor("SBd", [128, 8 * 768], F32).ap()
    extA = nc.dram_tensor("extA", [8, 384], F32).ap()
    SA_ = nc.dram_tensor("SAd", [128, 8 * 384], F32).ap()

    with ExitStack() as es:
        st = SemState(nc, es)

        def T(stack, name, shape, dt):
            return stack.enter_context(nc.sbuf_tensor(name, list(shape), dt))

        banks = [es.enter_context(nc.psum_tensor("bank%d" % i, [128, 512], F32)) for i in range(8)]
        bkey = ["bank%d" % i for i in range(8)]

        ident = T(es, "ident", [128, 128], BF16)
        identf = T(es, "identf", [128, 128], F32)
        adaT = T(es, "adaT", [128, 48, 8], F32)
        A1 = T(es, "A1", [128, 8, 8], F32)
        A2 = T(es, "A2", [128, 8, 8], F32)
        bgT = T(es, "bgT", [128, 16], F32)
        expsink = T(es, "expsink", [128, 8], F32)
        wr_sb = T(es, "wr_sb", [128, 8, 36], BF16)
        rbias = T(es, "rbias", [128, 36], F32)
        rstd_all = T(es, "rstd_all", [128, 68], F32)

        with ExitStack() as e0:
            s = Sched(nc, st)
            wada = T(e0, "wada", [128, 8, 6 * D], BF16)
            c8 = T(e0, "c8", [8, D], F32)
            sc8 = T(e0, "sc8", [8, D], BF16)
            cT = T(e0, "cT", [128, 8, 8], BF16)
            colsrc = T(e0, "colsrc", [80, 128], F32)
            colT = T(e0, "colT", [128, 80], F32)
            t5src = T(e0, "t5src", [32, 8], F32)
            t5T = T(e0, "t5T", [8, 32], F32)
            extA_sb = T(e0, "extA_sb", [8, 384], F32)
            extB_sb = T(e0, "extB_sb", [8, 768], F32)
            tmp88 = T(e0, "tmp88", [128, 8, 8], F32)
            bada8 = T(e0, "bada8", [8, 2, D], F32)
            gpost8 = T(e0, "gpost8", [8, 2, D], F32)
            G8 = T(e0, "G8", [8, 2, D], F32)
            sink_t = T(e0, "sink_t", [128, 8], F32)

            s.pool(lambda e: e.memset(identf[:], 0.0), writes=["identf"])
            s.pool(lambda e: e.affine_select(out=identf[:], in_=identf[:], pattern=[[-1, 128]],
                                              compare_op=ALU.not_equal, fill=1.0, base=0, channel_multiplier=1),
                   writes=["identf"])
            s.dve(lambda e: e.tensor_copy(out=ident[:], in_=identf[:]), reads=["identf"], writes=["ident"])

            s.dma("sp", sink_t[:], AP(a_sinks, 0, [[0, 128], [1, 8]]), writes=["sink_t"])
            s.act(lambda e: e.activation(out=expsink[:], in_=sink_t[:], func=AF.Exp), reads=["sink_t"], writes=["expsink"])
            s.dma("pool", wr_sb[:, :, 0:4], w_rg.rearrange("(c p) n -> p c n", p=128), writes=["wr"])
            s.dma("pool", wr_sb[:, :, 4:36], w_re.rearrange("(c p) n -> p c n", p=128), writes=["wr"])
            s.dma("sp", rbias[:, 0:4], AP(b_rg, 0, [[0, 128], [1, 4]]), writes=["rbias"])
            s.dma("sp", rbias[:, 4:36], AP(b_re, 0, [[0, 128], [1, 32]]), writes=["rbias"])
            s.dma("sp", extB_sb[:, 0:384], b_rel[:, 129:513], writes=["extB_sb"])
            s.dve(lambda e: e.tensor_copy(out=extB_sb[:, 384:768], in_=bcl(extB_sb[:, 383], 384)), writes=["extB_sb"])
            s.dma("sp", extB, extB_sb[:], reads=["extB_sb"], writes=["extB"])
            s.dma("sp", SB_, AP(extB, 0, [[0, 128], [1, 8 * 768]]), reads=["extB"], writes=["SBd"])
            s.dma("sp", t5src[:], t5_table, writes=["t5src"])
            s.pe(lambda e: e.transpose(out=banks[3][0:8, 0:32], in_=t5src[:], identity=identf[0:32, 0:32]),
                 reads=["t5src", "identf"], writes=[bkey[3]])
            s.dve(lambda e: e.tensor_copy(out=t5T[:], in_=banks[3][0:8, 0:32]), writes=[bkey[3], "t5T"])
            for (j0, j1, bu) in t5_runs():
                s.dve(lambda e, j0=j0, j1=j1, bu=bu: e.tensor_copy(out=extA_sb[:, j0:j1], in_=bcl(t5T[:, bu], j1 - j0)),
                      reads=["t5T"], writes=["extA_sb"])
            s.dve(lambda e: e.tensor_copy(out=extA_sb[:, 383:384], in_=t5T[:, 0:1]), reads=["t5T"], writes=["extA_sb"])
            s.dma("sp", extA, extA_sb[:], reads=["extA_sb"], writes=["extA"])
            s.dma("sp", SA_, AP(extA, 0, [[0, 128], [1, 8 * 384]]), reads=["extA"], writes=["SAd"])
            s.dma("sp", c8[0:4, :], cpr, writes=["c8"])
            s.dma("sp", c8[4:8, :], csm, writes=["c8"])
            s.act(lambda e: e.activation(out=sc8[:], in_=c8[:], func=AF.Silu), reads=["c8"], writes=["sc8"])
            tp = banks[0][:].bitcast(BF16)
            for k in range(8):
                s.pe(lambda e, k=k: e.transpose(out=tp[:, k * 8:(k + 1) * 8], in_=sc8[0:8, k * 128:(k + 1) * 128],
                                                identity=ident[0:8, 0:8]),
                     reads=["sc8", "ident"], writes=[bkey[0]])
            s.dve(lambda e: e.tensor_copy(out=cT[:].rearrange("p k b -> p (k b)"), in_=tp[:, 0:64]),
                  writes=[bkey[0], "cT"])
            for k in range(8):
                s.dma("pool", wada[:, k, :], w_ada[k * 128:(k + 1) * 128, :], writes=["wada%d" % k])
            s.dma("sp", colsrc[0:48, :], b_ada.rearrange("(c p) -> c p", p=128), writes=["colsrc"])
            s.dma("sp", colsrc[48:56, :], g_pre_mix.rearrange("(c p) -> c p", p=128), writes=["colsrc"])
            s.dma("sp", colsrc[56:64, :], g_pre_ffn.rearrange("(c p) -> c p", p=128), writes=["colsrc"])
            s.dma("sp", colsrc[64:80, :], b_gate.rearrange("(c p) -> c p", p=128), writes=["colsrc"])
            s.pe(lambda e: e.transpose(out=banks[2][:, 0:80], in_=colsrc[0:80, :], identity=identf[0:80, 0:80]),
                 reads=["colsrc", "identf"], writes=[bkey[2]])
            s.dve(lambda e: e.tensor_copy(out=colT[:], in_=banks[2][:, 0:80]), writes=[bkey[2], "colT"])
            s.dve(lambda e: e.tensor_copy(out=bgT[:], in_=colT[:, 64:80]), reads=["colT"], writes=["bgT"])
            pa_ = banks[1][:, 0:384].rearrange("p (f b) -> p f b", b=8)
            for f in range(48):
                for k in range(8):
                    s.pe(lambda e, f=f, k=k: e.matmul(pa_[:, f, :], lhsT=wada[:, k, f * 128:(f + 1) * 128],
                                                      rhs=cT[:, k, :], start=(k == 0), stop=(k == 7)),
                         reads=["cT", "wada%d" % k], writes=[bkey[1]])
            s.dve(lambda e: e.tensor_tensor(out=adaT[:], in0=pa_, in1=bcl(colT[:, 0:48], 8), op=ALU.add),
                  reads=["colT"], writes=[bkey[1], "adaT"])
            for (Ax, gp, c0, nm) in ((A1, colT[:, 48:56], 8, "A1"), (A2, colT[:, 56:64], 32, "A2")):
                s.dve(lambda e, c0=c0: e.tensor_scalar(out=tmp88[:], in0=adaT[:, c0:c0 + 8, :], scalar1=1.0, scalar2=None,
                                                       op0=ALU.add), reads=["adaT"], writes=["tmp88"])
                s.dve(lambda e, Ax=Ax, gp=gp: e.tensor_tensor(out=Ax[:], in0=tmp88[:], in1=bcl(gp, 8),
                                                              op=ALU.mult), reads=["tmp88", "colT"], writes=[nm])
            for v, c0 in ((0, 2 * D), (1, 5 * D)):
                s.dma("sp", bada8[:, v, :], AP(b_ada, c0, [[0, 8], [1, D]]), writes=["bada8"])
            s.dma("sp", gpost8[:, 0, :], AP(g_post_mix, 0, [[0, 8], [1, D]]), writes=["gpost8"])
            s.dma("sp", gpost8[:, 1, :], AP(g_post_ffn, 0, [[0, 8], [1, D]]), writes=["gpost8"])
            for v, c0 in ((0, 2 * D), (1, 5 * D)):
                for sl in range(2):
                    bk = 2 + (v * 2 + sl) % 2
                    for k in range(8):
                        s.pe(lambda e, k=k, bk=bk, c=c0 + sl * 512: e.matmul(
                            banks[bk][0:8, :], lhsT=cT[:, k, :], rhs=wada[:, k, c:c + 512],
                            start=(k == 0), stop=(k == 7)),
                            reads=["cT", "wada%d" % k], writes=[bkey[bk]])
                    s.dve(lambda e, v=v, sl=sl, bk=bk: e.tensor_tensor(
                        out=G8[:, v, sl * 512:(sl + 1) * 512], in0=banks[bk][0:8, :],
                        in1=bada8[:, v, sl * 512:(sl + 1) * 512], op=ALU.add),
                        reads=["bada8"], writes=[bkey[bk], "G8"])
            s.dve(lambda e: e.tensor_tensor(out=G8[:], in0=G8[:], in1=gpost8[:], op=ALU.mult),
                  reads=["gpost8"], writes=["G8"])
            s.dma("sp", Gd, G8[:], reads=["G8"], writes=["Gd"])
            s.emit()

        with ExitStack() as e1:
            s = Sched(nc, st)
            biasB = T(e1, "biasB", [128, 8, 5, 128], F32)
            biasA = T(e1, "biasA", [128, 8, 2, 128], F32)
            w_in_sb = T(e1, "w_in_sb", [128, 8, 2304], BF16)
            wg_sb = T(e1, "wg_sb", [128, 8, 2048], BF16)
            wpa_sb = T(e1, "wpa_sb", [128, 4, D], BF16)
            wpb_sb = T(e1, "wpb_sb", [128, 4, D], BF16)
            wo_sb = T(e1, "wo_sb", [128, 8, D], BF16)
            xt = [T(e1, "xt%d" % i, [128, 2, D], F32) for i in range(2)]
            xnb = [T(e1, "xn%d" % i, [128, D], BF16) for i in range(2)]
            hTb = [T(e1, "hT%d" % i, [128, 8, 256], BF16) for i in range(2)]
            qaT = T(e1, "qaT", [128, 4, 256], BF16)
            qbT = T(e1, "qbT", [128, 4, 256], BF16)
            kaT = T(e1, "kaT", [128, 1024], BF16)
            kbT = T(e1, "kbT", [128, 4, 1024], BF16)
            va = T(e1, "va", [128, 8, 2, 65], BF16)
            vb = T(e1, "vb", [128, 8, 8, 65], BF16)
            tmpf = T(e1, "tmpf", [128, 512], F32)
            PT = [T(e1, "PT%d" % i, [128, 512], BF16) for i in range(3)]
            on = T(e1, "on", [128, D], BF16)
            junk = on[:]
            oT = T(e1, "oT", [128, 8, 256], BF16)
            sig = T(e1, "sig", [128, 512], F32)
            prod = T(e1, "prod", [128, 512], F32)
            mT = T(e1, "mT", [128, 8, 256], BF16)
            G1bc = T(e1, "G1bc", [128, D], F32)
            stt = T(e1, "stt", [128, 32], F32)
            kst = [sig, prod]
            xt1b = xt[1][:].bitcast(BF16)
            ctile = xt1b[:, 0, :].rearrange("p (t f) -> p t f", t=4)
            catile = xt1b[:, 1, 0:128]

            for g in range(4):
                for kv in range(2):
                    s.dma("pool", w_in_sb[:, :, g * 128 + kv * 64:g * 128 + (kv + 1) * 64],
                          AP(w_in, kv * 256 + g * 64, [[2304, 128], [128 * 2304, 8], [1, 64]]), writes=["w_in"])
            s.dma("pool", w_in_sb[:, :, 512:2304], w_in[:, 512:2304].rearrange("(c p) n -> p c n", p=128), writes=["w_in"])
            s.dma("pool", wg_sb[:], w_gate.rearrange("(c p) n -> p c n", p=128), writes=["wg"])
            s.dma("pool", wpa_sb[:], w_pa.rearrange("(c p) n -> p c n", p=128), writes=["wpa"])
            s.dma("pool", wpb_sb[:], w_pb.rearrange("(c p) n -> p c n", p=128), writes=["wpb"])
            s.dma("pool", wo_sb[:], w_o.rearrange("(c p) n -> p c n", p=128), writes=["wo"])
            s.dma("sp", biasB[:], AP(SB_, 127, [[8 * 768 - 1, 128], [768, 8], [128, 5], [1, 128]]), writes=["biasB"])
            s.dma("sp", biasA[:], AP(SA_, 127, [[8 * 384 - 1, 128], [384, 8], [128, 2], [1, 128]]), writes=["biasA"])
            s.dve(lambda e: e.memset(biasB[64:128, :, 0, 0:64], NEG), writes=["biasB"])
            s.dve(lambda e: e.memset(biasB[0:64, :, 4, 64:128], NEG), writes=["biasB"])
            s.dve(lambda e: e.memset(biasA[64:128, :, 0, 0:64], NEG), writes=["biasA"])
            s.dve(lambda e: e.memset(biasA[0:64, :, 1, 64:128], NEG), writes=["biasA"])
            s.pool(lambda e: e.memset(va[:, :, :, 64:65], 1.0), writes=["va%d" % i for i in range(8)])
            s.pool(lambda e: e.memset(vb[:, :, :, 64:65], 1.0), writes=["vb%d" % i for i in range(8)])

            gen_banks = [4, 5, 6, 7]
            gctr = [0]

            def gbank():
                b = gen_banks[gctr[0] % len(gen_banks)]
                gctr[0] += 1
                return b

            wide_banks = [4, 5, 6, 7, 1, 2, 3]
            wctr = [0]

            def wbank():
                b = wide_banks[wctr[0] % len(wide_banks)]
                wctr[0] += 1
                return b

            sctr = [0]
            evc = [0]

            def evac_copy(out, in_, reads, writes, scale=None):
                evc[0] += 1
                if evc[0] % 2 == 0:
                    if scale is None:
                        s.act(lambda e: e.activation(out=out, in_=in_, func=AF.Copy), reads=reads, writes=writes)
                    else:
                        s.act(lambda e: e.activation(out=out, in_=in_, func=AF.Copy, scale=scale), reads=reads, writes=writes)
                else:
                    if scale is None:
                        s.dve(lambda e: e.tensor_copy(out=out, in_=in_), reads=reads, writes=writes)
                    else:
                        s.dve(lambda e: e.tensor_scalar(out=out, in0=in_, scalar1=scale, scalar2=None, op0=ALU.mult),
                              reads=reads, writes=writes)

            def rstd_from(ms_col, out_col, key):
                s.act(lambda e: e.activation(out=stt[:, 15:16], in_=ms_col, func=AF.Ln, bias=1e-6, scale=1.0),
                      reads=[key], writes=["stt_ln"])
                s.act(lambda e: e.activation(out=out_col, in_=stt[:, 15:16], func=AF.Exp, scale=-0.5),
                      reads=["stt_ln"], writes=[key])

            xctr = [0]

            NSB = 3
            SCB = [1, 2, 3]

            def attention_groups(W0, iA, iB):
                out = []
                oA = [gbank(), gbank()]
                for kv in range(2):
                    ob = oA[kv]
                    ov = banks[ob][:, 0:260].rearrange("p (h e) -> p h e", e=65)
                    dl = [d_ for d_ in (1, 0) if iA - d_ >= 0]
                    for di, d_ in enumerate(dl):
                        slot = (iA - d_) % 8
                        pb0 = kv * 64
                        first = (di == 0)
                        last = (di == len(dl) - 1)

                        def S(i2, slot=slot, pb0=pb0):
                            sb = SCB[i2]
                            s.pe(lambda e: e.matmul(
                                banks[sb][:].rearrange("p (g t) -> p g t", g=4),
                                lhsT=kaT[pb0:pb0 + 64, slot * 128:(slot + 1) * 128],
                                rhs=qaT[pb0:pb0 + 64, :, W0:W0 + 128], start=True, stop=True),
                                reads=["kaT%d" % slot, "qaT"], writes=[bkey[sb]])

                        def E(i2, kv=kv, d_=d_):
                            sb = SCB[i2]
                            s.dve(lambda e: e.tensor_tensor(
                                out=banks[sb][:].rearrange("p (g t) -> p g t", g=4),
                                in0=banks[sb][:].rearrange("p (g t) -> p g t", g=4),
                                in1=biasA[:, kv * 4:(kv + 1) * 4, d_, :], op=ALU.add),
                                reads=["biasA"], writes=[bkey[sb]])
                            s.act(lambda e: e.activation(out=PT[i2][:], in_=banks[sb][:], func=AF.Exp),
                                  writes=[bkey[sb], "PT%d" % i2])

                        def V(i2, slot=slot, kv=kv, ov=ov, ob=ob, first=first, last=last):
                            for g in range(4):
                                s.pe(lambda e, g=g: e.matmul(
                                    ov[:, g, :], lhsT=PT[i2][:, g * 128:(g + 1) * 128], rhs=va[:, slot, kv, :],
                                    start=(first and g == 0), stop=False, skip_group_check=True),
                                    reads=["PT%d" % i2, "va%d" % slot], writes=[bkey[ob]])
                            if last:
                                s.dve(lambda e: e.tensor_tensor(
                                    out=stt[:, 0:4], in0=ov[:, :, 64], in1=expsink[:, kv * 4:(kv + 1) * 4], op=ALU.add),
                                    reads=["expsink"], writes=[bkey[ob], "stt_a"])
                                s.dve(lambda e: e.reciprocal(out=stt[:, 4:8], in_=stt[:, 0:4]), reads=["stt_a"], writes=["stt_b"])
                                s.dve(lambda e: e.tensor_tensor(
                                    out=on[:, kv * 256:(kv + 1) * 256].rearrange("p (h d) -> p h d", d=64),
                                    in0=ov[:, :, 0:64], in1=bcl(stt[:, 4:8], 64), op=ALU.mult),
                                    reads=["stt_b"], writes=[bkey[ob], "on"])
                        out.append((S, E, V))
                oB = [gbank(), gbank()]
                blocks = [(h, d_) for h in range(8) for d_ in range(5) if iB - d_ >= 0]
                groups = []
                for blk in blocks:
                    if groups and len(groups[-1]) < 4 and groups[-1][-1][0] == blk[0]:
                        groups[-1].append(blk)
                    else:
                        groups.append([blk])
                nB = len(groups)
                for gi_, grp in enumerate(groups):
                    n = len(grp)
                    h = grp[0][0]
                    d_lo = grp[0][1]
                    ob = oB[h // 4]
                    ov = banks[ob][:, 0:260].rearrange("p (h e) -> p h e", e=65)
                    firstbank = all(g2[0][0] // 4 != h // 4 for g2 in groups[:gi_])
                    lastbank = all(g2[0][0] // 4 != h // 4 for g2 in groups[gi_ + 1:])

                    def S(i2, grp=grp):
                        sb = SCB[i2]
                        for j, (h_, d_) in enumerate(grp):
                            slot = (iB - d_) % 8
                            c, pb0 = h_ // 2, (h_ % 2) * 64
                            s.pe(lambda e, j=j, slot=slot, c=c, pb0=pb0: e.matmul(
                                banks[sb][:, j * 128:(j + 1) * 128],
                                lhsT=kbT[pb0:pb0 + 64, c, slot * 128:(slot + 1) * 128],
                                rhs=qbT[pb0:pb0 + 64, c, W0:W0 + 128], start=True, stop=True),
                                reads=["kbT%d" % slot, "qbT"], writes=[bkey[sb]])

                    def E(i2, n=n, h=h, d_lo=d_lo):
                        sb = SCB[i2]
                        s.dve(lambda e: e.tensor_tensor(
                            out=banks[sb][:, 0:n * 128].rearrange("p (g t) -> p g t", g=n),
                            in0=banks[sb][:, 0:n * 128].rearrange("p (g t) -> p g t", g=n),
                            in1=biasB[:, h, d_lo:d_lo + n, :], op=ALU.add),
                            reads=["biasB"], writes=[bkey[sb]])
                        s.act(lambda e: e.activation(out=PT[i2][:, 0:n * 128], in_=banks[sb][:, 0:n * 128], func=AF.Exp),
                              writes=[bkey[sb], "PT%d" % i2])

                    def V(i2, grp=grp, h=h, ov=ov, ob=ob, firstbank=firstbank, lastbank=lastbank):
                        for j, (h_, d_) in enumerate(grp):
                            slot = (iB - d_) % 8
                            s.pe(lambda e, j=j, slot=slot: e.matmul(
                                ov[:, h % 4, :], lhsT=PT[i2][:, j * 128:(j + 1) * 128], rhs=vb[:, slot, h, :],
                                start=(firstbank and j == 0), stop=False, skip_group_check=True),
                                reads=["PT%d" % i2, "vb%d" % slot], writes=[bkey[ob]])
                        if lastbank:
                            hh = h // 4
                            s.dve(lambda e: e.reciprocal(out=stt[:, 8:12], in_=ov[:, :, 64]), writes=[bkey[ob], "stt_c"])
                            s.dve(lambda e: e.tensor_tensor(
                                out=on[:, 512 + hh * 256:512 + (hh + 1) * 256].rearrange("p (h d) -> p h d", d=64),
                                in0=ov[:, :, 0:64], in1=bcl(stt[:, 8:12], 64), op=ALU.mult),
                                reads=["stt_c"], writes=[bkey[ob], "on"])
                    out.append((S, E, V))

                def Vt(i2):
                    tpv = banks[0][:].bitcast(BF16).rearrange("p (c t) -> p c t", c=8)
                    for c in range(8):
                        s.pe(lambda e, c=c: e.transpose(out=tpv[:, c, :], in_=on[:, c * 128:(c + 1) * 128], identity=ident[:]),
                             reads=["on", "ident"], writes=[bkey[0]])
                    evac_copy(oT[:, :, W0:W0 + 128], tpv, reads=[], writes=[bkey[0], "oT"])
                out.append((None, None, Vt))
                return out

            def run_attention(glist, L=2):
                pend = []
                for (S, E, V) in glist:
                    if S is not None:
                        i2 = sctr[0] % NSB
                        sctr[0] += 1
                        S(i2)
                        E(i2)
                    else:
                        i2 = None
                    pend.append((V, i2))
                    if len(pend) > L:
                        V0, j2 = pend.pop(0)
                        V0(j2)
                while pend:
                    V0, j2 = pend.pop(0)
                    V0(j2)

            xloaded = set()

            def xload(kind, b, s0, nt, n):
                slot_x = (n % 2) if kind == "p" else 0
                xs = xt[slot_x]
                xkey = "xt%d" % slot_x
                xloaded.add(n)
                if kind == "p":
                    s.dma("sp", xs[:, 0:nt, :], xp[b, s0 * 128:(s0 + nt) * 128, :].rearrange("(t p) d -> p t d", p=128),
                          writes=[xkey])
                else:
                    s.dma("sp", xs[0:64, 0, :], xsm[b, :, :], writes=[xkey])

            def prep(kind, b, s0, nt, n):
                slot_x = (n % 2) if kind == "p" else 0
                xs = xt[slot_x]
                xkey = "xt%d" % slot_x
                hT = hTb[n % 2]
                hkey = "hT%d" % (n % 2)
                bb = b if kind == "p" else 4 + b
                if n not in xloaded:
                    xload(kind, b, s0, nt, n)
                for t in range(nt):
                    xn = xnb[t % 2]
                    xnk = "xn%d" % (t % 2)
                    s.act(lambda e, t=t: e.activation(out=junk, in_=xs[:, t, :], func=AF.Square, scale=1.0 / 32,
                                                      accum_out=stt[:, 12:13]), reads=[xkey], writes=["on", "stt_ms"])
                    rstd_from(stt[:, 12:13], stt[:, 13:14], "stt_ms")
                    s.dve(lambda e, t=t, xn=xn: e.tensor_scalar(out=xn[:], in0=xs[:, t, :], scalar1=stt[:, 13:14], scalar2=None,
                                                                op0=ALU.mult), reads=[xkey, "stt_ms"], writes=[xnk])
                    yield
                for t in range(nt):
                    xn = xnb[t % 2]
                    xnk = "xn%d" % (t % 2)
                    tpv = banks[0][:].bitcast(BF16).rearrange("p (c t) -> p c t", c=8)
                    for c in range(8):
                        s.pe(lambda e, c=c, tpv=tpv, xn=xn: e.transpose(out=tpv[:, c, :], in_=xn[:, c * 128:(c + 1) * 128],
                                                                        identity=ident[:]),
                             reads=[xnk, "ident"], writes=[bkey[0]])
                    for c in range(8):
                        if c % 2 == 0:
                            s.act(lambda e, c=c, t=t, tpv=tpv: e.activation(
                                out=hT[:, c, t * 128:(t + 1) * 128], in_=tpv[:, c, :], func=AF.Identity,
                                scale=A1[:, c, bb:bb + 1], bias=adaT[:, c, bb:bb + 1]),
                                reads=["A1", "adaT"], writes=[bkey[0], hkey])
                        else:
                            s.dve(lambda e, c=c, t=t, tpv=tpv: e.tensor_scalar(
                                out=hT[:, c, t * 128:(t + 1) * 128], in0=tpv[:, c, :],
                                scalar1=A1[:, c, bb:bb + 1], scalar2=adaT[:, c, bb:bb + 1], op0=ALU.mult, op1=ALU.add),
                                reads=["A1", "adaT"], writes=[bkey[0], hkey])
                    yield

            def supertile(kind, b, s0, nt, n, nxt_gen):
                W = 128 * nt
                hA = 0 if kind == "p" else 1
                hB = 0 if kind == "p" else 4
                slot_x = (n % 2) if kind == "p" else 0
                xs = xt[slot_x]
                xkey = "xt%d" % slot_x
                hT = hTb[n % 2]
                hkey = "hT%d" % (n % 2)
                bb = b if kind == "p" else 4 + b
                slotA0 = (s0 + hA) % 8
                slotB0 = (s0 + hB) % 8
                ka_keys = ["kaT%d" % ((slotA0 + t) % 8) for t in range(nt)]
                kb_keys = ["kbT%d" % ((slotB0 + t) % 8) for t in range(nt)]
                jobs = [
                    ([0, 128], qaT[:, 0:2, 0:W], 0.125, ["qaT"]),
                    ([256, 384], qaT[:, 2:4, 0:W], 0.125, ["qaT"]),
                    ([768, 896], qbT[:, 0:2, 0:W], 0.125, ["qbT"]),
                    ([1024, 1152], qbT[:, 2:4, 0:W], 0.125, ["qbT"]),
                    ([1280, 1408], kbT[:, 0:2, slotB0 * 128:slotB0 * 128 + W], None, kb_keys),
                    ([1536, 1664], kbT[:, 2:4, slotB0 * 128:slotB0 * 128 + W], None, kb_keys),
                    ([512], kaT[:, slotA0 * 128:slotA0 * 128 + W], None, ka_keys),
                ]
                for cols, dest, scale, wkeys in jobs:
                    bk = wbank()
                    pv = banks[bk][:, 0:len(cols) * W].rearrange("p (j w) -> p j w", j=len(cols))
                    for j, c0 in enumerate(cols):
                        for k in range(8):
                            s.pe(lambda e, pv=pv, j=j, c0=c0, k=k: e.matmul(
                                pv[:, j, :], lhsT=w_in_sb[:, k, c0:c0 + 128], rhs=hT[:, k, 0:W],
                                start=(k == 0), stop=(k == 7)),
                                reads=[hkey, "w_in"], writes=[bkey[bk]])
                    src = pv if len(cols) == 2 else pv[:, 0, :]
                    evac_copy(dest, src, reads=[], writes=[bkey[bk]] + wkeys, scale=scale)
                for t in range(nt):
                    ti = s0 + t
                    sA = (ti + hA) % 8
                    sB = (ti + hB) % 8
                    bk = wbank()
                    for k in range(8):
                        s.pe(lambda e, bk=bk, k=k, t=t: e.matmul(
                            banks[bk][:], lhsT=hT[:, k, t * 128:(t + 1) * 128], rhs=w_in_sb[:, k, 1792:2304],
                            start=(k == 0), stop=(k == 7)), reads=[hkey, "w_in"], writes=[bkey[bk]])
                    evac_copy(vb[:, sB, :, 0:64], banks[bk][:].rearrange("p (h d) -> p h d", d=64),
                              reads=[], writes=[bkey[bk], "vb%d" % sB])
                    outB = (kind == "p" and ti >= 12) or kind == "s"
                    outA = (kind == "p" and ti == 15) or kind == "s"
                    if outB:
                        s.dve(lambda e, bk=bk: e.tensor_copy(out=kst[0][:], in_=banks[bk][:]), writes=[bkey[bk], "sig"])
                        if kind == "p":
                            s.dma("sp", nbvp[b, (ti - 12) * 128:(ti - 11) * 128, :], kst[0][:], reads=["sig"])
                        else:
                            s.dma("sp", nbvs[b, 448:512, :], kst[0][0:64, :], reads=["sig"])
                            s.dma("sp", nbvs[b, 0:448, :], cbv[b, 64:512, :])
                        bk2 = wbank()
                        for k in range(8):
                            s.pe(lambda e, bk2=bk2, k=k, t=t: e.matmul(
                                banks[bk2][:], lhsT=hT[:, k, t * 128:(t + 1) * 128], rhs=w_in_sb[:, k, 1280:1792],
                                start=(k == 0), stop=(k == 7)), reads=[hkey, "w_in"], writes=[bkey[bk2]])
                        s.act(lambda e, bk2=bk2: e.activation(out=kst[1][:], in_=banks[bk2][:], func=AF.Copy),
                              writes=[bkey[bk2], "prod"])
                        if kind == "p":
                            s.dma("sp", nbkp[b, (ti - 12) * 128:(ti - 11) * 128, :], kst[1][:], reads=["prod"])
                        else:
                            s.dma("sp", nbks[b, 448:512, :], kst[1][0:64, :], reads=["prod"])
                            s.dma("sp", nbks[b, 0:448, :], cbk[b, 64:512, :])
                    bk = wbank()
                    for k in range(8):
                        s.pe(lambda e, bk=bk, k=k, t=t: e.matmul(
                            banks[bk][:, 0:128], lhsT=hT[:, k, t * 128:(t + 1) * 128], rhs=w_in_sb[:, k, 640:768],
                            start=(k == 0), stop=(k == 7)), reads=[hkey, "w_in"], writes=[bkey[bk]])
                    if outA:
                        for k in range(8):
                            s.pe(lambda e, bk=bk, k=k, t=t: e.matmul(
                                banks[bk][:, 128:256], lhsT=hT[:, k, t * 128:(t + 1) * 128], rhs=w_in_sb[:, k, 512:640],
                                start=(k == 0), stop=(k == 7)), reads=[hkey, "w_in"], writes=[bkey[bk]])
                    evac_copy(va[:, sA, :, 0:64], banks[bk][:, 0:128].rearrange("p (h d) -> p h d", d=64),
                              reads=[], writes=[bkey[bk], "va%d" % sA])
                    if outA:
                        s.dve(lambda e, bk=bk: e.tensor_copy(out=tmpf[:, 0:256], in_=banks[bk][:, 0:256]),
                              writes=[bkey[bk], "tmpf"])
                        if kind == "p":
                            s.dma("sp", navp[b, :, :], tmpf[:, 0:128], reads=["tmpf"])
                            s.dma("sp", nakp[b, :, :], tmpf[:, 128:256], reads=["tmpf"])
                        else:
                            s.dma("sp", navs[b, 64:128, :], tmpf[0:64, 0:128], reads=["tmpf"])
                            s.dma("sp", naks[b, 64:128, :], tmpf[0:64, 128:256], reads=["tmpf"])
                            s.dma("sp", navs[b, 0:64, :], cav[b, 64:128, :])
                            s.dma("sp", naks[b, 0:64, :], cak[b, 64:128, :])
                gl = []
                for t in range(nt):
                    gl += attention_groups(t * 128, s0 + t + hA, s0 + t + hB)
                run_attention(gl)
                if nxt_gen is not None:
                    for _ in range(2):
                        next(nxt_gen, None)
                for f in range(8):
                    bz = wbank()
                    bp = wbank()
                    zv = banks[bz][:, 0:2 * W].rearrange("p (j w) -> p j w", j=2)
                    pv = banks[bp][:, 0:2 * W].rearrange("p (j w) -> p j w", j=2)
                    for j in range(2):
                        for k in range(8):
                            s.pe(lambda e, zv=zv, j=j, k=k, f=f: e.matmul(
                                zv[:, j, :], lhsT=wg_sb[:, k, j * 1024 + f * 128:j * 1024 + (f + 1) * 128],
                                rhs=hT[:, k, 0:W], start=(k == 0), stop=(k == 7)),
                                reads=[hkey, "wg"], writes=[bkey[bz]])
                    for j, wsb in enumerate((wpa_sb, wpb_sb)):
                        for k in range(4):
                            s.pe(lambda e, pv=pv, j=j, k=k, f=f, wsb=wsb: e.matmul(
                                pv[:, j, :], lhsT=wsb[:, k, f * 128:(f + 1) * 128], rhs=oT[:, j * 4 + k, 0:W],
                                start=(k == 0), stop=(k == 3)),
                                reads=["oT", "wpa", "wpb"], writes=[bkey[bp]])
                    for j in range(2):
                        s.act(lambda e, zv=zv, j=j, f=f: e.activation(
                            out=sig[:, j * W:(j + 1) * W], in_=zv[:, j, :], func=AF.Sigmoid,
                            bias=bgT[:, j * 8 + f:j * 8 + f + 1]), reads=["bgT"], writes=[bkey[bz], "sig"])
                    s.dve(lambda e, bp=bp: e.tensor_tensor(out=prod[:, 0:2 * W], in0=banks[bp][:, 0:2 * W],
                                                           in1=sig[:, 0:2 * W], op=ALU.mult),
                          reads=["sig"], writes=[bkey[bp], "prod"])
                    s.dve(lambda e, f=f: e.tensor_tensor(out=mT[:, f, 0:W], in0=prod[:, 0:W], in1=prod[:, W:2 * W],
                                                         op=ALU.add), reads=["prod"], writes=["mT"])
                    if nxt_gen is not None and f in (1, 4):
                        next(nxt_gen, None)
                for t in range(nt):
                    ti = s0 + t
                    bks = [wbank(), wbank()]
                    for sl in range(2):
                        for k in range(8):
                            s.pe(lambda e, sl=sl, k=k, t=t, bk=bks[sl]: e.matmul(
                                banks[bk][:], lhsT=mT[:, k, t * 128:(t + 1) * 128], rhs=wo_sb[:, k, sl * 512:(sl + 1) * 512],
                                start=(k == 0), stop=(k == 7)), reads=["mT", "wo"], writes=[bkey[bks[sl]]])
                    for sl in range(2):
                        s.act(lambda e, sl=sl, bk=bks[sl]: e.activation(
                            out=junk[:, 0:512], in_=banks[bk][:], func=AF.Square, scale=1.0 / 32,
                            accum_out=(stt[:, 10:11] if sl == 0 else stt[:, 11:12])),
                            writes=[bkey[bks[sl]], "on", "stt_w%d" % sl])
                    s.dve(lambda e: e.tensor_tensor(out=stt[:, 14:15], in0=stt[:, 10:11], in1=stt[:, 11:12], op=ALU.add),
                          reads=["stt_w0", "stt_w1"], writes=["stt_w"])
                    rstd_from(stt[:, 14:15], stt[:, 14:15], "stt_w")
                    for sl in range(2):
                        s.dve(lambda e, sl=sl, bk=bks[sl]: e.scalar_tensor_tensor(
                            out=tmpf[:], in0=banks[bk][:], scalar=stt[:, 14:15], in1=G1bc[:, sl * 512:(sl + 1) * 512],
                            op0=ALU.mult, op1=ALU.mult), reads=["stt_w", "G1bc"], writes=[bkey[bks[sl]], "tmpf"])
                        s.dve(lambda e, sl=sl, t=t: e.tensor_tensor(
                            out=xs[:, t, sl * 512:(sl + 1) * 512], in0=tmpf[:], in1=xs[:, t, sl * 512:(sl + 1) * 512],
                            op=ALU.add), reads=["tmpf"], writes=[xkey])
                    gt = (b * 16 + ti) if kind == "p" else (64 + b)
                    s.dma("sp", X1d[gt * 128:(gt + 1) * 128, :], xs[:, t, :], reads=[xkey], writes=["X1d"])
                    s.act(lambda e, t=t: e.activation(out=junk, in_=xs[:, t, :], func=AF.Square, scale=1.0 / 32,
                                                      accum_out=stt[:, 16:17]), reads=[xkey], writes=["on", "stt_x1"])
                    s.act(lambda e: e.activation(out=stt[:, 17:18], in_=stt[:, 16:17], func=AF.Ln, bias=1e-6, scale=1.0),
                          reads=["stt_x1"], writes=["stt_x1ln"])
                    s.act(lambda e, gt=gt: e.activation(out=rstd_all[:, gt:gt + 1], in_=stt[:, 17:18], func=AF.Exp, scale=-0.5),
                          reads=["stt_x1ln"], writes=["rstd_all"])

            stiles = [("p", b, s0, 2) for b in range(NB) for s0 in range(0, 16, 2)] + [("s", b, 0, 1) for b in range(NB)]

            def sample_history(b):
                s.dma("pool", ctile, cbk[b].rearrange("(t p) f -> p t f", p=128), writes=["xt1"])
                s.dma("pool", catile, cak[b], writes=["xt1"])
                for t in range(4):
                    s.dma("pool", vb[:, t, :, 0:64], cbv[b, t * 128:(t + 1) * 128, :].rearrange("p (h d) -> p h d", d=64),
                          writes=["vb%d" % t])
                s.dma("pool", va[:, 0, :, 0:64], cav[b].rearrange("p (h d) -> p h d", d=64), writes=["va0"])
                tpv = banks[0][:].bitcast(BF16).rearrange("p (c t) -> p c t", c=8)
                for t in range(4):
                    for c in range(4):
                        s.pe(lambda e, t=t, c=c, tpv=tpv: e.transpose(out=tpv[:, c, :], in_=ctile[:, t, c * 128:(c + 1) * 128],
                                                                      identity=ident[:]),
                             reads=["xt1", "ident"], writes=[bkey[0]])
                    evac_copy(kbT[:, :, t * 128:(t + 1) * 128], tpv[:, 0:4, :], reads=[], writes=[bkey[0], "kbT%d" % t])
                s.pe(lambda e, tpv=tpv: e.transpose(out=tpv[:, 0, :], in_=catile, identity=ident[:]),
                     reads=["xt1", "ident"], writes=[bkey[0]])
                evac_copy(kaT[:, 0:128], tpv[:, 0, :], reads=[], writes=[bkey[0], "kaT0"])

            cur_gen = prep(*stiles[0], 0)
            for _ in cur_gen:
                pass
            for n, (kind, b, s0, nt) in enumerate(stiles):
                if s0 == 0:
                    bb_ = b if kind == "p" else 4 + b
                    s.dma("sp", G1bc[:], AP(Gd, bb_ * 2 * D, [[0, 128], [1, D]]), writes=["G1bc"])
                if kind == "s":
                    sample_history(b)
                nxt = stiles[n + 1] if n + 1 < len(stiles) else None
                inter = nxt is not None and nxt[0] == "p"
                if inter:
                    xload(*nxt, n + 1)
                g = prep(*nxt, n + 1) if nxt is not None else None
                supertile(kind, b, s0, nt, n, g if inter else None)
                if g is not None:
                    for _ in g:
                        pass
            print("phase1 sbuf remaining", nc.sbuf_bytes_remaining)
            s.emit()

        with ExitStack() as e2:
            s = Sched(nc, st)
            h2T = [T(e2, "h2T%d" % i, [128, 8, 1024], BF16) for i in range(2)]
            yacc = T(e2, "yacc", [128, 8, D], F32)
            he = [T(e2, "he%d" % i, [128, 4, 1024], BF16) for i in range(2)]
            sg = [T(e2, "sg%d" % i, [128, 512], BF16) for i in range(2)]
            sgc = [T(e2, "sgc%d" % i, [128, 512], BF16) for i in range(2)]
            cb_sb = [T(e2, "cb_sb%d" % i, [128, 2, 1024], BF16) for i in range(2)]
            combT = T(e2, "combT", [32, 1024], F32)
            chi = [T(e2, "chi%d" % i, [32, 1024], BF16) for i in range(2)]
            wg2 = [T(e2, "wg2_%d" % i, [128, 8, 512], BF16) for i in range(2)]
            wu2 = [T(e2, "wu2_%d" % i, [128, 8, 512], BF16) for i in range(2)]
            wd2 = [T(e2, "wd2_%d" % i, [128, 4, D], BF16) for i in range(2)]
            xin = [T(e2, "xin%d" % i, [128, D], F32) for i in range(2)]
            xfin = [T(e2, "xfin%d" % i, [128, D], F32) for i in range(2)]
            G2t = [T(e2, "G2t%d" % i, [128, D], F32) for i in range(2)]
            xn2 = [T(e2, "xn2_%d" % i, [128, D], BF16) for i in range(2)]
            junk2 = T(e2, "junk2", [128, D], BF16)
            sel = T(e2, "sel", [32, 32, 128], BF16)
            rs = T(e2, "rs", [128, 8, 16], F32)
            LG = T(e2, "LG", [128, 8, 36], F32)
            ohg = T(e2, "ohg", [128, 8, 4], F32)
            rtmp = T(e2, "rtmp", [128, 8, 32], F32)
            r8 = T(e2, "r8", [128, 6, 8, 8], F32)
            comb = T(e2, "comb", [128, 8, 32], F32)
            fs = T(e2, "fs", [128, 24], F32)
            tmp2 = T(e2, "tmp2", [128, D], F32)
            print("phase2 sbuf remaining", nc.sbuf_bytes_remaining)

            s.dve(lambda e: e.tensor_copy(out=sel[:], in_=bcl(identf[0:32, 0:32], 128)), writes=["sel"])

            groups = []
            for b in range(NB):
                for half in range(2):
                    groups.append([("p", b, half * 8 + i) for i in range(8)])
            groups.append([("s", b, 0) for b in range(NB)])

            def gtile(kind, b, ti):
                return (b * 16 + ti) if kind == "p" else (64 + b)

            xic = [0]

            def prologue(gi):
                grp = groups[gi]
                ntl = len(grp)
                G = ntl * 128
                hb = gi % 2
                H = h2T[hb]
                base_x = xic[0]
                xic[0] += ntl

                def x1load(i):
                    kind, b, ti = grp[i]
                    gt = gtile(kind, b, ti)
                    xs_ = (base_x + i) % 2
                    s.dma("sp", xin[xs_][:], X1d[gt * 128:(gt + 1) * 128, :], writes=["xin%d" % xs_])

                x1load(0)
                for i, (kind, b, ti) in enumerate(grp):
                    bb = b if kind == "p" else 4 + b
                    xs_ = (base_x + i) % 2
                    xk = "xin%d" % xs_
                    gt = gtile(kind, b, ti)
                    if i + 1 < ntl:
                        x1load(i + 1)
                    s.dve(lambda e, xs_=xs_, gt=gt: e.tensor_scalar(out=xn2[xs_][:], in0=xin[xs_][:],
                                                                    scalar1=rstd_all[:, gt:gt + 1], scalar2=None, op0=ALU.mult),
                          reads=[xk], writes=["xn2_%d" % xs_])
                    yield
                    tb = xs_
                    tpv = banks[tb][:].bitcast(BF16).rearrange("p (c t) -> p c t", c=8)
                    for c in range(8):
                        s.pe(lambda e, c=c, tpv=tpv, xs_=xs_: e.transpose(out=tpv[:, c, :], in_=xn2[xs_][:, c * 128:(c + 1) * 128],
                                                                          identity=ident[:]),
                             reads=["xn2_%d" % xs_, "ident"], writes=[bkey[tb]])
                    for c in range(8):
                        if c % 2 == 0:
                            s.act(lambda e, c=c, i=i, tpv=tpv, bb=bb: e.activation(
                                out=H[:, c, i * 128:(i + 1) * 128], in_=tpv[:, c, :], func=AF.Identity,
                                scale=A2[:, c, bb:bb + 1], bias=adaT[:, 24 + c, bb:bb + 1]),
                                reads=["A2", "adaT"], writes=[bkey[tb], "h2T%d_%d" % (hb, i)])
                        else:
                            s.dve(lambda e, c=c, i=i, tpv=tpv, bb=bb: e.tensor_scalar(
                                out=H[:, c, i * 128:(i + 1) * 128], in0=tpv[:, c, :],
                                scalar1=A2[:, c, bb:bb + 1], scalar2=adaT[:, 24 + c, bb:bb + 1], op0=ALU.mult, op1=ALU.add),
                                reads=["A2", "adaT"], writes=[bkey[tb], "h2T%d_%d" % (hb, i)])
                    yield
                lgp = banks[1][:, 0:ntl * 36].rearrange("p (t x) -> p t x", x=36)
                for i in range(ntl):
                    for k in range(8):
                        s.pe(lambda e, k=k, i=i: e.matmul(lgp[:, i, :], lhsT=H[:, k, i * 128:(i + 1) * 128],
                                                          rhs=wr_sb[:, k, :], start=(k == 0), stop=(k == 7)),
                             reads=["h2T%d_%d" % (hb, i), "wr"], writes=[bkey[1]])
                    if i % 4 == 3:
                        yield
                R = ["rt"]
                Tn = ntl
                lgv = LG[:, 0:Tn, :]
                rb3 = bass.AP(rbias, rbias[:].offset, [list(rbias[:].ap[0]), [0, Tn], [1, 36]])
                s.dve(lambda e: e.tensor_tensor(out=lgv, in0=lgp, in1=rb3, op=ALU.add), reads=["rbias"], writes=[bkey[1]] + R)
                gmax = rs[:, 0:Tn, 3]
                s.dve(lambda e: e.tensor_reduce(out=gmax, in_=LG[:, 0:Tn, 0:4], axis=AX.X, op=ALU.max), writes=R)
                s.dve(lambda e: e.tensor_tensor(out=rtmp[:, 0:Tn, 0:4], in0=LG[:, 0:Tn, 0:4], in1=bcl(gmax, 4), op=ALU.subtract),
                      writes=R)
                s.act(lambda e: e.activation(out=rtmp[:, 0:Tn, 4:8], in_=rtmp[:, 0:Tn, 0:4], func=AF.Exp), writes=R)
                s.dve(lambda e: e.tensor_reduce(out=rs[:, 0:Tn, 4], in_=rtmp[:, 0:Tn, 4:8], axis=AX.X, op=ALU.add), writes=R)
                s.dve(lambda e: e.reciprocal(out=rs[:, 0:Tn, 5], in_=rs[:, 0:Tn, 4]), writes=R)
                s.dve(lambda e: e.tensor_tensor(out=ohg[:, 0:Tn, :], in0=LG[:, 0:Tn, 0:4], in1=bcl(gmax, 4), op=ALU.is_equal),
                      writes=R)
                yield
                s.dve(lambda e: e.tensor_tensor(out=rtmp[:, 0:Tn, :].rearrange("p t (g x) -> p t g x", g=4),
                                                in0=LG[:, 0:Tn, 4:36].rearrange("p t (g x) -> p t g x", g=4),
                                                in1=bcl(ohg[:, 0:Tn, :], 8), op=ALU.mult), writes=R)
                esel, oh1, msk, oh2, c8, t8 = (r8[:, j, 0:Tn, :] for j in range(6))
                s.dve(lambda e: e.tensor_reduce(out=esel, in_=rtmp[:, 0:Tn, :].rearrange("p t (g x) -> p t x g", g=4),
                                                axis=AX.X, op=ALU.add), writes=R)
                m1 = rs[:, 0:Tn, 6]
                m2 = rs[:, 0:Tn, 7]
                s.dve(lambda e: e.tensor_reduce(out=m1, in_=esel, axis=AX.X, op=ALU.max), writes=R)
                s.dve(lambda e: e.tensor_tensor(out=oh1, in0=esel, in1=bcl(m1, 8), op=ALU.is_equal), writes=R)
                s.dve(lambda e: e.scalar_tensor_tensor(out=msk, in0=oh1, scalar=-1e9, in1=esel, op0=ALU.mult, op1=ALU.add),
                      writes=R)
                s.dve(lambda e: e.tensor_reduce(out=m2, in_=msk, axis=AX.X, op=ALU.max), writes=R)
                s.dve(lambda e: e.tensor_tensor(out=oh2, in0=msk, in1=bcl(m2, 8), op=ALU.is_equal), writes=R)
                yield
                s.dve(lambda e: e.tensor_tensor(out=rs[:, 0:Tn, 8], in0=m2, in1=m1, op=ALU.subtract), writes=R)
                s.act(lambda e: e.activation(out=rs[:, 0:Tn, 9], in_=rs[:, 0:Tn, 8], func=AF.Exp), writes=R)
                s.dve(lambda e: e.tensor_scalar(out=rs[:, 0:Tn, 10], in0=rs[:, 0:Tn, 9], scalar1=1.0, scalar2=None, op0=ALU.add),
                      writes=R)
                s.dve(lambda e: e.reciprocal(out=rs[:, 0:Tn, 11], in_=rs[:, 0:Tn, 10]), writes=R)
                s.dve(lambda e: e.tensor_tensor(out=rs[:, 0:Tn, 12], in0=rs[:, 0:Tn, 11], in1=rs[:, 0:Tn, 5], op=ALU.mult),
                      writes=R)
                s.dve(lambda e: e.tensor_tensor(out=rs[:, 0:Tn, 13], in0=rs[:, 0:Tn, 12], in1=rs[:, 0:Tn, 9], op=ALU.mult),
                      writes=R)
                s.dve(lambda e: e.tensor_tensor(out=c8, in0=oh1, in1=bcl(rs[:, 0:Tn, 12], 8), op=ALU.mult), writes=R)
                s.dve(lambda e: e.tensor_tensor(out=t8, in0=oh2, in1=bcl(rs[:, 0:Tn, 13], 8), op=ALU.mult), writes=R)
                s.dve(lambda e: e.tensor_tensor(out=c8, in0=c8, in1=t8, op=ALU.add), writes=R)
                c8b = bass.AP(r8, r8[:, 4, 0:Tn, :].offset, [list(r8[:].ap[0]), [8, Tn], [0, 4], [1, 8]])
                s.dve(lambda e: e.tensor_tensor(out=comb[:, 0:Tn, :].rearrange("p t (g x) -> p t g x", g=4),
                                                in0=bcl(ohg[:, 0:Tn, :], 8), in1=c8b, op=ALU.mult), writes=R + ["comb"])
                yield
                for i in range(ntl):
                    tb = 1 - (i // 4)
                    s.pe(lambda e, i=i, tb=tb: e.transpose(out=banks[tb][0:32, (i % 4) * 128:(i % 4 + 1) * 128],
                                                           in_=comb[:, i, :], identity=identf[:]),
                         reads=["comb", "identf"], writes=[bkey[tb]])
                for half in range((ntl + 3) // 4):
                    tb = 1 - half
                    s.act(lambda e, half=half, tb=tb: e.activation(out=combT[:, half * 512:(half + 1) * 512],
                                                                   in_=banks[tb][0:32, :], func=AF.Copy),
                          writes=[bkey[tb], "combT"])
                s.dve(lambda e: e.tensor_copy(out=chi[hb][:, 0:G], in_=combT[:, 0:G]), reads=["combT"], writes=["chi%d" % hb])
                yield

            fic = [0]

            def finalize(gi):
                grp = groups[gi]
                ntl = len(grp)
                base_f = fic[0]
                fic[0] += ntl

                def floads(i):
                    kind, b, ti = grp[i]
                    fsl = (base_f + i) % 2
                    bb = b if kind == "p" else 4 + b
                    gt = gtile(kind, b, ti)
                    s.dma("sp", xfin[fsl][:], X1d[gt * 128:(gt + 1) * 128, :], writes=["xfin%d" % fsl])
                    s.dma("sp", G2t[fsl][:], AP(Gd, (bb * 2 + 1) * D, [[0, 128], [1, D]]), writes=["G2t%d" % fsl])

                floads(0)
                for i in range(ntl):
                    s.act(lambda e, i=i: e.activation(out=junk2[:], in_=yacc[:, i, :], func=AF.Square, scale=1.0 / 32,
                                                      accum_out=fs[:, i:i + 1]), reads=["yacc%d" % i], writes=["junk2", "fs_ms"])
                    if i % 2 == 1:
                        yield
                s.act(lambda e: e.activation(out=fs[:, 8:8 + ntl], in_=fs[:, 0:ntl], func=AF.Ln, bias=1e-6, scale=1.0),
                      reads=["fs_ms"], writes=["fs_ln"])
                s.act(lambda e: e.activation(out=fs[:, 16:16 + ntl], in_=fs[:, 8:8 + ntl], func=AF.Exp, scale=-0.5),
                      reads=["fs_ln"], writes=["fs_rstd"])
                for i, (kind, b, ti) in enumerate(grp):
                    fsl = (base_f + i) % 2
                    xk = "xfin%d" % fsl
                    gk = "G2t%d" % fsl
                    yk = "yacc%d" % i
                    if i + 1 < ntl:
                        floads(i + 1)
                    s.dve(lambda e, i=i, fsl=fsl: e.scalar_tensor_tensor(
                        out=tmp2[:], in0=yacc[:, i, :], scalar=fs[:, 16 + i:17 + i], in1=G2t[fsl][:],
                        op0=ALU.mult, op1=ALU.mult), reads=[yk, "fs_rstd", gk], writes=["tmp2"])
                    s.dve(lambda e, fsl=fsl: e.tensor_tensor(out=xfin[fsl][:], in0=tmp2[:], in1=xfin[fsl][:], op=ALU.add),
                          reads=["tmp2"], writes=[xk])
                    if kind == "p":
                        s.dma("sp", yp[b, ti * 128:(ti + 1) * 128, :], xfin[fsl][:], reads=[xk])
                    else:
                        s.dma("sp", ys[b, :, :], xfin[fsl][0:64, :], reads=[xk])
                    yield

            ldc = [0]

            def load_gu(slot, eb):
                for j in range(2):
                    e_ = eb * 2 + j
                    s.dma("pool", wg2[slot][:, :, j * 256:(j + 1) * 256], w_eg[e_].rearrange("(c p) n -> p c n", p=128),
                          writes=["wg2_%d" % slot])
                    s.dma("pool", wu2[slot][:, :, j * 256:(j + 1) * 256], w_eu[e_].rearrange("(c p) n -> p c n", p=128),
                          writes=["wu2_%d" % slot])

            def load_d(slot, eb):
                for j in range(2):
                    e_ = eb * 2 + j
                    s.dma("pool", wd2[slot][:, 2 * j:2 * j + 2, :], w_ed[e_].rearrange("(c p) n -> p c n", p=128),
                          writes=["wd2_%d" % slot])

            aux = []
            fin_ids = set()

            def aux_step():
                while aux:
                    try:
                        next(aux[0])
                        return
                    except StopIteration:
                        aux.pop(0)

            def aux_drain(n_keep=0):
                while len(aux) > n_keep:
                    try:
                        next(aux[0])
                    except StopIteration:
                        aux.pop(0)

            for _ in prologue(0):
                pass
            load_gu(0, 0)
            load_d(0, 0)

            def run_group(gi, grp):
                ntl = len(grp)
                G = ntl * 128
                nblk = (G + 511) // 512
                bw = min(512, G)
                hb = gi % 2
                H = h2T[hb]

                def gu(eb):
                    slot = ldc[0] % 2
                    hs = eb % 2
                    for j in range(2):
                        e_ = eb * 2 + j
                        for blk in range(nblk):
                            bk = blk % 2
                            s.pe(lambda e, bk=bk, e_=e_, blk=blk: e.matmul(
                                banks[bk][:, 0:bw], lhsT=sel[:, e_, :], rhs=chi[hb][:, blk * 512:blk * 512 + bw],
                                start=True, stop=True), reads=["sel", "chi%d" % hb], writes=[bkey[bk]])
                            s.act(lambda e, bk=bk, j=j, blk=blk, hs=hs: e.activation(
                                out=cb_sb[hs][:, j, blk * 512:blk * 512 + bw], in_=banks[bk][:, 0:bw], func=AF.Copy),
                                writes=[bkey[bk], "cb%d" % hs])
                    for fc in range(4):
                        j = fc // 2
                        for blk in range(nblk):
                            n_ = fc * nblk + blk
                            bg = 2 + n_ % 2
                            bu = 4 + n_ % 2
                            i2 = n_ % 2
                            hkeys = ["h2T%d_%d" % (hb, i) for i in range(blk * 4, min(ntl, blk * 4 + 4))]
                            for k in range(8):
                                s.pe(lambda e, bg=bg, k=k, fc=fc, blk=blk, slot=slot: e.matmul(
                                    banks[bg][:, 0:bw], lhsT=wg2[slot][:, k, fc * 128:(fc + 1) * 128],
                                    rhs=H[:, k, blk * 512:blk * 512 + bw], start=(k == 0), stop=(k == 7)),
                                    reads=hkeys + ["wg2_%d" % slot], writes=[bkey[bg]])
                            for k in range(8):
                                s.pe(lambda e, bu=bu, k=k, fc=fc, blk=blk, slot=slot: e.matmul(
                                    banks[bu][:, 0:bw], lhsT=wu2[slot][:, k, fc * 128:(fc + 1) * 128],
                                    rhs=H[:, k, blk * 512:blk * 512 + bw], start=(k == 0), stop=(k == 7)),
                                    reads=hkeys + ["wu2_%d" % slot], writes=[bkey[bu]])
                            s.act(lambda e, bg=bg, i2=i2: e.activation(out=sg[i2][:, 0:bw], in_=banks[bg][:, 0:bw], func=AF.Silu),
                                  writes=[bkey[bg], "sg%d" % i2])
                            s.dve(lambda e, i2=i2, j=j, blk=blk, hs=hs: e.tensor_tensor(
                                out=sgc[i2][:, 0:bw], in0=sg[i2][:, 0:bw], in1=cb_sb[hs][:, j, blk * 512:blk * 512 + bw],
                                op=ALU.mult), reads=["sg%d" % i2, "cb%d" % hs], writes=["sgc%d" % i2])
                            s.dve(lambda e, bu=bu, i2=i2, fc=fc, blk=blk, hs=hs: e.tensor_tensor(
                                out=he[hs][:, fc, blk * 512:blk * 512 + bw], in0=banks[bu][:, 0:bw], in1=sgc[i2][:, 0:bw],
                                op=ALU.mult), reads=["sgc%d" % i2], writes=[bkey[bu], "he%d" % hs])
                            aux_step()

                def down(eb, dslot):
                    hs = eb % 2
                    for i in range(ntl):
                        for sl in range(2):
                            n_ = i * 2 + sl
                            bd = 6 + n_ % 2
                            for fc in range(4):
                                s.pe(lambda e, bd=bd, fc=fc, i=i, sl=sl, hs=hs, dslot=dslot: e.matmul(
                                    banks[bd][:], lhsT=he[hs][:, fc, i * 128:(i + 1) * 128],
                                    rhs=wd2[dslot][:, fc, sl * 512:(sl + 1) * 512], start=(fc == 0), stop=(fc == 3)),
                                    reads=["he%d" % hs, "wd2_%d" % dslot], writes=[bkey[bd]])
                            yk = "yacc%d" % i
                            if eb == 0:
                                s.dve(lambda e, bd=bd, i=i, sl=sl: e.tensor_copy(out=yacc[:, i, sl * 512:(sl + 1) * 512],
                                                                                in_=banks[bd][:]), writes=[bkey[bd], yk])
                            else:
                                s.dve(lambda e, bd=bd, i=i, sl=sl: e.tensor_tensor(
                                    out=yacc[:, i, sl * 512:(sl + 1) * 512], in0=banks[bd][:],
                                    in1=yacc[:, i, sl * 512:(sl + 1) * 512], op=ALU.add), writes=[bkey[bd], yk])

                dslots = {}
                for eb in range(17):
                    if eb == 3 and gi + 1 < len(groups):
                        aux.append(prologue(gi + 1))
                    if eb < 16:
                        dslots[eb] = ldc[0] % 2
                        if eb + 1 < 16:
                            load_gu((ldc[0] + 1) % 2, eb + 1)
                        elif gi + 1 < len(groups):
                            load_gu((ldc[0] + 1) % 2, 0)
                        gu(eb)
                    if eb == 1:
                        while aux and id(aux[0]) in fin_ids:
                            for _ in aux[0]:
                                pass
                            aux.pop(0)
                    if eb >= 1:
                        down(eb - 1, dslots[eb - 1])
                    if eb < 16:
                        if eb + 1 < 16:
                            load_d((ldc[0] + 1) % 2, eb + 1)
                        elif gi + 1 < len(groups):
                            load_d((ldc[0] + 1) % 2, 0)
                        ldc[0] += 1
                aux_drain(0)
                fin = finalize(gi)
                aux.append(fin)
                fin_ids.add(id(fin))

            for gi, grp in enumerate(groups):
                run_group(gi, grp)
            aux_drain(0)
            s.emit()
    return nc


_PROG = {}


def kernel(**inputs):
    f = lambda a: np.ascontiguousarray(np.asarray(a, dtype=np.float32))
    if "nc" not in _PROG:
        _PROG["nc"] = build_program()
    nc = _PROG["nc"]
    shared = {
        "w_ada": f(inputs["w_ada"][0]), "b_ada": f(inputs["b_ada"][0]),
        "g_pre_mix": f(inputs["g_pre_mix"][0]), "g_post_mix": f(inputs["g_post_mix"][0]),
        "g_pre_ffn": f(inputs["g_pre_ffn"][0]), "g_post_ffn": f(inputs["g_post_ffn"][0]),
        "w_in": f(inputs["w_in"][0]), "a_sinks": f(inputs["a_sinks"][0]), "t5_table": f(inputs["t5_table"]),
        "b_rel": f(inputs["b_rel_table"][0]), "w_pa": f(inputs["w_proj_a"][0]), "w_pb": f(inputs["w_proj_b"][0]),
        "w_gate": f(inputs["w_gate"][0]), "b_gate": f(inputs["b_gate"][0]), "w_o": f(inputs["w_o"][0]),
        "w_rg": f(inputs["w_route_g"][0]), "b_rg": f(inputs["b_route_g"][0]),
        "w_re": f(inputs["w_route_e"][0]), "b_re": f(inputs["b_route_e"][0]),
        "w_eg": f(inputs["w_e_gate"][0]), "w_eu": f(inputs["w_e_up"][0]), "w_ed": f(inputs["w_e_down"][0]),
    }
    in_maps = []
    for c in range(NCORES):
        sl = slice(c * NB, (c + 1) * NB)
        m = dict(shared)
        m["xp"] = f(inputs["x_prompt"][sl]); m["xs"] = f(inputs["x_sample"][sl])
        m["cp"] = f(inputs["c_prompt"][sl]); m["cs"] = f(inputs["c_sample"][sl])
        m["cak"] = f(inputs["cache_a_k"][0, sl]).reshape(NB, 128, 128)
        m["cav"] = f(inputs["cache_a_v"][0, sl]).reshape(NB, 128, 128)
        m["cbk"] = f(inputs["cache_b_k"][0, sl]).reshape(NB, 512, 512)
        m["cbv"] = f(inputs["cache_b_v"][0, sl]).reshape(NB, 512, 512)
        in_maps.append(m)
    res = run_bass_kernel_spmd(nc, in_maps, core_ids=list(range(NCORES)))
    R = res.results
    cat = lambda k: np.concatenate([np.asarray(r[k], dtype=np.float32) for r in R], axis=0)
    y_p = cat("yp"); y_s = cat("ys")
    nakp = cat("nakp").reshape(1, 32, 128, 2, 64); navp = cat("navp").reshape(1, 32, 128, 2, 64)
    nbkp = cat("nbkp").reshape(1, 32, 512, 8, 64); nbvp = cat("nbvp").reshape(1, 32, 512, 8, 64)
    naks = cat("naks").reshape(1, 32, 128, 2, 64); navs = cat("navs").reshape(1, 32, 128, 2, 64)
    nbks = cat("nbks").reshape(1, 32, 512, 8, 64); nbvs = cat("nbvs").reshape(1, 32, 512, 8, 64)
    return (y_p, y_s, nakp, navp, nbkp, nbvp, naks, navs, nbks, nbvs)
```

```python
import math
from contextlib import ExitStack

import numpy as np
import jax
import jax.numpy as jnp

import concourse.bass as bass
import concourse.mybir as mybir
from concourse.bass_utils import run_bass_kernel_spmd

F32 = mybir.dt.float32
BF16 = mybir.dt.bfloat16
AF = mybir.ActivationFunctionType
ALU = mybir.AluOpType
AX = mybir.AxisListType

ENGS = ("pe", "act", "dve", "pool", "sp")
NDSEM = 8
NCORES = 8
NB = 4
SEQ = 2048
D = 1024
NEG = -30000.0


class SemState:
    def __init__(self, nc, stack):
        self.esem = {e: stack.enter_context(nc.semaphore("s_" + e)) for e in ENGS}
        self.ecnt = {e: 0 for e in ENGS}
        self.dsem = {q: [stack.enter_context(nc.semaphore("d_%s%d" % (q, i))) for i in range(NDSEM)]
                     for q in ("sp", "act", "pool")}
        self.dcnt = {q: 0 for q in ("sp", "act", "pool")}
        self.seen = {e: {} for e in ENGS}


class Op:
    __slots__ = ("eng", "fn", "dma", "idx", "deps", "sig", "sigval", "dsem", "dval", "dprev")

    def __init__(self, eng, fn, dma):
        self.eng = eng
        self.fn = fn
        self.dma = dma
        self.deps = []
        self.sig = False
        self.sigval = None
        self.dsem = None


class Sched:
    def __init__(self, nc, st):
        self.nc = nc
        self.st = st
        self.ops = {e: [] for e in ENGS}
        self.last_w = {}
        self.readers = {}

    def add(self, eng, fn, reads=(), writes=(), dma=False):
        op = Op(eng, fn, dma)
        op.idx = len(self.ops[eng])
        deps = {}
        for k in reads:
            w = self.last_w.get(k)
            if w is not None:
                deps[id(w)] = w
        for k in writes:
            w = self.last_w.get(k)
            if w is not None:
                deps[id(w)] = w
            for r in self.readers.get(k, ()):
                deps[id(r)] = r
        op.deps = list(deps.values())
        for k in writes:
            self.last_w[k] = op
            self.readers[k] = []
        for k in reads:
            if k not in writes:
                self.readers.setdefault(k, []).append(op)
        self.ops[eng].append(op)
        return op

    def pe(self, fn, reads=(), writes=()):
        return self.add("pe", fn, reads, writes)

    def act(self, fn, reads=(), writes=()):
        return self.add("act", fn, reads, writes)

    def dve(self, fn, reads=(), writes=()):
        return self.add("dve", fn, reads, writes)

    def pool(self, fn, reads=(), writes=()):
        return self.add("pool", fn, reads, writes)

    def dma(self, q, out, in_, reads=(), writes=(), **kw):
        return self.add(q, lambda e: e.dma_start(out=out, in_=in_, **kw), reads, writes, dma=True)

    @staticmethod
    def _needs_sync(p, c):
        if p.dma or c.dma:
            return True
        if p.eng != c.eng:
            return True
        if p.eng == "pe":
            return False
        return (c.idx - p.idx) <= 2

    def emit(self):
        nc, st = self.nc, self.st
        for e in ENGS:
            for o in reversed(self.ops[e]):
                if not o.dma:
                    o.sig = True
                    break
        for e in ENGS:
            for c in self.ops[e]:
                for p in c.deps:
                    if (not p.dma) and self._needs_sync(p, c):
                        p.sig = True
        for e in ENGS:
            for o in self.ops[e]:
                if o.dma:
                    n = st.dcnt[e]
                    st.dcnt[e] += 1
                    o.dsem = st.dsem[e][n % NDSEM]
                    o.dval = 16 * (n // NDSEM + 1)
                    o.dprev = 16 * (n // NDSEM)
                elif o.sig:
                    st.ecnt[e] += 1
                    o.sigval = st.ecnt[e]
        end_e = dict(st.ecnt)
        end_d = dict(st.dcnt)

        def emit_engine(e, eng):
            seen = st.seen[e]

            def wait(sem, val):
                k = id(sem)
                if val <= 0 or seen.get(k, 0) >= val:
                    return
                eng.wait_ge(sem, val)
                seen[k] = val

            for o in self.ops[e]:
                for p in o.deps:
                    if p.dma:
                        wait(p.dsem, p.dval)
                    elif self._needs_sync(p, o):
                        wait(st.esem[p.eng], p.sigval)
                if o.dma:
                    wait(o.dsem, o.dprev)
                    o.fn(eng).then_inc(o.dsem, 16)
                else:
                    ins = o.fn(eng)
                    if o.sig:
                        ins.then_inc(st.esem[e], 1)
            for e2 in ENGS:
                if e2 != e:
                    wait(st.esem[e2], end_e[e2])
            for q in st.dsem:
                n = end_d[q]
                for i in range(NDSEM):
                    if n > i:
                        last_n = ((n - 1 - i) // NDSEM) * NDSEM + i
                        wait(st.dsem[q][i], 16 * (last_n // NDSEM + 1))

        with nc.Block() as block:
            @block.tensor
            def _(eng):
                emit_engine("pe", eng)

            @block.scalar
            def _(eng):
                emit_engine("act", eng)

            @block.vector
            def _(eng):
                emit_engine("dve", eng)

            @block.gpsimd
            def _(eng):
                emit_engine("pool", eng)

            @block.sync
            def _(eng):
                emit_engine("sp", eng)


def AP(t, off, ap):
    return bass.AP(t.tensor, off, ap)


def bcl(ap, n):
    return bass.AP(ap.tensor, ap.offset, [list(x) for x in ap.ap] + [[0, n]])


def t5_runs():
    cpu = jax.devices("cpu")[0]
    with jax.default_device(cpu):
        rel = -(jnp.arange(383, dtype=jnp.int32) - 127)
        half, exact = 16, 8
        ret = jnp.where(rel > 0, half, 0)
        n = jnp.abs(rel)
        nf = jnp.maximum(n, 1).astype(jnp.float32)
        large = exact + (jnp.log(nf / exact) / math.log(128 / exact) * (half - exact)).astype(jnp.int32)
        large = jnp.minimum(large, half - 1)
        bucket = np.asarray(ret + jnp.where(n < exact, n, large))
    runs = []
    j0 = 0
    for j in range(1, 384):
        if j == 383 or bucket[j] != bucket[j0]:
            runs.append((j0, j, int(bucket[j0])))
            j0 = j
    return runs


def build_program():
    nc = bass.Bass("TRN2", target_bir_lowering=False)

    def din(name, shape):
        return nc.dram_tensor(name, list(shape), F32, kind="ExternalInput").ap()

    def dout(name, shape):
        return nc.dram_tensor(name, list(shape), F32, kind="ExternalOutput").ap()

    xp = din("xp", [NB, SEQ, D]); xsm = din("xs", [NB, 64, D])
    cpr = din("cp", [NB, D]); csm = din("cs", [NB, D])
    cak = din("cak", [NB, 128, 128]); cav = din("cav", [NB, 128, 128])
    cbk = din("cbk", [NB, 512, 512]); cbv = din("cbv", [NB, 512, 512])
    w_ada = din("w_ada", [D, 6 * D]); b_ada = din("b_ada", [6 * D])
    g_pre_mix = din("g_pre_mix", [D]); g_post_mix = din("g_post_mix", [D])
    g_pre_ffn = din("g_pre_ffn", [D]); g_post_ffn = din("g_post_ffn", [D])
    w_in = din("w_in", [D, 2304]); a_sinks = din("a_sinks", [8]); t5_table = din("t5_table", [32, 8])
    b_rel = din("b_rel", [8, 513]); w_pa = din("w_pa", [512, D]); w_pb = din("w_pb", [512, D])
    w_gate = din("w_gate", [D, 2 * D]); b_gate = din("b_gate", [2 * D]); w_o = din("w_o", [D, D])
    w_rg = din("w_rg", [D, 4]); b_rg = din("b_rg", [4]); w_re = din("w_re", [D, 32]); b_re = din("b_re", [32])
    w_eg = din("w_eg", [32, D, 256]); w_eu = din("w_eu", [32, D, 256]); w_ed = din("w_ed", [32, 256, D])

    yp = dout("yp", [NB, SEQ, D]); ys = dout("ys", [NB, 64, D])
    nakp = dout("nakp", [NB, 128, 128]); navp = dout("navp", [NB, 128, 128])
    nbkp = dout("nbkp", [NB, 512, 512]); nbvp = dout("nbvp", [NB, 512, 512])
    naks = dout("naks", [NB, 128, 128]); navs = dout("navs", [NB, 128, 128])
    nbks = dout("nbks", [NB, 512, 512]); nbvs = dout("nbvs", [NB, 512, 512])

    NT = NB * 16 + NB
    X1d = nc.dram_tensor("X1d", [NT * 128, D], F32).ap()
    Gd = nc.dram_tensor("Gd", [8, 2, D], F32).ap()
    extB = nc.dram_tensor("extB", [8, 768], F32).ap()
    SB_ = nc.dram_tensor("SBd", [128, 8 * 768], F32).ap()
    extA = nc.dram_tensor("extA", [8, 384], F32).ap()
    SA_ = nc.dram_tensor("SAd", [128, 8 * 384], F32).ap()

    with ExitStack() as es:
        st = SemState(nc, es)

        def T(stack, name, shape, dt):
            return stack.enter_context(nc.sbuf_tensor(name, list(shape), dt))

        banks = [es.enter_context(nc.psum_tensor("bank%d" % i, [128, 512], F32)) for i in range(8)]
        bkey = ["bank%d" % i for i in range(8)]

        ident = T(es, "ident", [128, 128], BF16)
        identf = T(es, "identf", [128, 128], F32)
        adaT = T(es, "adaT", [128, 48, 8], F32)
        A1 = T(es, "A1", [128, 8, 8], F32)
        A2 = T(es, "A2", [128, 8, 8], F32)
        bgT = T(es, "bgT", [128, 16], F32)
        expsink = T(es, "expsink", [128, 8], F32)
        wr_sb = T(es, "wr_sb", [128, 8, 36], BF16)
        rbias = T(es, "rbias", [128, 36], F32)
        rstd_all = T(es, "rstd_all", [128, 68], F32)

        with ExitStack() as e0:
            s = Sched(nc, st)
            wada = T(e0, "wada", [128, 8, 6 * D], BF16)
            c8 = T(e0, "c8", [8, D], F32)
            sc8 = T(e0, "sc8", [8, D], BF16)
            cT = T(e0, "cT", [128, 8, 8], BF16)
            colsrc = T(e0, "colsrc", [80, 128], F32)
            colT = T(e0, "colT", [128, 80], F32)
            t5src = T(e0, "t5src", [32, 8], F32)
            t5T = T(e0, "t5T", [8, 32], F32)
            extA_sb = T(e0, "extA_sb", [8, 384], F32)
            extB_sb = T(e0, "extB_sb", [8, 768], F32)
            tmp88 = T(e0, "tmp88", [128, 8, 8], F32)
            bada8 = T(e0, "bada8", [8, 2, D], F32)
            gpost8 = T(e0, "gpost8", [8, 2, D], F32)
            G8 = T(e0, "G8", [8, 2, D], F32)
            sink_t = T(e0, "sink_t", [128, 8], F32)

            s.pool(lambda e: e.memset(identf[:], 0.0), writes=["identf"])
            s.pool(lambda e: e.affine_select(out=identf[:], in_=identf[:], pattern=[[-1, 128]],
                                              compare_op=ALU.not_equal, fill=1.0, base=0, channel_multiplier=1),
                   writes=["identf"])
            s.dve(lambda e: e.tensor_copy(out=ident[:], in_=identf[:]), reads=["identf"], writes=["ident"])

            s.dma("sp", sink_t[:], AP(a_sinks, 0, [[0, 128], [1, 8]]), writes=["sink_t"])
            s.act(lambda e: e.activation(out=expsink[:], in_=sink_t[:], func=AF.Exp), reads=["sink_t"], writes=["expsink"])
            s.dma("pool", wr_sb[:, :, 0:4], w_rg.rearrange("(c p) n -> p c n", p=128), writes=["wr"])
            s.dma("pool", wr_sb[:, :, 4:36], w_re.rearrange("(c p) n -> p c n", p=128), writes=["wr"])
            s.dma("sp", rbias[:, 0:4], AP(b_rg, 0, [[0, 128], [1, 4]]), writes=["rbias"])
            s.dma("sp", rbias[:, 4:36], AP(b_re, 0, [[0, 128], [1, 32]]), writes=["rbias"])
            s.dma("sp", extB_sb[:, 0:384], b_rel[:, 129:513], writes=["extB_sb"])
            s.dve(lambda e: e.tensor_copy(out=extB_sb[:, 384:768], in_=bcl(extB_sb[:, 383], 384)), writes=["extB_sb"])
            s.dma("sp", extB, extB_sb[:], reads=["extB_sb"], writes=["extB"])
            s.dma("sp", SB_, AP(extB, 0, [[0, 128], [1, 8 * 768]]), reads=["extB"], writes=["SBd"])
            s.dma("sp", t5src[:], t5_table, writes=["t5src"])
            s.pe(lambda e: e.transpose(out=banks[3][0:8, 0:32], in_=t5src[:], identity=identf[0:32, 0:32]),
                 reads=["t5src", "identf"], writes=[bkey[3]])
            s.dve(lambda e: e.tensor_copy(out=t5T[:], in_=banks[3][0:8, 0:32]), writes=[bkey[3], "t5T"])
            for (j0, j1, bu) in t5_runs():
                s.dve(lambda e, j0=j0, j1=j1, bu=bu: e.tensor_copy(out=extA_sb[:, j0:j1], in_=bcl(t5T[:, bu], j1 - j0)),
                      reads=["t5T"], writes=["extA_sb"])
            s.dve(lambda e: e.tensor_copy(out=extA_sb[:, 383:384], in_=t5T[:, 0:1]), reads=["t5T"], writes=["extA_sb"])
            s.dma("sp", extA, extA_sb[:], reads=["extA_sb"], writes=["extA"])
            s.dma("sp", SA_, AP(extA, 0, [[0, 128], [1, 8 * 384]]), reads=["extA"], writes=["SAd"])
            s.dma("sp", c8[0:4, :], cpr, writes=["c8"])
            s.dma("sp", c8[4:8, :], csm, writes=["c8"])
            s.act(lambda e: e.activation(out=sc8[:], in_=c8[:], func=AF.Silu), reads=["c8"], writes=["sc8"])
            tp = banks[0][:].bitcast(BF16)
            for k in range(8):
                s.pe(lambda e, k=k: e.transpose(out=tp[:, k * 8:(k + 1) * 8], in_=sc8[0:8, k * 128:(k + 1) * 128],
                                                identity=ident[0:8, 0:8]),
                     reads=["sc8", "ident"], writes=[bkey[0]])
            s.dve(lambda e: e.tensor_copy(out=cT[:].rearrange("p k b -> p (k b)"), in_=tp[:, 0:64]),
                  writes=[bkey[0], "cT"])
            for k in range(8):
                s.dma("pool", wada[:, k, :], w_ada[k * 128:(k + 1) * 128, :], writes=["wada%d" % k])
            s.dma("sp", colsrc[0:48, :], b_ada.rearrange("(c p) -> c p", p=128), writes=["colsrc"])
            s.dma("sp", colsrc[48:56, :], g_pre_mix.rearrange("(c p) -> c p", p=128), writes=["colsrc"])
            s.dma("sp", colsrc[56:64, :], g_pre_ffn.rearrange("(c p) -> c p", p=128), writes=["colsrc"])
            s.dma("sp", colsrc[64:80, :], b_gate.rearrange("(c p) -> c p", p=128), writes=["colsrc"])
            s.pe(lambda e: e.transpose(out=banks[2][:, 0:80], in_=colsrc[0:80, :], identity=identf[0:80, 0:80]),
                 reads=["colsrc", "identf"], writes=[bkey[2]])
            s.dve(lambda e: e.tensor_copy(out=colT[:], in_=banks[2][:, 0:80]), writes=[bkey[2], "colT"])
            s.dve(lambda e: e.tensor_copy(out=bgT[:], in_=colT[:, 64:80]), reads=["colT"], writes=["bgT"])
            pa_ = banks[1][:, 0:384].rearrange("p (f b) -> p f b", b=8)
            for f in range(48):
                for k in range(8):
                    s.pe(lambda e, f=f, k=k: e.matmul(pa_[:, f, :], lhsT=wada[:, k, f * 128:(f + 1) * 128],
                                                      rhs=cT[:, k, :], start=(k == 0), stop=(k == 7)),
                         reads=["cT", "wada%d" % k], writes=[bkey[1]])
            s.dve(lambda e: e.tensor_tensor(out=adaT[:], in0=pa_, in1=bcl(colT[:, 0:48], 8), op=ALU.add),
                  reads=["colT"], writes=[bkey[1], "adaT"])
            for (Ax, gp, c0, nm) in ((A1, colT[:, 48:56], 8, "A1"), (A2, colT[:, 56:64], 32, "A2")):
                s.dve(lambda e, c0=c0: e.tensor_scalar(out=tmp88[:], in0=adaT[:, c0:c0 + 8, :], scalar1=1.0, scalar2=None,
                                                       op0=ALU.add), reads=["adaT"], writes=["tmp88"])
                s.dve(lambda e, Ax=Ax, gp=gp: e.tensor_tensor(out=Ax[:], in0=tmp88[:], in1=bcl(gp, 8),
                                                              op=ALU.mult), reads=["tmp88", "colT"], writes=[nm])
            for v, c0 in ((0, 2 * D), (1, 5 * D)):
                s.dma("sp", bada8[:, v, :], AP(b_ada, c0, [[0, 8], [1, D]]), writes=["bada8"])
            s.dma("sp", gpost8[:, 0, :], AP(g_post_mix, 0, [[0, 8], [1, D]]), writes=["gpost8"])
            s.dma("sp", gpost8[:, 1, :], AP(g_post_ffn, 0, [[0, 8], [1, D]]), writes=["gpost8"])
            for v, c0 in ((0, 2 * D), (1, 5 * D)):
                for sl in range(2):
                    bk = 2 + (v * 2 + sl) % 2
                    for k in range(8):
                        s.pe(lambda e, k=k, bk=bk, c=c0 + sl * 512: e.matmul(
                            banks[bk][0:8, :], lhsT=cT[:, k, :], rhs=wada[:, k, c:c + 512],
                            start=(k == 0), stop=(k == 7)),
                            reads=["cT", "wada%d" % k], writes=[bkey[bk]])
                    s.dve(lambda e, v=v, sl=sl, bk=bk: e.tensor_tensor(
                        out=G8[:, v, sl * 512:(sl + 1) * 512], in0=banks[bk][0:8, :],
                        in1=bada8[:, v, sl * 512:(sl + 1) * 512], op=ALU.add),
                        reads=["bada8"], writes=[bkey[bk], "G8"])
            s.dve(lambda e: e.tensor_tensor(out=G8[:], in0=G8[:], in1=gpost8[:], op=ALU.mult),
                  reads=["gpost8"], writes=["G8"])
            s.dma("sp", Gd, G8[:], reads=["G8"], writes=["Gd"])
            s.emit()

        with ExitStack() as e1:
            s = Sched(nc, st)
            biasB = T(e1, "biasB", [128, 8, 5, 128], F32)
            biasA = T(e1, "biasA", [128, 8, 2, 128], F32)
            w_in_sb = T(e1, "w_in_sb", [128, 8, 2304], BF16)
            wg_sb = T(e1, "wg_sb", [128, 8, 2048], BF16)
            wpa_sb = T(e1, "wpa_sb", [128, 4, D], BF16)
            wpb_sb = T(e1, "wpb_sb", [128, 4, D], BF16)
            wo_sb = T(e1, "wo_sb", [128, 8, D], BF16)
            xt = [T(e1, "xt%d" % i, [128, 2, D], F32) for i in range(2)]
            xnb = [T(e1, "xn%d" % i, [128, D], BF16) for i in range(2)]
            hTb = [T(e1, "hT%d" % i, [128, 8, 256], BF16) for i in range(2)]
            qaT = T(e1, "qaT", [128, 4, 256], BF16)
            qbT = T(e1, "qbT", [128, 4, 256], BF16)
            kaT = T(e1, "kaT", [128, 1024], BF16)
            kbT = T(e1, "kbT", [128, 4, 1024], BF16)
            va = T(e1, "va", [128, 8, 2, 65], BF16)
            vb = T(e1, "vb", [128, 8, 8, 65], BF16)
            tmpf = T(e1, "tmpf", [128, 512], F32)
            PT = [T(e1, "PT%d" % i, [128, 512], BF16) for i in range(3)]
            on = T(e1, "on", [128, D], BF16)
            junk = on[:]
            oT = T(e1, "oT", [128, 8, 256], BF16)
            sig = T(e1, "sig", [128, 512], F32)
            prod = T(e1, "prod", [128, 512], F32)
            mT = T(e1, "mT", [128, 8, 256], BF16)
            G1bc = T(e1, "G1bc", [128, D], F32)
            stt = T(e1, "stt", [128, 32], F32)
            kst = [sig, prod]
            xt1b = xt[1][:].bitcast(BF16)
            ctile = xt1b[:, 0, :].rearrange("p (t f) -> p t f", t=4)
            catile = xt1b[:, 1, 0:128]

            s.dma("pool", w_in_sb[:, :, 512:2304], w_in[:, 512:2304].rearrange("(c p) n -> p c n", p=128), writes=["w_in_rest"])
            qst = xt[1][:].bitcast(BF16)[:, 0, :].rearrange("p (c n) -> p c n", c=4)
            qst2 = xt[1][:].bitcast(BF16)[:, 1, :].rearrange("p (c n) -> p c n", c=4)
            s.dma("pool", qst, w_in[0:512, 0:512].rearrange("(c p) n -> p c n", p=128), writes=["xt1"])
            s.dma("pool", qst2, w_in[512:1024, 0:512].rearrange("(c p) n -> p c n", p=128), writes=["xt1"])
            for half, src in ((0, qst), (1, qst2)):
                for g in range(4):
                    for kv in range(2):
                        dst = w_in_sb[:, half * 4:(half + 1) * 4, g * 128 + kv * 64:g * 128 + (kv + 1) * 64]
                        sv = src[:, :, kv * 256 + g * 64:kv * 256 + (g + 1) * 64]
                        if (g + kv) % 2 == 0:
                            s.dve(lambda e, dst=dst, sv=sv: e.tensor_copy(out=dst, in_=sv), reads=["xt1"], writes=["w_in"])
                        else:
                            s.pool(lambda e, dst=dst, sv=sv: e.tensor_copy(out=dst, in_=sv), reads=["xt1"], writes=["w_in"])
            s.dma("pool", wg_sb[:], w_gate.rearrange("(c p) n -> p c n", p=128), writes=["wg"])
            s.dma("pool", wpa_sb[:], w_pa.rearrange("(c p) n -> p c n", p=128), writes=["wpa"])
            s.dma("pool", wpb_sb[:], w_pb.rearrange("(c p) n -> p c n", p=128), writes=["wpb"])
            s.dma("pool", wo_sb[:], w_o.rearrange("(c p) n -> p c n", p=128), writes=["wo"])
            s.dma("sp", biasB[:], AP(SB_, 127, [[8 * 768 - 1, 128], [768, 8], [128, 5], [1, 128]]), writes=["biasB"])
            s.dma("sp", biasA[:], AP(SA_, 127, [[8 * 384 - 1, 128], [384, 8], [128, 2], [1, 128]]), writes=["biasA"])
            s.dve(lambda e: e.memset(biasB[64:128, :, 0, 0:64], NEG), writes=["biasB"])
            s.dve(lambda e: e.memset(biasB[0:64, :, 4, 64:128], NEG), writes=["biasB"])
            s.dve(lambda e: e.memset(biasA[64:128, :, 0, 0:64], NEG), writes=["biasA"])
            s.dve(lambda e: e.memset(biasA[0:64, :, 1, 64:128], NEG), writes=["biasA"])
            s.pool(lambda e: e.memset(va[:, :, :, 64:65], 1.0), writes=["va%d" % i for i in range(8)])
            s.pool(lambda e: e.memset(vb[:, :, :, 64:65], 1.0), writes=["vb%d" % i for i in range(8)])

            gen_banks = [4, 5, 6, 7]
            gctr = [0]

            def gbank():
                b = gen_banks[gctr[0] % len(gen_banks)]
                gctr[0] += 1
                return b

            wide_banks = [4, 5, 6, 7, 1, 2, 3]
            wctr = [0]

            def wbank():
                b = wide_banks[wctr[0] % len(wide_banks)]
                wctr[0] += 1
                return b

            sctr = [0]
            evc = [0]

            def evac_copy(out, in_, reads, writes, scale=None):
                evc[0] += 1
                if evc[0] % 2 == 0:
                    if scale is None:
                        s.act(lambda e: e.activation(out=out, in_=in_, func=AF.Copy), reads=reads, writes=writes)
                    else:
                        s.act(lambda e: e.activation(out=out, in_=in_, func=AF.Copy, scale=scale), reads=reads, writes=writes)
                else:
                    if scale is None:
                        s.dve(lambda e: e.tensor_copy(out=out, in_=in_), reads=reads, writes=writes)
                    else:
                        s.dve(lambda e: e.tensor_scalar(out=out, in0=in_, scalar1=scale, scalar2=None, op0=ALU.mult),
                              reads=reads, writes=writes)

            def rstd_from(ms_col, out_col, key):
                s.act(lambda e: e.activation(out=stt[:, 15:16], in_=ms_col, func=AF.Ln, bias=1e-6, scale=1.0),
                      reads=[key], writes=["stt_ln"])
                s.act(lambda e: e.activation(out=out_col, in_=stt[:, 15:16], func=AF.Exp, scale=-0.5),
                      reads=["stt_ln"], writes=[key])

            xctr = [0]

            NSB = 3
            SCB = [1, 2, 3]

            def attention_groups(W0, iA, iB):
                out = []
                oA = [gbank(), gbank()]
                for kv in range(2):
                    ob = oA[kv]
                    ov = banks[ob][:, 0:260].rearrange("p (h e) -> p h e", e=65)
                    dl = [d_ for d_ in (1, 0) if iA - d_ >= 0]
                    for di, d_ in enumerate(dl):
                        slot = (iA - d_) % 8
                        pb0 = kv * 64
                        first = (di == 0)
                        last = (di == len(dl) - 1)

                        def S(i2, slot=slot, pb0=pb0):
                            sb = SCB[i2]
                            s.pe(lambda e: e.matmul(
                                banks[sb][:].rearrange("p (g t) -> p g t", g=4),
                                lhsT=kaT[pb0:pb0 + 64, slot * 128:(slot + 1) * 128],
                                rhs=qaT[pb0:pb0 + 64, :, W0:W0 + 128], start=True, stop=True),
                                reads=["kaT%d" % slot, "qaT"], writes=[bkey[sb]])

                        def E(i2, kv=kv, d_=d_):
                            sb = SCB[i2]
                            s.dve(lambda e: e.tensor_tensor(
                                out=banks[sb][:].rearrange("p (g t) -> p g t", g=4),
                                in0=banks[sb][:].rearrange("p (g t) -> p g t", g=4),
                                in1=biasA[:, kv * 4:(kv + 1) * 4, d_, :], op=ALU.add),
                                reads=["biasA"], writes=[bkey[sb]])
                            s.act(lambda e: e.activation(out=PT[i2][:], in_=banks[sb][:], func=AF.Exp),
                                  writes=[bkey[sb], "PT%d" % i2])

                        def V(i2, slot=slot, kv=kv, ov=ov, ob=ob, first=first, last=last):
                            for g in range(4):
                                s.pe(lambda e, g=g: e.matmul(
                                    ov[:, g, :], lhsT=PT[i2][:, g * 128:(g + 1) * 128], rhs=va[:, slot, kv, :],
                                    start=(first and g == 0), stop=False, skip_group_check=True),
                                    reads=["PT%d" % i2, "va%d" % slot], writes=[bkey[ob]])
                            if last:
                                s.dve(lambda e: e.tensor_tensor(
                                    out=stt[:, 0:4], in0=ov[:, :, 64], in1=expsink[:, kv * 4:(kv + 1) * 4], op=ALU.add),
                                    reads=["expsink"], writes=[bkey[ob], "stt_a"])
                                s.dve(lambda e: e.reciprocal(out=stt[:, 4:8], in_=stt[:, 0:4]), reads=["stt_a"], writes=["stt_b"])
                                s.dve(lambda e: e.tensor_tensor(
                                    out=on[:, kv * 256:(kv + 1) * 256].rearrange("p (h d) -> p h d", d=64),
                                    in0=ov[:, :, 0:64], in1=bcl(stt[:, 4:8], 64), op=ALU.mult),
                                    reads=["stt_b"], writes=[bkey[ob], "on"])
                        out.append((S, E, V))
                oB = [gbank(), gbank()]
                blocks = [(h, d_) for h in range(8) for d_ in range(5) if iB - d_ >= 0]
                groups = []
                for blk in blocks:
                    if groups and len(groups[-1]) < 4 and groups[-1][-1][0] == blk[0]:
                        groups[-1].append(blk)
                    else:
                        groups.append([blk])
                nB = len(groups)
                for gi_, grp in enumerate(groups):
                    n = len(grp)
                    h = grp[0][0]
                    d_lo = grp[0][1]
                    ob = oB[h // 4]
                    ov = banks[ob][:, 0:260].rearrange("p (h e) -> p h e", e=65)
                    firstbank = all(g2[0][0] // 4 != h // 4 for g2 in groups[:gi_])
                    lastbank = all(g2[0][0] // 4 != h // 4 for g2 in groups[gi_ + 1:])

                    def S(i2, grp=grp):
                        sb = SCB[i2]
                        for j, (h_, d_) in enumerate(grp):
                            slot = (iB - d_) % 8
                            c, pb0 = h_ // 2, (h_ % 2) * 64
                            s.pe(lambda e, j=j, slot=slot, c=c, pb0=pb0: e.matmul(
                                banks[sb][:, j * 128:(j + 1) * 128],
                                lhsT=kbT[pb0:pb0 + 64, c, slot * 128:(slot + 1) * 128],
                                rhs=qbT[pb0:pb0 + 64, c, W0:W0 + 128], start=True, stop=True),
                                reads=["kbT%d" % slot, "qbT"], writes=[bkey[sb]])

                    def E(i2, n=n, h=h, d_lo=d_lo):
                        sb = SCB[i2]
                        s.dve(lambda e: e.tensor_tensor(
                            out=banks[sb][:, 0:n * 128].rearrange("p (g t) -> p g t", g=n),
                            in0=banks[sb][:, 0:n * 128].rearrange("p (g t) -> p g t", g=n),
                            in1=biasB[:, h, d_lo:d_lo + n, :], op=ALU.add),
                            reads=["biasB"], writes=[bkey[sb]])
                        s.act(lambda e: e.activation(out=PT[i2][:, 0:n * 128], in_=banks[sb][:, 0:n * 128], func=AF.Exp),
                              writes=[bkey[sb], "PT%d" % i2])

                    def V(i2, grp=grp, h=h, ov=ov, ob=ob, firstbank=firstbank, lastbank=lastbank):
                        for j, (h_, d_) in enumerate(grp):
                            slot = (iB - d_) % 8
                            s.pe(lambda e, j=j, slot=slot: e.matmul(
                                ov[:, h % 4, :], lhsT=PT[i2][:, j * 128:(j + 1) * 128], rhs=vb[:, slot, h, :],
                                start=(firstbank and j == 0), stop=False, skip_group_check=True),
                                reads=["PT%d" % i2, "vb%d" % slot], writes=[bkey[ob]])
                        if lastbank:
                            hh = h // 4
                            s.dve(lambda e: e.reciprocal(out=stt[:, 8:12], in_=ov[:, :, 64]), writes=[bkey[ob], "stt_c"])
                            s.dve(lambda e: e.tensor_tensor(
                                out=on[:, 512 + hh * 256:512 + (hh + 1) * 256].rearrange("p (h d) -> p h d", d=64),
                                in0=ov[:, :, 0:64], in1=bcl(stt[:, 8:12], 64), op=ALU.mult),
                                reads=["stt_c"], writes=[bkey[ob], "on"])
                    out.append((S, E, V))

                def Vt(i2):
                    tpv = banks[0][:].bitcast(BF16).rearrange("p (c t) -> p c t", c=8)
                    for c in range(8):
                        s.pe(lambda e, c=c: e.transpose(out=tpv[:, c, :], in_=on[:, c * 128:(c + 1) * 128], identity=ident[:]),
                             reads=["on", "ident"], writes=[bkey[0]])
                    evac_copy(oT[:, :, W0:W0 + 128], tpv, reads=[], writes=[bkey[0], "oT"])
                out.append((None, None, Vt))
                return out

            def run_attention(glist, L=2):
                pend = []
                for (S, E, V) in glist:
                    if S is not None:
                        i2 = sctr[0] % NSB
                        sctr[0] += 1
                        S(i2)
                        E(i2)
                    else:
                        i2 = None
                    pend.append((V, i2))
                    if len(pend) > L:
                        V0, j2 = pend.pop(0)
                        V0(j2)
                while pend:
                    V0, j2 = pend.pop(0)
                    V0(j2)

            xloaded = set()

            def xload(kind, b, s0, nt, n):
                slot_x = (n % 2) if kind == "p" else 0
                xs = xt[slot_x]
                xkey = "xt%d" % slot_x
                xloaded.add(n)
                if kind == "p":
                    s.dma("sp", xs[:, 0:nt, :], xp[b, s0 * 128:(s0 + nt) * 128, :].rearrange("(t p) d -> p t d", p=128),
                          writes=[xkey])
                else:
                    s.dma("sp", xs[0:64, 0, :], xsm[b, :, :], writes=[xkey])

            def prep(kind, b, s0, nt, n):
                slot_x = (n % 2) if kind == "p" else 0
                xs = xt[slot_x]
                xkey = "xt%d" % slot_x
                hT = hTb[n % 2]
                hkey = "hT%d" % (n % 2)
                bb = b if kind == "p" else 4 + b
                if n not in xloaded:
                    xload(kind, b, s0, nt, n)
                for t in range(nt):
                    xn = xnb[t % 2]
                    xnk = "xn%d" % (t % 2)
                    s.act(lambda e, t=t: e.activation(out=junk, in_=xs[:, t, :], func=AF.Square, scale=1.0 / 32,
                                                      accum_out=stt[:, 12:13]), reads=[xkey], writes=["on", "stt_ms"])
                    rstd_from(stt[:, 12:13], stt[:, 13:14], "stt_ms")
                    s.dve(lambda e, t=t, xn=xn: e.tensor_scalar(out=xn[:], in0=xs[:, t, :], scalar1=stt[:, 13:14], scalar2=None,
                                                                op0=ALU.mult), reads=[xkey, "stt_ms"], writes=[xnk])
                    yield
                for t in range(nt):
                    xn = xnb[t % 2]
                    xnk = "xn%d" % (t % 2)
                    tpv = banks[0][:].bitcast(BF16).rearrange("p (c t) -> p c t", c=8)
                    for c in range(8):
                        s.pe(lambda e, c=c, tpv=tpv, xn=xn: e.transpose(out=tpv[:, c, :], in_=xn[:, c * 128:(c + 1) * 128],
                                                                        identity=ident[:]),
                             reads=[xnk, "ident"], writes=[bkey[0]])
                    for c in range(8):
                        if c % 2 == 0:
                            s.act(lambda e, c=c, t=t, tpv=tpv: e.activation(
                                out=hT[:, c, t * 128:(t + 1) * 128], in_=tpv[:, c, :], func=AF.Identity,
                                scale=A1[:, c, bb:bb + 1], bias=adaT[:, c, bb:bb + 1]),
                                reads=["A1", "adaT"], writes=[bkey[0], hkey])
                        else:
                            s.dve(lambda e, c=c, t=t, tpv=tpv: e.tensor_scalar(
                                out=hT[:, c, t * 128:(t + 1) * 128], in0=tpv[:, c, :],
                                scalar1=A1[:, c, bb:bb + 1], scalar2=adaT[:, c, bb:bb + 1], op0=ALU.mult, op1=ALU.add),
                                reads=["A1", "adaT"], writes=[bkey[0], hkey])
                    yield

            def supertile(kind, b, s0, nt, n, nxt_gen):
                W = 128 * nt
                hA = 0 if kind == "p" else 1
                hB = 0 if kind == "p" else 4
                slot_x = (n % 2) if kind == "p" else 0
                xs = xt[slot_x]
                xkey = "xt%d" % slot_x
                hT = hTb[n % 2]
                hkey = "hT%d" % (n % 2)
                bb = b if kind == "p" else 4 + b
                slotA0 = (s0 + hA) % 8
                slotB0 = (s0 + hB) % 8
                ka_keys = ["kaT%d" % ((slotA0 + t) % 8) for t in range(nt)]
                kb_keys = ["kbT%d" % ((slotB0 + t) % 8) for t in range(nt)]
                jobs = [
                    ([0, 128], qaT[:, 0:2, 0:W], 0.125, ["qaT"]),
                    ([256, 384], qaT[:, 2:4, 0:W], 0.125, ["qaT"]),
                    ([768, 896], qbT[:, 0:2, 0:W], 0.125, ["qbT"]),
                    ([1024, 1152], qbT[:, 2:4, 0:W], 0.125, ["qbT"]),
                    ([1280, 1408], kbT[:, 0:2, slotB0 * 128:slotB0 * 128 + W], None, kb_keys),
                    ([1536, 1664], kbT[:, 2:4, slotB0 * 128:slotB0 * 128 + W], None, kb_keys),
                    ([512], kaT[:, slotA0 * 128:slotA0 * 128 + W], None, ka_keys),
                ]
                for cols, dest, scale, wkeys in jobs:
                    bk = wbank()
                    pv = banks[bk][:, 0:len(cols) * W].rearrange("p (j w) -> p j w", j=len(cols))
                    for j, c0 in enumerate(cols):
                        for k in range(8):
                            s.pe(lambda e, pv=pv, j=j, c0=c0, k=k: e.matmul(
                                pv[:, j, :], lhsT=w_in_sb[:, k, c0:c0 + 128], rhs=hT[:, k, 0:W],
                                start=(k == 0), stop=(k == 7)),
                                reads=[hkey, "w_in", "w_in_rest"], writes=[bkey[bk]])
                    src = pv if len(cols) == 2 else pv[:, 0, :]
                    evac_copy(dest, src, reads=[], writes=[bkey[bk]] + wkeys, scale=scale)
                for t in range(nt):
                    ti = s0 + t
                    sA = (ti + hA) % 8
                    sB = (ti + hB) % 8
                    bk = wbank()
                    for k in range(8):
                        s.pe(lambda e, bk=bk, k=k, t=t: e.matmul(
                            banks[bk][:], lhsT=hT[:, k, t * 128:(t + 1) * 128], rhs=w_in_sb[:, k, 1792:2304],
                            start=(k == 0), stop=(k == 7)), reads=[hkey, "w_in", "w_in_rest"], writes=[bkey[bk]])
                    evac_copy(vb[:, sB, :, 0:64], banks[bk][:].rearrange("p (h d) -> p h d", d=64),
                              reads=[], writes=[bkey[bk], "vb%d" % sB])
                    outB = (kind == "p" and ti >= 12) or kind == "s"
                    outA = (kind == "p" and ti == 15) or kind == "s"
                    if outB:
                        s.dve(lambda e, bk=bk: e.tensor_copy(out=kst[0][:], in_=banks[bk][:]), writes=[bkey[bk], "sig"])
                        if kind == "p":
                            s.dma("sp", nbvp[b, (ti - 12) * 128:(ti - 11) * 128, :], kst[0][:], reads=["sig"])
                        else:
                            s.dma("sp", nbvs[b, 448:512, :], kst[0][0:64, :], reads=["sig"])
                            s.dma("sp", nbvs[b, 0:448, :], cbv[b, 64:512, :])
                        bk2 = wbank()
                        for k in range(8):
                            s.pe(lambda e, bk2=bk2, k=k, t=t: e.matmul(
                                banks[bk2][:], lhsT=hT[:, k, t * 128:(t + 1) * 128], rhs=w_in_sb[:, k, 1280:1792],
                                start=(k == 0), stop=(k == 7)), reads=[hkey, "w_in", "w_in_rest"], writes=[bkey[bk2]])
                        s.act(lambda e, bk2=bk2: e.activation(out=kst[1][:], in_=banks[bk2][:], func=AF.Copy),
                              writes=[bkey[bk2], "prod"])
                        if kind == "p":
                            s.dma("sp", nbkp[b, (ti - 12) * 128:(ti - 11) * 128, :], kst[1][:], reads=["prod"])
                        else:
                            s.dma("sp", nbks[b, 448:512, :], kst[1][0:64, :], reads=["prod"])
                            s.dma("sp", nbks[b, 0:448, :], cbk[b, 64:512, :])
                    bk = wbank()
                    for k in range(8):
                        s.pe(lambda e, bk=bk, k=k, t=t: e.matmul(
                            banks[bk][:, 0:128], lhsT=hT[:, k, t * 128:(t + 1) * 128], rhs=w_in_sb[:, k, 640:768],
                            start=(k == 0), stop=(k == 7)), reads=[hkey, "w_in", "w_in_rest"], writes=[bkey[bk]])
                    if outA:
                        for k in range(8):
                            s.pe(lambda e, bk=bk, k=k, t=t: e.matmul(
                                banks[bk][:, 128:256], lhsT=hT[:, k, t * 128:(t + 1) * 128], rhs=w_in_sb[:, k, 512:640],
                                start=(k == 0), stop=(k == 7)), reads=[hkey, "w_in", "w_in_rest"], writes=[bkey[bk]])
                    evac_copy(va[:, sA, :, 0:64], banks[bk][:, 0:128].rearrange("p (h d) -> p h d", d=64),
                              reads=[], writes=[bkey[bk], "va%d" % sA])
                    if outA:
                        s.dve(lambda e, bk=bk: e.tensor_copy(out=tmpf[:, 0:256], in_=banks[bk][:, 0:256]),
                              writes=[bkey[bk], "tmpf"])
                        if kind == "p":
                            s.dma("sp", navp[b, :, :], tmpf[:, 0:128], reads=["tmpf"])
                            s.dma("sp", nakp[b, :, :], tmpf[:, 128:256], reads=["tmpf"])
                        else:
                            s.dma("sp", navs[b, 64:128, :], tmpf[0:64, 0:128], reads=["tmpf"])
                            s.dma("sp", naks[b, 64:128, :], tmpf[0:64, 128:256], reads=["tmpf"])
                            s.dma("sp", navs[b, 0:64, :], cav[b, 64:128, :])
                            s.dma("sp", naks[b, 0:64, :], cak[b, 64:128, :])
                gl = []
                for t in range(nt):
                    gl += attention_groups(t * 128, s0 + t + hA, s0 + t + hB)
                run_attention(gl)
                if nxt_gen is not None:
                    for _ in range(2):
                        next(nxt_gen, None)
                for f in range(8):
                    bz = wbank()
                    bp = wbank()
                    zv = banks[bz][:, 0:2 * W].rearrange("p (j w) -> p j w", j=2)
                    pv = banks[bp][:, 0:2 * W].rearrange("p (j w) -> p j w", j=2)
                    for j in range(2):
                        for k in range(8):
                            s.pe(lambda e, zv=zv, j=j, k=k, f=f: e.matmul(
                                zv[:, j, :], lhsT=wg_sb[:, k, j * 1024 + f * 128:j * 1024 + (f + 1) * 128],
                                rhs=hT[:, k, 0:W], start=(k == 0), stop=(k == 7)),
                                reads=[hkey, "wg"], writes=[bkey[bz]])
                    for j, wsb in enumerate((wpa_sb, wpb_sb)):
                        for k in range(4):
                            s.pe(lambda e, pv=pv, j=j, k=k, f=f, wsb=wsb: e.matmul(
                                pv[:, j, :], lhsT=wsb[:, k, f * 128:(f + 1) * 128], rhs=oT[:, j * 4 + k, 0:W],
                                start=(k == 0), stop=(k == 3)),
                                reads=["oT", "wpa", "wpb"], writes=[bkey[bp]])
                    for j in range(2):
                        s.act(lambda e, zv=zv, j=j, f=f: e.activation(
                            out=sig[:, j * W:(j + 1) * W], in_=zv[:, j, :], func=AF.Sigmoid,
                            bias=bgT[:, j * 8 + f:j * 8 + f + 1]), reads=["bgT"], writes=[bkey[bz], "sig"])
                    s.dve(lambda e, bp=bp: e.tensor_tensor(out=prod[:, 0:2 * W], in0=banks[bp][:, 0:2 * W],
                                                           in1=sig[:, 0:2 * W], op=ALU.mult),
                          reads=["sig"], writes=[bkey[bp], "prod"])
                    s.dve(lambda e, f=f: e.tensor_tensor(out=mT[:, f, 0:W], in0=prod[:, 0:W], in1=prod[:, W:2 * W],
                                                         op=ALU.add), reads=["prod"], writes=["mT"])
                    if nxt_gen is not None and f in (1, 4):
                        next(nxt_gen, None)
                for t in range(nt):
                    ti = s0 + t
                    bks = [wbank(), wbank()]
                    for sl in range(2):
                        for k in range(8):
                            s.pe(lambda e, sl=sl, k=k, t=t, bk=bks[sl]: e.matmul(
                                banks[bk][:], lhsT=mT[:, k, t * 128:(t + 1) * 128], rhs=wo_sb[:, k, sl * 512:(sl + 1) * 512],
                                start=(k == 0), stop=(k == 7)), reads=["mT", "wo"], writes=[bkey[bks[sl]]])
                    for sl in range(2):
                        s.act(lambda e, sl=sl, bk=bks[sl]: e.activation(
                            out=junk[:, 0:512], in_=banks[bk][:], func=AF.Square, scale=1.0 / 32,
                            accum_out=(stt[:, 10:11] if sl == 0 else stt[:, 11:12])),
                            writes=[bkey[bks[sl]], "on", "stt_w%d" % sl])
                    s.dve(lambda e: e.tensor_tensor(out=stt[:, 14:15], in0=stt[:, 10:11], in1=stt[:, 11:12], op=ALU.add),
                          reads=["stt_w0", "stt_w1"], writes=["stt_w"])
                    rstd_from(stt[:, 14:15], stt[:, 14:15], "stt_w")
                    for sl in range(2):
                        s.dve(lambda e, sl=sl, bk=bks[sl]: e.scalar_tensor_tensor(
                            out=tmpf[:], in0=banks[bk][:], scalar=stt[:, 14:15], in1=G1bc[:, sl * 512:(sl + 1) * 512],
                            op0=ALU.mult, op1=ALU.mult), reads=["stt_w", "G1bc"], writes=[bkey[bks[sl]], "tmpf"])
                        s.dve(lambda e, sl=sl, t=t: e.tensor_tensor(
                            out=xs[:, t, sl * 512:(sl + 1) * 512], in0=tmpf[:], in1=xs[:, t, sl * 512:(sl + 1) * 512],
                            op=ALU.add), reads=["tmpf"], writes=[xkey])
                    gt = (b * 16 + ti) if kind == "p" else (64 + b)
                    s.dma("sp", X1d[gt * 128:(gt + 1) * 128, :], xs[:, t, :], reads=[xkey], writes=["X1d"])
                    s.act(lambda e, t=t: e.activation(out=junk, in_=xs[:, t, :], func=AF.Square, scale=1.0 / 32,
                                                      accum_out=stt[:, 16:17]), reads=[xkey], writes=["on", "stt_x1"])
                    s.act(lambda e: e.activation(out=stt[:, 17:18], in_=stt[:, 16:17], func=AF.Ln, bias=1e-6, scale=1.0),
                          reads=["stt_x1"], writes=["stt_x1ln"])
                    s.act(lambda e, gt=gt: e.activation(out=rstd_all[:, gt:gt + 1], in_=stt[:, 17:18], func=AF.Exp, scale=-0.5),
                          reads=["stt_x1ln"], writes=["rstd_all"])

            stiles = [("p", b, s0, 2) for b in range(NB) for s0 in range(0, 16, 2)] + [("s", b, 0, 1) for b in range(NB)]

            def sample_history(b):
                s.dma("pool", ctile, cbk[b].rearrange("(t p) f -> p t f", p=128), writes=["xt1"])
                s.dma("pool", catile, cak[b], writes=["xt1"])
                for t in range(4):
                    s.dma("pool", vb[:, t, :, 0:64], cbv[b, t * 128:(t + 1) * 128, :].rearrange("p (h d) -> p h d", d=64),
                          writes=["vb%d" % t])
                s.dma("pool", va[:, 0, :, 0:64], cav[b].rearrange("p (h d) -> p h d", d=64), writes=["va0"])
                tpv = banks[0][:].bitcast(BF16).rearrange("p (c t) -> p c t", c=8)
                for t in range(4):
                    for c in range(4):
                        s.pe(lambda e, t=t, c=c, tpv=tpv: e.transpose(out=tpv[:, c, :], in_=ctile[:, t, c * 128:(c + 1) * 128],
                                                                      identity=ident[:]),
                             reads=["xt1", "ident"], writes=[bkey[0]])
                    evac_copy(kbT[:, :, t * 128:(t + 1) * 128], tpv[:, 0:4, :], reads=[], writes=[bkey[0], "kbT%d" % t])
                s.pe(lambda e, tpv=tpv: e.transpose(out=tpv[:, 0, :], in_=catile, identity=ident[:]),
                     reads=["xt1", "ident"], writes=[bkey[0]])
                evac_copy(kaT[:, 0:128], tpv[:, 0, :], reads=[], writes=[bkey[0], "kaT0"])

            cur_gen = prep(*stiles[0], 0)
            for _ in cur_gen:
                pass
            for n, (kind, b, s0, nt) in enumerate(stiles):
                if s0 == 0:
                    bb_ = b if kind == "p" else 4 + b
                    s.dma("sp", G1bc[:], AP(Gd, bb_ * 2 * D, [[0, 128], [1, D]]), writes=["G1bc"])
                if kind == "s":
                    sample_history(b)
                nxt = stiles[n + 1] if n + 1 < len(stiles) else None
                inter = nxt is not None and nxt[0] == "p"
                if inter:
                    xload(*nxt, n + 1)
                g = prep(*nxt, n + 1) if nxt is not None else None
                supertile(kind, b, s0, nt, n, g if inter else None)
                if g is not None:
                    for _ in g:
                        pass
            print("phase1 sbuf remaining", nc.sbuf_bytes_remaining)
            s.emit()

        with ExitStack() as e2:
            s = Sched(nc, st)
            h2T = [T(e2, "h2T%d" % i, [128, 8, 1024], BF16) for i in range(2)]
            yacc = T(e2, "yacc", [128, 8, D], F32)
            he = [T(e2, "he%d" % i, [128, 4, 1024], BF16) for i in range(2)]
            sg = [T(e2, "sg%d" % i, [128, 512], BF16) for i in range(2)]
            sgc = [T(e2, "sgc%d" % i, [128, 512], BF16) for i in range(2)]
            cb_sb = [T(e2, "cb_sb%d" % i, [128, 2, 1024], BF16) for i in range(2)]
            combT = T(e2, "combT", [32, 1024], F32)
            chi = [T(e2, "chi%d" % i, [32, 1024], BF16) for i in range(2)]
            wg2 = [T(e2, "wg2_%d" % i, [128, 8, 512], BF16) for i in range(2)]
            wu2 = [T(e2, "wu2_%d" % i, [128, 8, 512], BF16) for i in range(2)]
            wd2 = [T(e2, "wd2_%d" % i, [128, 4, D], BF16) for i in range(2)]
            xin = [T(e2, "xin%d" % i, [128, D], F32) for i in range(2)]
            xfin = [T(e2, "xfin%d" % i, [128, D], F32) for i in range(2)]
            G2t = [T(e2, "G2t%d" % i, [128, D], F32) for i in range(2)]
            xn2 = [T(e2, "xn2_%d" % i, [128, D], BF16) for i in range(2)]
            junk2 = T(e2, "junk2", [128, D], BF16)
            sel = T(e2, "sel", [32, 32, 128], BF16)
            rs = T(e2, "rs", [128, 8, 16], F32)
            LG = T(e2, "LG", [128, 8, 36], F32)
            ohg = T(e2, "ohg", [128, 8, 4], F32)
            rtmp = T(e2, "rtmp", [128, 8, 32], F32)
            r8 = T(e2, "r8", [128, 6, 8, 8], F32)
            comb = T(e2, "comb", [128, 8, 32], F32)
            fs = T(e2, "fs", [128, 24], F32)
            tmp2 = T(e2, "tmp2", [128, D], F32)
            print("phase2 sbuf remaining", nc.sbuf_bytes_remaining)

            s.dve(lambda e: e.tensor_copy(out=sel[:], in_=bcl(identf[0:32, 0:32], 128)), writes=["sel"])

            groups = []
            for b in range(NB):
                for half in range(2):
                    groups.append([("p", b, half * 8 + i) for i in range(8)])
            groups.append([("s", b, 0) for b in range(NB)])

            def gtile(kind, b, ti):
                return (b * 16 + ti) if kind == "p" else (64 + b)

            xic = [0]

            def prologue(gi):
                grp = groups[gi]
                ntl = len(grp)
                G = ntl * 128
                hb = gi % 2
                H = h2T[hb]
                base_x = xic[0]
                xic[0] += ntl

                def x1load(i):
                    kind, b, ti = grp[i]
                    gt = gtile(kind, b, ti)
                    xs_ = (base_x + i) % 2
                    s.dma("sp", xin[xs_][:], X1d[gt * 128:(gt + 1) * 128, :], writes=["xin%d" % xs_])

                x1load(0)
                for i, (kind, b, ti) in enumerate(grp):
                    bb = b if kind == "p" else 4 + b
                    xs_ = (base_x + i) % 2
                    xk = "xin%d" % xs_
                    gt = gtile(kind, b, ti)
                    if i + 1 < ntl:
                        x1load(i + 1)
                    s.dve(lambda e, xs_=xs_, gt=gt: e.tensor_scalar(out=xn2[xs_][:], in0=xin[xs_][:],
                                                                    scalar1=rstd_all[:, gt:gt + 1], scalar2=None, op0=ALU.mult),
                          reads=[xk], writes=["xn2_%d" % xs_])
                    yield
                    tb = xs_
                    tpv = banks[tb][:].bitcast(BF16).rearrange("p (c t) -> p c t", c=8)
                    for c in range(8):
                        s.pe(lambda e, c=c, tpv=tpv, xs_=xs_: e.transpose(out=tpv[:, c, :], in_=xn2[xs_][:, c * 128:(c + 1) * 128],
                                                                          identity=ident[:]),
                             reads=["xn2_%d" % xs_, "ident"], writes=[bkey[tb]])
                    for c in range(8):
                        if c % 2 == 0:
                            s.act(lambda e, c=c, i=i, tpv=tpv, bb=bb: e.activation(
                                out=H[:, c, i * 128:(i + 1) * 128], in_=tpv[:, c, :], func=AF.Identity,
                                scale=A2[:, c, bb:bb + 1], bias=adaT[:, 24 + c, bb:bb + 1]),
                                reads=["A2", "adaT"], writes=[bkey[tb], "h2T%d_%d" % (hb, i)])
                        else:
                            s.dve(lambda e, c=c, i=i, tpv=tpv, bb=bb: e.tensor_scalar(
                                out=H[:, c, i * 128:(i + 1) * 128], in0=tpv[:, c, :],
                                scalar1=A2[:, c, bb:bb + 1], scalar2=adaT[:, 24 + c, bb:bb + 1], op0=ALU.mult, op1=ALU.add),
                                reads=["A2", "adaT"], writes=[bkey[tb], "h2T%d_%d" % (hb, i)])
                    yield
                lgp = banks[1][:, 0:ntl * 36].rearrange("p (t x) -> p t x", x=36)
                for i in range(ntl):
                    for k in range(8):
                        s.pe(lambda e, k=k, i=i: e.matmul(lgp[:, i, :], lhsT=H[:, k, i * 128:(i + 1) * 128],
                                                          rhs=wr_sb[:, k, :], start=(k == 0), stop=(k == 7)),
                             reads=["h2T%d_%d" % (hb, i), "wr"], writes=[bkey[1]])
                    if i % 4 == 3:
                        yield
                R = ["rt"]
                Tn = ntl
                lgv = LG[:, 0:Tn, :]
                rb3 = bass.AP(rbias, rbias[:].offset, [list(rbias[:].ap[0]), [0, Tn], [1, 36]])
                s.dve(lambda e: e.tensor_tensor(out=lgv, in0=lgp, in1=rb3, op=ALU.add), reads=["rbias"], writes=[bkey[1]] + R)
                gmax = rs[:, 0:Tn, 3]
                s.dve(lambda e: e.tensor_reduce(out=gmax, in_=LG[:, 0:Tn, 0:4], axis=AX.X, op=ALU.max), writes=R)
                s.dve(lambda e: e.tensor_tensor(out=rtmp[:, 0:Tn, 0:4], in0=LG[:, 0:Tn, 0:4], in1=bcl(gmax, 4), op=ALU.subtract),
                      writes=R)
                s.act(lambda e: e.activation(out=rtmp[:, 0:Tn, 4:8], in_=rtmp[:, 0:Tn, 0:4], func=AF.Exp), writes=R)
                s.dve(lambda e: e.tensor_reduce(out=rs[:, 0:Tn, 4], in_=rtmp[:, 0:Tn, 4:8], axis=AX.X, op=ALU.add), writes=R)
                s.dve(lambda e: e.reciprocal(out=rs[:, 0:Tn, 5], in_=rs[:, 0:Tn, 4]), writes=R)
                s.dve(lambda e: e.tensor_tensor(out=ohg[:, 0:Tn, :], in0=LG[:, 0:Tn, 0:4], in1=bcl(gmax, 4), op=ALU.is_equal),
                      writes=R)
                yield
                s.dve(lambda e: e.tensor_tensor(out=rtmp[:, 0:Tn, :].rearrange("p t (g x) -> p t g x", g=4),
                                                in0=LG[:, 0:Tn, 4:36].rearrange("p t (g x) -> p t g x", g=4),
                                                in1=bcl(ohg[:, 0:Tn, :], 8), op=ALU.mult), writes=R)
                esel, oh1, msk, oh2, c8, t8 = (r8[:, j, 0:Tn, :] for j in range(6))
                s.dve(lambda e: e.tensor_reduce(out=esel, in_=rtmp[:, 0:Tn, :].rearrange("p t (g x) -> p t x g", g=4),
                                                axis=AX.X, op=ALU.add), writes=R)
                m1 = rs[:, 0:Tn, 6]
                m2 = rs[:, 0:Tn, 7]
                s.dve(lambda e: e.tensor_reduce(out=m1, in_=esel, axis=AX.X, op=ALU.max), writes=R)
                s.dve(lambda e: e.tensor_tensor(out=oh1, in0=esel, in1=bcl(m1, 8), op=ALU.is_equal), writes=R)
                s.dve(lambda e: e.scalar_tensor_tensor(out=msk, in0=oh1, scalar=-1e9, in1=esel, op0=ALU.mult, op1=ALU.add),
                      writes=R)
                s.dve(lambda e: e.tensor_reduce(out=m2, in_=msk, axis=AX.X, op=ALU.max), writes=R)
                s.dve(lambda e: e.tensor_tensor(out=oh2, in0=msk, in1=bcl(m2, 8), op=ALU.is_equal), writes=R)
                yield
                s.dve(lambda e: e.tensor_tensor(out=rs[:, 0:Tn, 8], in0=m2, in1=m1, op=ALU.subtract), writes=R)
                s.act(lambda e: e.activation(out=rs[:, 0:Tn, 9], in_=rs[:, 0:Tn, 8], func=AF.Exp), writes=R)
                s.dve(lambda e: e.tensor_scalar(out=rs[:, 0:Tn, 10], in0=rs[:, 0:Tn, 9], scalar1=1.0, scalar2=None, op0=ALU.add),
                      writes=R)
                s.dve(lambda e: e.reciprocal(out=rs[:, 0:Tn, 11], in_=rs[:, 0:Tn, 10]), writes=R)
                s.dve(lambda e: e.tensor_tensor(out=rs[:, 0:Tn, 12], in0=rs[:, 0:Tn, 11], in1=rs[:, 0:Tn, 5], op=ALU.mult),
                      writes=R)
                s.dve(lambda e: e.tensor_tensor(out=rs[:, 0:Tn, 13], in0=rs[:, 0:Tn, 12], in1=rs[:, 0:Tn, 9], op=ALU.mult),
                      writes=R)
                s.dve(lambda e: e.tensor_tensor(out=c8, in0=oh1, in1=bcl(rs[:, 0:Tn, 12], 8), op=ALU.mult), writes=R)
                s.dve(lambda e: e.tensor_tensor(out=t8, in0=oh2, in1=bcl(rs[:, 0:Tn, 13], 8), op=ALU.mult), writes=R)
                s.dve(lambda e: e.tensor_tensor(out=c8, in0=c8, in1=t8, op=ALU.add), writes=R)
                c8b = bass.AP(r8, r8[:, 4, 0:Tn, :].offset, [list(r8[:].ap[0]), [8, Tn], [0, 4], [1, 8]])
                s.dve(lambda e: e.tensor_tensor(out=comb[:, 0:Tn, :].rearrange("p t (g x) -> p t g x", g=4),
                                                in0=bcl(ohg[:, 0:Tn, :], 8), in1=c8b, op=ALU.mult), writes=R + ["comb"])
                yield
                for i in range(ntl):
                    tb = 1 - (i // 4)
                    s.pe(lambda e, i=i, tb=tb: e.transpose(out=banks[tb][0:32, (i % 4) * 128:(i % 4 + 1) * 128],
                                                           in_=comb[:, i, :], identity=identf[:]),
                         reads=["comb", "identf"], writes=[bkey[tb]])
                for half in range((ntl + 3) // 4):
                    tb = 1 - half
                    s.act(lambda e, half=half, tb=tb: e.activation(out=combT[:, half * 512:(half + 1) * 512],
                                                                   in_=banks[tb][0:32, :], func=AF.Copy),
                          writes=[bkey[tb], "combT"])
                s.dve(lambda e: e.tensor_copy(out=chi[hb][:, 0:G], in_=combT[:, 0:G]), reads=["combT"], writes=["chi%d" % hb])
                yield

            fic = [0]

            def finalize(gi):
                grp = groups[gi]
                ntl = len(grp)
                base_f = fic[0]
                fic[0] += ntl

                def floads(i):
                    kind, b, ti = grp[i]
                    fsl = (base_f + i) % 2
                    bb = b if kind == "p" else 4 + b
                    gt = gtile(kind, b, ti)
                    s.dma("sp", xfin[fsl][:], X1d[gt * 128:(gt + 1) * 128, :], writes=["xfin%d" % fsl])
                    s.dma("sp", G2t[fsl][:], AP(Gd, (bb * 2 + 1) * D, [[0, 128], [1, D]]), writes=["G2t%d" % fsl])

                floads(0)
                for i in range(ntl):
                    s.act(lambda e, i=i: e.activation(out=junk2[:], in_=yacc[:, i, :], func=AF.Square, scale=1.0 / 32,
                                                      accum_out=fs[:, i:i + 1]), reads=["yacc%d" % i], writes=["junk2", "fs_ms"])
                    if i % 2 == 1:
                        yield
                s.act(lambda e: e.activation(out=fs[:, 8:8 + ntl], in_=fs[:, 0:ntl], func=AF.Ln, bias=1e-6, scale=1.0),
                      reads=["fs_ms"], writes=["fs_ln"])
                s.act(lambda e: e.activation(out=fs[:, 16:16 + ntl], in_=fs[:, 8:8 + ntl], func=AF.Exp, scale=-0.5),
                      reads=["fs_ln"], writes=["fs_rstd"])
                for i, (kind, b, ti) in enumerate(grp):
                    fsl = (base_f + i) % 2
                    xk = "xfin%d" % fsl
                    gk = "G2t%d" % fsl
                    yk = "yacc%d" % i
                    if i + 1 < ntl:
                        floads(i + 1)
                    s.dve(lambda e, i=i, fsl=fsl: e.scalar_tensor_tensor(
                        out=tmp2[:], in0=yacc[:, i, :], scalar=fs[:, 16 + i:17 + i], in1=G2t[fsl][:],
                        op0=ALU.mult, op1=ALU.mult), reads=[yk, "fs_rstd", gk], writes=["tmp2"])
                    s.dve(lambda e, fsl=fsl: e.tensor_tensor(out=xfin[fsl][:], in0=tmp2[:], in1=xfin[fsl][:], op=ALU.add),
                          reads=["tmp2"], writes=[xk])
                    if kind == "p":
                        s.dma("sp", yp[b, ti * 128:(ti + 1) * 128, :], xfin[fsl][:], reads=[xk])
                    else:
                        s.dma("sp", ys[b, :, :], xfin[fsl][0:64, :], reads=[xk])
                    yield

            ldc = [0]

            def load_gu(slot, eb):
                for j in range(2):
                    e_ = eb * 2 + j
                    s.dma("pool", wg2[slot][:, :, j * 256:(j + 1) * 256], w_eg[e_].rearrange("(c p) n -> p c n", p=128),
                          writes=["wg2_%d" % slot])
                    s.dma("pool", wu2[slot][:, :, j * 256:(j + 1) * 256], w_eu[e_].rearrange("(c p) n -> p c n", p=128),
                          writes=["wu2_%d" % slot])

            def load_d(slot, eb):
                for j in range(2):
                    e_ = eb * 2 + j
                    s.dma("pool", wd2[slot][:, 2 * j:2 * j + 2, :], w_ed[e_].rearrange("(c p) n -> p c n", p=128),
                          writes=["wd2_%d" % slot])

            aux = []
            fin_ids = set()

            def aux_step():
                while aux:
                    try:
                        next(aux[0])
                        return
                    except StopIteration:
                        aux.pop(0)

            def aux_drain(n_keep=0):
                while len(aux) > n_keep:
                    try:
                        next(aux[0])
                    except StopIteration:
                        aux.pop(0)

            for _ in prologue(0):
                pass
            load_gu(0, 0)
            load_d(0, 0)

            def run_group(gi, grp):
                ntl = len(grp)
                G = ntl * 128
                nblk = (G + 511) // 512
                bw = min(512, G)
                hb = gi % 2
                H = h2T[hb]

                def gu(eb):
                    slot = ldc[0] % 2
                    hs = eb % 2
                    for j in range(2):
                        e_ = eb * 2 + j
                        for blk in range(nblk):
                            bk = blk % 2
                            s.pe(lambda e, bk=bk, e_=e_, blk=blk: e.matmul(
                                banks[bk][:, 0:bw], lhsT=sel[:, e_, :], rhs=chi[hb][:, blk * 512:blk * 512 + bw],
                                start=True, stop=True), reads=["sel", "chi%d" % hb], writes=[bkey[bk]])
                            s.act(lambda e, bk=bk, j=j, blk=blk, hs=hs: e.activation(
                                out=cb_sb[hs][:, j, blk * 512:blk * 512 + bw], in_=banks[bk][:, 0:bw], func=AF.Copy),
                                writes=[bkey[bk], "cb%d" % hs])
                    for fc in range(4):
                        j = fc // 2
                        for blk in range(nblk):
                            n_ = fc * nblk + blk
                            bg = 2 + n_ % 2
                            bu = 4 + n_ % 2
                            i2 = n_ % 2
                            hkeys = ["h2T%d_%d" % (hb, i) for i in range(blk * 4, min(ntl, blk * 4 + 4))]
                            for k in range(8):
                                s.pe(lambda e, bg=bg, k=k, fc=fc, blk=blk, slot=slot: e.matmul(
                                    banks[bg][:, 0:bw], lhsT=wg2[slot][:, k, fc * 128:(fc + 1) * 128],
                                    rhs=H[:, k, blk * 512:blk * 512 + bw], start=(k == 0), stop=(k == 7)),
                                    reads=hkeys + ["wg2_%d" % slot], writes=[bkey[bg]])
                            for k in range(8):
                                s.pe(lambda e, bu=bu, k=k, fc=fc, blk=blk, slot=slot: e.matmul(
                                    banks[bu][:, 0:bw], lhsT=wu2[slot][:, k, fc * 128:(fc + 1) * 128],
                                    rhs=H[:, k, blk * 512:blk * 512 + bw], start=(k == 0), stop=(k == 7)),
                                    reads=hkeys + ["wu2_%d" % slot], writes=[bkey[bu]])
                            s.act(lambda e, bg=bg, i2=i2: e.activation(out=sg[i2][:, 0:bw], in_=banks[bg][:, 0:bw], func=AF.Silu),
                                  writes=[bkey[bg], "sg%d" % i2])
                            s.dve(lambda e, i2=i2, j=j, blk=blk, hs=hs: e.tensor_tensor(
                                out=sgc[i2][:, 0:bw], in0=sg[i2][:, 0:bw], in1=cb_sb[hs][:, j, blk * 512:blk * 512 + bw],
                                op=ALU.mult), reads=["sg%d" % i2, "cb%d" % hs], writes=["sgc%d" % i2])
                            s.dve(lambda e, bu=bu, i2=i2, fc=fc, blk=blk, hs=hs: e.tensor_tensor(
                                out=he[hs][:, fc, blk * 512:blk * 512 + bw], in0=banks[bu][:, 0:bw], in1=sgc[i2][:, 0:bw],
                                op=ALU.mult), reads=["sgc%d" % i2], writes=[bkey[bu], "he%d" % hs])
                            aux_step()

                def down(eb, dslot):
                    hs = eb % 2
                    for i in range(ntl):
                        for sl in range(2):
                            n_ = i * 2 + sl
                            bd = 6 + n_ % 2
                            for fc in range(4):
                                s.pe(lambda e, bd=bd, fc=fc, i=i, sl=sl, hs=hs, dslot=dslot: e.matmul(
                                    banks[bd][:], lhsT=he[hs][:, fc, i * 128:(i + 1) * 128],
                                    rhs=wd2[dslot][:, fc, sl * 512:(sl + 1) * 512], start=(fc == 0), stop=(fc == 3)),
                                    reads=["he%d" % hs, "wd2_%d" % dslot], writes=[bkey[bd]])
                            yk = "yacc%d" % i
                            if eb == 0:
                                s.dve(lambda e, bd=bd, i=i, sl=sl: e.tensor_copy(out=yacc[:, i, sl * 512:(sl + 1) * 512],
                                                                                in_=banks[bd][:]), writes=[bkey[bd], yk])
                            else:
                                s.dve(lambda e, bd=bd, i=i, sl=sl: e.tensor_tensor(
                                    out=yacc[:, i, sl * 512:(sl + 1) * 512], in0=banks[bd][:],
                                    in1=yacc[:, i, sl * 512:(sl + 1) * 512], op=ALU.add), writes=[bkey[bd], yk])

                dslots = {}
                for eb in range(17):
                    if eb == 3 and gi + 1 < len(groups):
                        aux.append(prologue(gi + 1))
                    if eb < 16:
                        dslots[eb] = ldc[0] % 2
                        if eb + 1 < 16:
                            load_gu((ldc[0] + 1) % 2, eb + 1)
                        elif gi + 1 < len(groups):
                            load_gu((ldc[0] + 1) % 2, 0)
                        gu(eb)
                    if eb == 1:
                        while aux and id(aux[0]) in fin_ids:
                            for _ in aux[0]:
                                pass
                            aux.pop(0)
                    if eb >= 1:
                        down(eb - 1, dslots[eb - 1])
                    if eb < 16:
                        if eb + 1 < 16:
                            load_d((ldc[0] + 1) % 2, eb + 1)
                        elif gi + 1 < len(groups):
                            load_d((ldc[0] + 1) % 2, 0)
                        ldc[0] += 1
                aux_drain(0)
                fin = finalize(gi)
                aux.append(fin)
                fin_ids.add(id(fin))

            for gi, grp in enumerate(groups):
                run_group(gi, grp)
            aux_drain(0)
            s.emit()
    return nc


_PROG = {}


def kernel(**inputs):
    f = lambda a: np.ascontiguousarray(np.asarray(a, dtype=np.float32))
    if "nc" not in _PROG:
        _PROG["nc"] = build_program()
    nc = _PROG["nc"]
    shared = {
        "w_ada": f(inputs["w_ada"][0]), "b_ada": f(inputs["b_ada"][0]),
        "g_pre_mix": f(inputs["g_pre_mix"][0]), "g_post_mix": f(inputs["g_post_mix"][0]),
        "g_pre_ffn": f(inputs["g_pre_ffn"][0]), "g_post_ffn": f(inputs["g_post_ffn"][0]),
        "w_in": f(inputs["w_in"][0]), "a_sinks": f(inputs["a_sinks"][0]), "t5_table": f(inputs["t5_table"]),
        "b_rel": f(inputs["b_rel_table"][0]), "w_pa": f(inputs["w_proj_a"][0]), "w_pb": f(inputs["w_proj_b"][0]),
        "w_gate": f(inputs["w_gate"][0]), "b_gate": f(inputs["b_gate"][0]), "w_o": f(inputs["w_o"][0]),
        "w_rg": f(inputs["w_route_g"][0]), "b_rg": f(inputs["b_route_g"][0]),
        "w_re": f(inputs["w_route_e"][0]), "b_re": f(inputs["b_route_e"][0]),
        "w_eg": f(inputs["w_e_gate"][0]), "w_eu": f(inputs["w_e_up"][0]), "w_ed": f(inputs["w_e_down"][0]),
    }
    in_maps = []
    for c in range(NCORES):
        sl = slice(c * NB, (c + 1) * NB)
        m = dict(shared)
        m["xp"] = f(inputs["x_prompt"][sl]); m["xs"] = f(inputs["x_sample"][sl])
        m["cp"] = f(inputs["c_prompt"][sl]); m["cs"] = f(inputs["c_sample"][sl])
        m["cak"] = f(inputs["cache_a_k"][0, sl]).reshape(NB, 128, 128)
        m["cav"] = f(inputs["cache_a_v"][0, sl]).reshape(NB, 128, 128)
        m["cbk"] = f(inputs["cache_b_k"][0, sl]).reshape(NB, 512, 512)
        m["cbv"] = f(inputs["cache_b_v"][0, sl]).reshape(NB, 512, 512)
        in_maps.append(m)
    res = run_bass_kernel_spmd(nc, in_maps, core_ids=list(range(NCORES)))
    R = res.results
    cat = lambda k: np.concatenate([np.asarray(r[k], dtype=np.float32) for r in R], axis=0)
    y_p = cat("yp"); y_s = cat("ys")
    nakp = cat("nakp").reshape(1, 32, 128, 2, 64); navp = cat("navp").reshape(1, 32, 128, 2, 64)
    nbkp = cat("nbkp").reshape(1, 32, 512, 8, 64); nbvp = cat("nbvp").reshape(1, 32, 512, 8, 64)
    naks = cat("naks").reshape(1, 32, 128, 2, 64); navs = cat("navs").reshape(1, 32, 128, 2, 64)
    nbks = cat("nbks").reshape(1, 32, 512, 8, 64); nbvs = cat("nbvs").reshape(1, 32, 512, 8, 64)
    return (y_p, y_s, nakp, navp, nbkp, nbvp, naks, navs, nbks, nbvs)
```

```python
import math
from contextlib import ExitStack

import numpy as np
import jax
import jax.numpy as jnp

import concourse.bass as bass
import concourse.mybir as mybir
from concourse.bass_utils import run_bass_kernel_spmd

F32 = mybir.dt.float32
BF16 = mybir.dt.bfloat16
AF = mybir.ActivationFunctionType
ALU = mybir.AluOpType
AX = mybir.AxisListType

ENGS = ("pe", "act", "dve", "pool", "sp")
NDSEM = 8
NCORES = 8
NB = 4
SEQ = 2048
D = 1024
NEG = -30000.0


class SemState:
    def __init__(self, nc, stack):
        self.esem = {e: stack.enter_context(nc.semaphore("s_" + e)) for e in ENGS}
        self.ecnt = {e: 0 for e in ENGS}
        self.dsem = {q: [stack.enter_context(nc.semaphore("d_%s%d" % (q, i))) for i in range(NDSEM)]
                     for q in ("sp", "act", "pool")}
        self.dcnt = {q: 0 for q in ("sp", "act", "pool")}
        self.seen = {e: {} for e in ENGS}


class Op:
    __slots__ = ("eng", "fn", "dma", "idx", "deps", "sig", "sigval", "dsem", "dval", "dprev")

    def __init__(self, eng, fn, dma):
        self.eng = eng
        self.fn = fn
        self.dma = dma
        self.deps = []
        self.sig = False
        self.sigval = None
        self.dsem = None


class Sched:
    def __init__(self, nc, st):
        self.nc = nc
        self.st = st
        self.ops = {e: [] for e in ENGS}
        self.last_w = {}
        self.readers = {}

    def add(self, eng, fn, reads=(), writes=(), dma=False):
        op = Op(eng, fn, dma)
        op.idx = len(self.ops[eng])
        deps = {}
        for k in reads:
            w = self.last_w.get(k)
            if w is not None:
                deps[id(w)] = w
        for k in writes:
            w = self.last_w.get(k)
            if w is not None:
                deps[id(w)] = w
            for r in self.readers.get(k, ()):
                deps[id(r)] = r
        op.deps = list(deps.values())
        for k in writes:
            self.last_w[k] = op
            self.readers[k] = []
        for k in reads:
            if k not in writes:
                self.readers.setdefault(k, []).append(op)
        self.ops[eng].append(op)
        return op

    def pe(self, fn, reads=(), writes=()):
        return self.add("pe", fn, reads, writes)

    def act(self, fn, reads=(), writes=()):
        return self.add("act", fn, reads, writes)

    def dve(self, fn, reads=(), writes=()):
        return self.add("dve", fn, reads, writes)

    def pool(self, fn, reads=(), writes=()):
        return self.add("pool", fn, reads, writes)

    def dma(self, q, out, in_, reads=(), writes=(), **kw):
        return self.add(q, lambda e: e.dma_start(out=out, in_=in_, **kw), reads, writes, dma=True)

    @staticmethod
    def _needs_sync(p, c):
        if p.dma or c.dma:
            return True
        if p.eng != c.eng:
            return True
        if p.eng == "pe":
            return False
        return (c.idx - p.idx) <= 2

    def emit(self):
        nc, st = self.nc, self.st
        for e in ENGS:
            for o in reversed(self.ops[e]):
                if not o.dma:
                    o.sig = True
                    break
        for e in ENGS:
            for c in self.ops[e]:
                for p in c.deps:
                    if (not p.dma) and self._needs_sync(p, c):
                        p.sig = True
        for e in ENGS:
            for o in self.ops[e]:
                if o.dma:
                    n = st.dcnt[e]
                    st.dcnt[e] += 1
                    o.dsem = st.dsem[e][n % NDSEM]
                    o.dval = 16 * (n // NDSEM + 1)
                    o.dprev = 16 * (n // NDSEM)
                elif o.sig:
                    st.ecnt[e] += 1
                    o.sigval = st.ecnt[e]
        end_e = dict(st.ecnt)
        end_d = dict(st.dcnt)

        def emit_engine(e, eng):
            seen = st.seen[e]

            def wait(sem, val):
                k = id(sem)
                if val <= 0 or seen.get(k, 0) >= val:
                    return
                eng.wait_ge(sem, val)
                seen[k] = val

            for o in self.ops[e]:
                for p in o.deps:
                    if p.dma:
                        wait(p.dsem, p.dval)
                    elif self._needs_sync(p, o):
                        wait(st.esem[p.eng], p.sigval)
                if o.dma:
                    wait(o.dsem, o.dprev)
                    o.fn(eng).then_inc(o.dsem, 16)
                else:
                    ins = o.fn(eng)
                    if o.sig:
                        ins.then_inc(st.esem[e], 1)
            for e2 in ENGS:
                if e2 != e:
                    wait(st.esem[e2], end_e[e2])
            for q in st.dsem:
                n = end_d[q]
                for i in range(NDSEM):
                    if n > i:
                        last_n = ((n - 1 - i) // NDSEM) * NDSEM + i
                        wait(st.dsem[q][i], 16 * (last_n // NDSEM + 1))

        with nc.Block() as block:
            @block.tensor
            def _(eng):
                emit_engine("pe", eng)

            @block.scalar
            def _(eng):
                emit_engine("act", eng)

            @block.vector
            def _(eng):
                emit_engine("dve", eng)

            @block.gpsimd
            def _(eng):
                emit_engine("pool", eng)

            @block.sync
            def _(eng):
                emit_engine("sp", eng)


def AP(t, off, ap):
    return bass.AP(t.tensor, off, ap)


def bcl(ap, n):
    return bass.AP(ap.tensor, ap.offset, [list(x) for x in ap.ap] + [[0, n]])


def t5_runs():
    cpu = jax.devices("cpu")[0]
    with jax.default_device(cpu):
        rel = -(jnp.arange(383, dtype=jnp.int32) - 127)
        half, exact = 16, 8
        ret = jnp.where(rel > 0, half, 0)
        n = jnp.abs(rel)
        nf = jnp.maximum(n, 1).astype(jnp.float32)
        large = exact + (jnp.log(nf / exact) / math.log(128 / exact) * (half - exact)).astype(jnp.int32)
        large = jnp.minimum(large, half - 1)
        bucket = np.asarray(ret + jnp.where(n < exact, n, large))
    runs = []
    j0 = 0
    for j in range(1, 384):
        if j == 383 or bucket[j] != bucket[j0]:
            runs.append((j0, j, int(bucket[j0])))
            j0 = j
    return runs


def build_program():
    nc = bass.Bass("TRN2", target_bir_lowering=False)

    def din(name, shape):
        return nc.dram_tensor(name, list(shape), F32, kind="ExternalInput").ap()

    def dout(name, shape):
        return nc.dram_tensor(name, list(shape), F32, kind="ExternalOutput").ap()

    xp = din("xp", [NB, SEQ, D]); xsm = din("xs", [NB, 64, D])
    cpr = din("cp", [NB, D]); csm = din("cs", [NB, D])
    cak = din("cak", [NB, 128, 128]); cav = din("cav", [NB, 128, 128])
    cbk = din("cbk", [NB, 512, 512]); cbv = din("cbv", [NB, 512, 512])
    w_ada = din("w_ada", [D, 6 * D]); b_ada = din("b_ada", [6 * D])
    g_pre_mix = din("g_pre_mix", [D]); g_post_mix = din("g_post_mix", [D])
    g_pre_ffn = din("g_pre_ffn", [D]); g_post_ffn = din("g_post_ffn", [D])
    w_in = din("w_in", [D, 2304]); a_sinks = din("a_sinks", [8]); t5_table = din("t5_table", [32, 8])
    b_rel = din("b_rel", [8, 513]); w_pa = din("w_pa", [512, D]); w_pb = din("w_pb", [512, D])
    w_gate = din("w_gate", [D, 2 * D]); b_gate = din("b_gate", [2 * D]); w_o = din("w_o", [D, D])
    w_rg = din("w_rg", [D, 4]); b_rg = din("b_rg", [4]); w_re = din("w_re", [D, 32]); b_re = din("b_re", [32])
    w_eg = din("w_eg", [32, D, 256]); w_eu = din("w_eu", [32, D, 256]); w_ed = din("w_ed", [32, 256, D])

    yp = dout("yp", [NB, SEQ, D]); ys = dout("ys", [NB, 64, D])
    nakp = dout("nakp", [NB, 128, 128]); navp = dout("navp", [NB, 128, 128])
    nbkp = dout("nbkp", [NB, 512, 512]); nbvp = dout("nbvp", [NB, 512, 512])
    naks = dout("naks", [NB, 128, 128]); navs = dout("navs", [NB, 128, 128])
    nbks = dout("nbks", [NB, 512, 512]); nbvs = dout("nbvs", [NB, 512, 512])

    NT = NB * 16 + NB
    X1d = nc.dram_tensor("X1d", [NT * 128, D], F32).ap()
    Gd = nc.dram_tensor("Gd", [8, 2, D], F32).ap()
    extB = nc.dram_tensor("extB", [8, 768], F32).ap()
    SB_ = nc.dram_tensor("SBd", [128, 8 * 768], F32).ap()
    extA = nc.dram_tensor("extA", [8, 384], F32).ap()
    SA_ = nc.dram_tensor("SAd", [128, 8 * 384], F32).ap()

    with ExitStack() as es:
        st = SemState(nc, es)

        def T(stack, name, shape, dt):
            return stack.enter_context(nc.sbuf_tensor(name, list(shape), dt))

        banks = [es.enter_context(nc.psum_tensor("bank%d" % i, [128, 512], F32)) for i in range(8)]
        bkey = ["bank%d" % i for i in range(8)]

        ident = T(es, "ident", [128, 128], BF16)
        identf = T(es, "identf", [128, 128], F32)
        adaT = T(es, "adaT", [128, 48, 8], F32)
        A1 = T(es, "A1", [128, 8, 8], F32)
        A2 = T(es, "A2", [128, 8, 8], F32)
        bgT = T(es, "bgT", [128, 16], F32)
        expsink = T(es, "expsink", [128, 8], F32)
        wr_sb = T(es, "wr_sb", [128, 8, 36], BF16)
        rbias = T(es, "rbias", [128, 36], F32)
        rstd_all = T(es, "rstd_all", [128, 68], F32)

        with ExitStack() as e0:
            s = Sched(nc, st)
            wada = T(e0, "wada", [128, 8, 6 * D], BF16)
            c8 = T(e0, "c8", [8, D], F32)
            sc8 = T(e0, "sc8", [8, D], BF16)
            cT = T(e0, "cT", [128, 8, 8], BF16)
            colsrc = T(e0, "colsrc", [80, 128], F32)
            colT = T(e0, "colT", [128, 80], F32)
            t5src = T(e0, "t5src", [32, 8], F32)
            t5T = T(e0, "t5T", [8, 32], F32)
            extA_sb = T(e0, "extA_sb", [8, 384], F32)
            extB_sb = T(e0, "extB_sb", [8, 768], F32)
            tmp88 = T(e0, "tmp88", [128, 8, 8], F32)
            bada8 = T(e0, "bada8", [8, 2, D], F32)
            gpost8 = T(e0, "gpost8", [8, 2, D], F32)
            G8 = T(e0, "G8", [8, 2, D], F32)
            sink_t = T(e0, "sink_t", [128, 8], F32)

            s.pool(lambda e: e.memset(identf[:], 0.0), writes=["identf"])
            s.pool(lambda e: e.affine_select(out=identf[:], in_=identf[:], pattern=[[-1, 128]],
                                              compare_op=ALU.not_equal, fill=1.0, base=0, channel_multiplier=1),
                   writes=["identf"])
            s.dve(lambda e: e.tensor_copy(out=ident[:], in_=identf[:]), reads=["identf"], writes=["ident"])

            s.dma("sp", sink_t[:], AP(a_sinks, 0, [[0, 128], [1, 8]]), writes=["sink_t"])
            s.act(lambda e: e.activation(out=expsink[:], in_=sink_t[:], func=AF.Exp), reads=["sink_t"], writes=["expsink"])
            s.dma("pool", wr_sb[:, :, 0:4], w_rg.rearrange("(c p) n -> p c n", p=128), writes=["wr"])
            s.dma("pool", wr_sb[:, :, 4:36], w_re.rearrange("(c p) n -> p c n", p=128), writes=["wr"])
            s.dma("sp", rbias[:, 0:4], AP(b_rg, 0, [[0, 128], [1, 4]]), writes=["rbias"])
            s.dma("sp", rbias[:, 4:36], AP(b_re, 0, [[0, 128], [1, 32]]), writes=["rbias"])
            s.dma("sp", extB_sb[:, 0:384], b_rel[:, 129:513], writes=["extB_sb"])
            s.dve(lambda e: e.tensor_copy(out=extB_sb[:, 384:768], in_=bcl(extB_sb[:, 383], 384)), writes=["extB_sb"])
            s.dma("sp", extB, extB_sb[:], reads=["extB_sb"], writes=["extB"])
            s.dma("sp", SB_, AP(extB, 0, [[0, 128], [1, 8 * 768]]), reads=["extB"], writes=["SBd"])
            s.dma("sp", t5src[:], t5_table, writes=["t5src"])
            s.pe(lambda e: e.transpose(out=banks[3][0:8, 0:32], in_=t5src[:], identity=identf[0:32, 0:32]),
                 reads=["t5src", "identf"], writes=[bkey[3]])
            s.dve(lambda e: e.tensor_copy(out=t5T[:], in_=banks[3][0:8, 0:32]), writes=[bkey[3], "t5T"])
            for (j0, j1, bu) in t5_runs():
                s.dve(lambda e, j0=j0, j1=j1, bu=bu: e.tensor_copy(out=extA_sb[:, j0:j1], in_=bcl(t5T[:, bu], j1 - j0)),
                      reads=["t5T"], writes=["extA_sb"])
            s.dve(lambda e: e.tensor_copy(out=extA_sb[:, 383:384], in_=t5T[:, 0:1]), reads=["t5T"], writes=["extA_sb"])
            s.dma("sp", extA, extA_sb[:], reads=["extA_sb"], writes=["extA"])
            s.dma("sp", SA_, AP(extA, 0, [[0, 128], [1, 8 * 384]]), reads=["extA"], writes=["SAd"])
            s.dma("sp", c8[0:4, :], cpr, writes=["c8"])
            s.dma("sp", c8[4:8, :], csm, writes=["c8"])
            s.act(lambda e: e.activation(out=sc8[:], in_=c8[:], func=AF.Silu), reads=["c8"], writes=["sc8"])
            tp = banks[0][:].bitcast(BF16)
            for k in range(8):
                s.pe(lambda e, k=k: e.transpose(out=tp[:, k * 8:(k + 1) * 8], in_=sc8[0:8, k * 128:(k + 1) * 128],
                                                identity=ident[0:8, 0:8]),
                     reads=["sc8", "ident"], writes=[bkey[0]])
            s.dve(lambda e: e.tensor_copy(out=cT[:].rearrange("p k b -> p (k b)"), in_=tp[:, 0:64]),
                  writes=[bkey[0], "cT"])
            for k in range(8):
                s.dma("pool", wada[:, k, :], w_ada[k * 128:(k + 1) * 128, :], writes=["wada%d" % k])
            s.dma("sp", colsrc[0:48, :], b_ada.rearrange("(c p) -> c p", p=128), writes=["colsrc"])
            s.dma("sp", colsrc[48:56, :], g_pre_mix.rearrange("(c p) -> c p", p=128), writes=["colsrc"])
            s.dma("sp", colsrc[56:64, :], g_pre_ffn.rearrange("(c p) -> c p", p=128), writes=["colsrc"])
            s.dma("sp", colsrc[64:80, :], b_gate.rearrange("(c p) -> c p", p=128), writes=["colsrc"])
            s.pe(lambda e: e.transpose(out=banks[2][:, 0:80], in_=colsrc[0:80, :], identity=identf[0:80, 0:80]),
                 reads=["colsrc", "identf"], writes=[bkey[2]])
            s.dve(lambda e: e.tensor_copy(out=colT[:], in_=banks[2][:, 0:80]), writes=[bkey[2], "colT"])
            s.dve(lambda e: e.tensor_copy(out=bgT[:], in_=colT[:, 64:80]), reads=["colT"], writes=["bgT"])
            pa_ = banks[1][:, 0:384].rearrange("p (f b) -> p f b", b=8)
            for f in range(48):
                for k in range(8):
                    s.pe(lambda e, f=f, k=k: e.matmul(pa_[:, f, :], lhsT=wada[:, k, f * 128:(f + 1) * 128],
                                                      rhs=cT[:, k, :], start=(k == 0), stop=(k == 7)),
                         reads=["cT", "wada%d" % k], writes=[bkey[1]])
            s.dve(lambda e: e.tensor_tensor(out=adaT[:], in0=pa_, in1=bcl(colT[:, 0:48], 8), op=ALU.add),
                  reads=["colT"], writes=[bkey[1], "adaT"])
            for (Ax, gp, c0, nm) in ((A1, colT[:, 48:56], 8, "A1"), (A2, colT[:, 56:64], 32, "A2")):
                s.dve(lambda e, c0=c0: e.tensor_scalar(out=tmp88[:], in0=adaT[:, c0:c0 + 8, :], scalar1=1.0, scalar2=None,
                                                       op0=ALU.add), reads=["adaT"], writes=["tmp88"])
                s.dve(lambda e, Ax=Ax, gp=gp: e.tensor_tensor(out=Ax[:], in0=tmp88[:], in1=bcl(gp, 8),
                                                              op=ALU.mult), reads=["tmp88", "colT"], writes=[nm])
            for v, c0 in ((0, 2 * D), (1, 5 * D)):
                s.dma("sp", bada8[:, v, :], AP(b_ada, c0, [[0, 8], [1, D]]), writes=["bada8"])
            s.dma("sp", gpost8[:, 0, :], AP(g_post_mix, 0, [[0, 8], [1, D]]), writes=["gpost8"])
            s.dma("sp", gpost8[:, 1, :], AP(g_post_ffn, 0, [[0, 8], [1, D]]), writes=["gpost8"])
            for v, c0 in ((0, 2 * D), (1, 5 * D)):
                for sl in range(2):
                    bk = 2 + (v * 2 + sl) % 2
                    for k in range(8):
                        s.pe(lambda e, k=k, bk=bk, c=c0 + sl * 512: e.matmul(
                            banks[bk][0:8, :], lhsT=cT[:, k, :], rhs=wada[:, k, c:c + 512],
                            start=(k == 0), stop=(k == 7)),
                            reads=["cT", "wada%d" % k], writes=[bkey[bk]])
                    s.dve(lambda e, v=v, sl=sl, bk=bk: e.tensor_tensor(
                        out=G8[:, v, sl * 512:(sl + 1) * 512], in0=banks[bk][0:8, :],
                        in1=bada8[:, v, sl * 512:(sl + 1) * 512], op=ALU.add),
                        reads=["bada8"], writes=[bkey[bk], "G8"])
            s.dve(lambda e: e.tensor_tensor(out=G8[:], in0=G8[:], in1=gpost8[:], op=ALU.mult),
                  reads=["gpost8"], writes=["G8"])
            s.dma("sp", Gd, G8[:], reads=["G8"], writes=["Gd"])
            s.emit()

        with ExitStack() as e1:
            s = Sched(nc, st)
            biasB = T(e1, "biasB", [128, 8, 5, 128], F32)
            biasA = T(e1, "biasA", [128, 8, 2, 128], F32)
            w_in_sb = T(e1, "w_in_sb", [128, 8, 2304], BF16)
            wg_sb = T(e1, "wg_sb", [128, 8, 2048], BF16)
            wpa_sb = T(e1, "wpa_sb", [128, 4, D], BF16)
            wpb_sb = T(e1, "wpb_sb", [128, 4, D], BF16)
            wo_sb = T(e1, "wo_sb", [128, 8, D], BF16)
            xt = [T(e1, "xt%d" % i, [128, 2, D], F32) for i in range(2)]
            xnb = [T(e1, "xn%d" % i, [128, D], BF16) for i in range(2)]
            hTb = [T(e1, "hT%d" % i, [128, 8, 256], BF16) for i in range(2)]
            qaT = T(e1, "qaT", [128, 4, 256], BF16)
            qbT = T(e1, "qbT", [128, 4, 256], BF16)
            kaT = T(e1, "kaT", [128, 1024], BF16)
            kbT = T(e1, "kbT", [128, 4, 1024], BF16)
            va = T(e1, "va", [128, 8, 2, 65], BF16)
            vb = T(e1, "vb", [128, 8, 8, 65], BF16)
            tmpf = T(e1, "tmpf", [128, 512], F32)
            PT = [T(e1, "PT%d" % i, [128, 512], BF16) for i in range(3)]
            on = T(e1, "on", [128, D], BF16)
            junk = on[:]
            oT = T(e1, "oT", [128, 8, 256], BF16)
            sig = T(e1, "sig", [128, 512], F32)
            prod = T(e1, "prod", [128, 512], F32)
            mT = T(e1, "mT", [128, 8, 256], BF16)
            G1bc = T(e1, "G1bc", [128, D], F32)
            stt = T(e1, "stt", [128, 32], F32)
            kst = [sig, prod]
            xt1b = xt[1][:].bitcast(BF16)
            ctile = xt1b[:, 0, :].rearrange("p (t f) -> p t f", t=4)
            catile = xt1b[:, 1, 0:128]

            s.dma("pool", w_in_sb[:, :, 512:2304], w_in[:, 512:2304].rearrange("(c p) n -> p c n", p=128), writes=["w_in_rest"])
            qst = xt[1][:].bitcast(BF16)[:, 0, :].rearrange("p (c n) -> p c n", c=4)
            qst2 = xt[1][:].bitcast(BF16)[:, 1, :].rearrange("p (c n) -> p c n", c=4)
            s.dma("pool", qst, w_in[0:512, 0:512].rearrange("(c p) n -> p c n", p=128), writes=["xt1"])
            s.dma("pool", qst2, w_in[512:1024, 0:512].rearrange("(c p) n -> p c n", p=128), writes=["xt1"])
            for half, src in ((0, qst), (1, qst2)):
                for g in range(4):
                    for kv in range(2):
                        dst = w_in_sb[:, half * 4:(half + 1) * 4, g * 128 + kv * 64:g * 128 + (kv + 1) * 64]
                        sv = src[:, :, kv * 256 + g * 64:kv * 256 + (g + 1) * 64]
                        if (g + kv) % 2 == 0:
                            s.dve(lambda e, dst=dst, sv=sv: e.tensor_copy(out=dst, in_=sv), reads=["xt1"], writes=["w_in"])
                        else:
                            s.pool(lambda e, dst=dst, sv=sv: e.tensor_copy(out=dst, in_=sv), reads=["xt1"], writes=["w_in"])
            s.dma("pool", wg_sb[:], w_gate.rearrange("(c p) n -> p c n", p=128), writes=["wg"])
            s.dma("pool", wpa_sb[:], w_pa.rearrange("(c p) n -> p c n", p=128), writes=["wpa"])
            s.dma("pool", wpb_sb[:], w_pb.rearrange("(c p) n -> p c n", p=128), writes=["wpb"])
            s.dma("pool", wo_sb[:], w_o.rearrange("(c p) n -> p c n", p=128), writes=["wo"])
            s.dma("sp", biasB[:], AP(SB_, 127, [[8 * 768 - 1, 128], [768, 8], [128, 5], [1, 128]]), writes=["biasB"])
            s.dma("sp", biasA[:], AP(SA_, 127, [[8 * 384 - 1, 128], [384, 8], [128, 2], [1, 128]]), writes=["biasA"])
            s.dve(lambda e: e.memset(biasB[64:128, :, 0, 0:64], NEG), writes=["biasB"])
            s.dve(lambda e: e.memset(biasB[0:64, :, 4, 64:128], NEG), writes=["biasB"])
            s.dve(lambda e: e.memset(biasA[64:128, :, 0, 0:64], NEG), writes=["biasA"])
            s.dve(lambda e: e.memset(biasA[0:64, :, 1, 64:128], NEG), writes=["biasA"])
            s.pool(lambda e: e.memset(va[:, :, :, 64:65], 1.0), writes=["va%d" % i for i in range(8)])
            s.pool(lambda e: e.memset(vb[:, :, :, 64:65], 1.0), writes=["vb%d" % i for i in range(8)])

            gen_banks = [4, 5, 6, 7]
            gctr = [0]

            def gbank():
                b = gen_banks[gctr[0] % len(gen_banks)]
                gctr[0] += 1
                return b

            wide_banks = [4, 5, 6, 7, 1, 2, 3]
            wctr = [0]

            def wbank():
                b = wide_banks[wctr[0] % len(wide_banks)]
                wctr[0] += 1
                return b

            sctr = [0]
            evc = [0]

            def evac_copy(out, in_, reads, writes, scale=None):
                evc[0] += 1
                if evc[0] % 2 == 0:
                    if scale is None:
                        s.act(lambda e: e.activation(out=out, in_=in_, func=AF.Copy), reads=reads, writes=writes)
                    else:
                        s.act(lambda e: e.activation(out=out, in_=in_, func=AF.Copy, scale=scale), reads=reads, writes=writes)
                else:
                    if scale is None:
                        s.dve(lambda e: e.tensor_copy(out=out, in_=in_), reads=reads, writes=writes)
                    else:
                        s.dve(lambda e: e.tensor_scalar(out=out, in0=in_, scalar1=scale, scalar2=None, op0=ALU.mult),
                              reads=reads, writes=writes)

            def rstd_from(ms_col, out_col, key):
                s.act(lambda e: e.activation(out=stt[:, 15:16], in_=ms_col, func=AF.Ln, bias=1e-6, scale=1.0),
                      reads=[key], writes=["stt_ln"])
                s.act(lambda e: e.activation(out=out_col, in_=stt[:, 15:16], func=AF.Exp, scale=-0.5),
                      reads=["stt_ln"], writes=[key])

            xctr = [0]

            NSB = 3
            SCB = [1, 2, 3]

            def attention_groups(W0, iA, iB):
                out = []
                oA = [gbank(), gbank()]
                for kv in range(2):
                    ob = oA[kv]
                    ov = banks[ob][:, 0:260].rearrange("p (h e) -> p h e", e=65)
                    dl = [d_ for d_ in (1, 0) if iA - d_ >= 0]
                    for di, d_ in enumerate(dl):
                        slot = (iA - d_) % 8
                        pb0 = kv * 64
                        first = (di == 0)
                        last = (di == len(dl) - 1)

                        def S(i2, slot=slot, pb0=pb0):
                            sb = SCB[i2]
                            s.pe(lambda e: e.matmul(
                                banks[sb][:].rearrange("p (g t) -> p g t", g=4),
                                lhsT=kaT[pb0:pb0 + 64, slot * 128:(slot + 1) * 128],
                                rhs=qaT[pb0:pb0 + 64, :, W0:W0 + 128], start=True, stop=True),
                                reads=["kaT%d" % slot, "qaT"], writes=[bkey[sb]])

                        def E(i2, kv=kv, d_=d_):
                            sb = SCB[i2]
                            s.dve(lambda e: e.tensor_tensor(
                                out=banks[sb][:].rearrange("p (g t) -> p g t", g=4),
                                in0=banks[sb][:].rearrange("p (g t) -> p g t", g=4),
                                in1=biasA[:, kv * 4:(kv + 1) * 4, d_, :], op=ALU.add),
                                reads=["biasA"], writes=[bkey[sb]])
                            s.act(lambda e: e.activation(out=PT[i2][:], in_=banks[sb][:], func=AF.Exp),
                                  writes=[bkey[sb], "PT%d" % i2])

                        def V(i2, slot=slot, kv=kv, ov=ov, ob=ob, first=first, last=last):
                            for g in range(4):
                                s.pe(lambda e, g=g: e.matmul(
                                    ov[:, g, :], lhsT=PT[i2][:, g * 128:(g + 1) * 128], rhs=va[:, slot, kv, :],
                                    start=(first and g == 0), stop=False, skip_group_check=True),
                                    reads=["PT%d" % i2, "va%d" % slot], writes=[bkey[ob]])
                            if last:
                                s.dve(lambda e: e.tensor_tensor(
                                    out=stt[:, 0:4], in0=ov[:, :, 64], in1=expsink[:, kv * 4:(kv + 1) * 4], op=ALU.add),
                                    reads=["expsink"], writes=[bkey[ob], "stt_a"])
                                s.dve(lambda e: e.reciprocal(out=stt[:, 4:8], in_=stt[:, 0:4]), reads=["stt_a"], writes=["stt_b"])
                                s.dve(lambda e: e.tensor_tensor(
                                    out=on[:, kv * 256:(kv + 1) * 256].rearrange("p (h d) -> p h d", d=64),
                                    in0=ov[:, :, 0:64], in1=bcl(stt[:, 4:8], 64), op=ALU.mult),
                                    reads=["stt_b"], writes=[bkey[ob], "on"])
                        out.append((S, E, V))
                oB = [gbank(), gbank()]
                blocks = [(h, d_) for h in range(8) for d_ in range(5) if iB - d_ >= 0]
                groups = []
                for blk in blocks:
                    if groups and len(groups[-1]) < 4 and groups[-1][-1][0] == blk[0]:
                        groups[-1].append(blk)
                    else:
                        groups.append([blk])
                nB = len(groups)
                for gi_, grp in enumerate(groups):
                    n = len(grp)
                    h = grp[0][0]
                    d_lo = grp[0][1]
                    ob = oB[h // 4]
                    ov = banks[ob][:, 0:260].rearrange("p (h e) -> p h e", e=65)
                    firstbank = all(g2[0][0] // 4 != h // 4 for g2 in groups[:gi_])
                    lastbank = all(g2[0][0] // 4 != h // 4 for g2 in groups[gi_ + 1:])

                    def S(i2, grp=grp):
                        sb = SCB[i2]
                        for j, (h_, d_) in enumerate(grp):
                            slot = (iB - d_) % 8
                            c, pb0 = h_ // 2, (h_ % 2) * 64
                            s.pe(lambda e, j=j, slot=slot, c=c, pb0=pb0: e.matmul(
                                banks[sb][:, j * 128:(j + 1) * 128],
                                lhsT=kbT[pb0:pb0 + 64, c, slot * 128:(slot + 1) * 128],
                                rhs=qbT[pb0:pb0 + 64, c, W0:W0 + 128], start=True, stop=True),
                                reads=["kbT%d" % slot, "qbT"], writes=[bkey[sb]])

                    def E(i2, n=n, h=h, d_lo=d_lo):
                        sb = SCB[i2]
                        s.dve(lambda e: e.tensor_tensor(
                            out=banks[sb][:, 0:n * 128].rearrange("p (g t) -> p g t", g=n),
                            in0=banks[sb][:, 0:n * 128].rearrange("p (g t) -> p g t", g=n),
                            in1=biasB[:, h, d_lo:d_lo + n, :], op=ALU.add),
                            reads=["biasB"], writes=[bkey[sb]])
                        s.act(lambda e: e.activation(out=PT[i2][:, 0:n * 128], in_=banks[sb][:, 0:n * 128], func=AF.Exp),
                              writes=[bkey[sb], "PT%d" % i2])

                    def V(i2, grp=grp, h=h, ov=ov, ob=ob, firstbank=firstbank, lastbank=lastbank):
                        for j, (h_, d_) in enumerate(grp):
                            slot = (iB - d_) % 8
                            s.pe(lambda e, j=j, slot=slot: e.matmul(
                                ov[:, h % 4, :], lhsT=PT[i2][:, j * 128:(j + 1) * 128], rhs=vb[:, slot, h, :],
                                start=(firstbank and j == 0), stop=False, skip_group_check=True),
                                reads=["PT%d" % i2, "vb%d" % slot], writes=[bkey[ob]])
                        if lastbank:
                            hh = h // 4
                            s.dve(lambda e: e.reciprocal(out=stt[:, 8:12], in_=ov[:, :, 64]), writes=[bkey[ob], "stt_c"])
                            s.dve(lambda e: e.tensor_tensor(
                                out=on[:, 512 + hh * 256:512 + (hh + 1) * 256].rearrange("p (h d) -> p h d", d=64),
                                in0=ov[:, :, 0:64], in1=bcl(stt[:, 8:12], 64), op=ALU.mult),
                                reads=["stt_c"], writes=[bkey[ob], "on"])
                    out.append((S, E, V))

                def Vt(i2):
                    tpv = banks[0][:].bitcast(BF16).rearrange("p (c t) -> p c t", c=8)
                    for c in range(8):
                        s.pe(lambda e, c=c: e.transpose(out=tpv[:, c, :], in_=on[:, c * 128:(c + 1) * 128], identity=ident[:]),
                             reads=["on", "ident"], writes=[bkey[0]])
                    evac_copy(oT[:, :, W0:W0 + 128], tpv, reads=[], writes=[bkey[0], "oT"])
                out.append((None, None, Vt))
                return out

            def run_attention(glist, L=2):
                pend = []
                for (S, E, V) in glist:
                    if S is not None:
                        i2 = sctr[0] % NSB
                        sctr[0] += 1
                        S(i2)
                        E(i2)
                    else:
                        i2 = None
                    pend.append((V, i2))
                    if len(pend) > L:
                        V0, j2 = pend.pop(0)
                        V0(j2)
                while pend:
                    V0, j2 = pend.pop(0)
                    V0(j2)

            xloaded = set()

            def xload(kind, b, s0, nt, n):
                slot_x = (n % 2) if kind == "p" else 0
                xs = xt[slot_x]
                xkey = "xt%d" % slot_x
                xloaded.add(n)
                if kind == "p":
                    s.dma("sp", xs[:, 0:nt, :], xp[b, s0 * 128:(s0 + nt) * 128, :].rearrange("(t p) d -> p t d", p=128),
                          writes=[xkey])
                else:
                    s.dma("sp", xs[0:64, 0, :], xsm[b, :, :], writes=[xkey])

            def prep(kind, b, s0, nt, n):
                slot_x = (n % 2) if kind == "p" else 0
                xs = xt[slot_x]
                xkey = "xt%d" % slot_x
                hT = hTb[n % 2]
                hkey = "hT%d" % (n % 2)
                bb = b if kind == "p" else 4 + b
                if n not in xloaded:
                    xload(kind, b, s0, nt, n)
                for t in range(nt):
                    xn = xnb[t % 2]
                    xnk = "xn%d" % (t % 2)
                    s.act(lambda e, t=t: e.activation(out=junk, in_=xs[:, t, :], func=AF.Square, scale=1.0 / 32,
                                                      accum_out=stt[:, 12:13]), reads=[xkey], writes=["on", "stt_ms"])
                    rstd_from(stt[:, 12:13], stt[:, 13:14], "stt_ms")
                    s.dve(lambda e, t=t, xn=xn: e.tensor_scalar(out=xn[:], in0=xs[:, t, :], scalar1=stt[:, 13:14], scalar2=None,
                                                                op0=ALU.mult), reads=[xkey, "stt_ms"], writes=[xnk])
                    yield
                for t in range(nt):
                    xn = xnb[t % 2]
                    xnk = "xn%d" % (t % 2)
                    tpv = banks[0][:].bitcast(BF16).rearrange("p (c t) -> p c t", c=8)
                    for c in range(8):
                        s.pe(lambda e, c=c, tpv=tpv, xn=xn: e.transpose(out=tpv[:, c, :], in_=xn[:, c * 128:(c + 1) * 128],
                                                                        identity=ident[:]),
                             reads=[xnk, "ident"], writes=[bkey[0]])
                    for c in range(8):
                        if c % 2 == 0:
                            s.act(lambda e, c=c, t=t, tpv=tpv: e.activation(
                                out=hT[:, c, t * 128:(t + 1) * 128], in_=tpv[:, c, :], func=AF.Identity,
                                scale=A1[:, c, bb:bb + 1], bias=adaT[:, c, bb:bb + 1]),
                                reads=["A1", "adaT"], writes=[bkey[0], hkey])
                        else:
                            s.dve(lambda e, c=c, t=t, tpv=tpv: e.tensor_scalar(
                                out=hT[:, c, t * 128:(t + 1) * 128], in0=tpv[:, c, :],
                                scalar1=A1[:, c, bb:bb + 1], scalar2=adaT[:, c, bb:bb + 1], op0=ALU.mult, op1=ALU.add),
                                reads=["A1", "adaT"], writes=[bkey[0], hkey])
                    yield

            def supertile(kind, b, s0, nt, n, nxt_gen):
                W = 128 * nt
                hA = 0 if kind == "p" else 1
                hB = 0 if kind == "p" else 4
                slot_x = (n % 2) if kind == "p" else 0
                xs = xt[slot_x]
                xkey = "xt%d" % slot_x
                hT = hTb[n % 2]
                hkey = "hT%d" % (n % 2)
                bb = b if kind == "p" else 4 + b
                slotA0 = (s0 + hA) % 8
                slotB0 = (s0 + hB) % 8
                ka_keys = ["kaT%d" % ((slotA0 + t) % 8) for t in range(nt)]
                kb_keys = ["kbT%d" % ((slotB0 + t) % 8) for t in range(nt)]
                jobs = [
                    ([0, 128], qaT[:, 0:2, 0:W], 0.125, ["qaT"]),
                    ([256, 384], qaT[:, 2:4, 0:W], 0.125, ["qaT"]),
                    ([768, 896], qbT[:, 0:2, 0:W], 0.125, ["qbT"]),
                    ([1024, 1152], qbT[:, 2:4, 0:W], 0.125, ["qbT"]),
                    ([1280, 1408], kbT[:, 0:2, slotB0 * 128:slotB0 * 128 + W], None, kb_keys),
                    ([1536, 1664], kbT[:, 2:4, slotB0 * 128:slotB0 * 128 + W], None, kb_keys),
                    ([512], kaT[:, slotA0 * 128:slotA0 * 128 + W], None, ka_keys),
                ]
                for cols, dest, scale, wkeys in jobs:
                    bk = wbank()
                    pv = banks[bk][:, 0:len(cols) * W].rearrange("p (j w) -> p j w", j=len(cols))
                    for j, c0 in enumerate(cols):
                        for k in range(8):
                            s.pe(lambda e, pv=pv, j=j, c0=c0, k=k: e.matmul(
                                pv[:, j, :], lhsT=w_in_sb[:, k, c0:c0 + 128], rhs=hT[:, k, 0:W],
                                start=(k == 0), stop=(k == 7)),
                                reads=[hkey, "w_in", "w_in_rest"], writes=[bkey[bk]])
                    src = pv if len(cols) == 2 else pv[:, 0, :]
                    evac_copy(dest, src, reads=[], writes=[bkey[bk]] + wkeys, scale=scale)
                for t in range(nt):
                    ti = s0 + t
                    sA = (ti + hA) % 8
                    sB = (ti + hB) % 8
                    bk = wbank()
                    for k in range(8):
                        s.pe(lambda e, bk=bk, k=k, t=t: e.matmul(
                            banks[bk][:], lhsT=hT[:, k, t * 128:(t + 1) * 128], rhs=w_in_sb[:, k, 1792:2304],
                            start=(k == 0), stop=(k == 7)), reads=[hkey, "w_in", "w_in_rest"], writes=[bkey[bk]])
                    evac_copy(vb[:, sB, :, 0:64], banks[bk][:].rearrange("p (h d) -> p h d", d=64),
                              reads=[], writes=[bkey[bk], "vb%d" % sB])
                    outB = (kind == "p" and ti >= 12) or kind == "s"
                    outA = (kind == "p" and ti == 15) or kind == "s"
                    if outB:
                        s.dve(lambda e, bk=bk: e.tensor_copy(out=kst[0][:], in_=banks[bk][:]), writes=[bkey[bk], "sig"])
                        if kind == "p":
                            s.dma("sp", nbvp[b, (ti - 12) * 128:(ti - 11) * 128, :], kst[0][:], reads=["sig"])
                        else:
                            s.dma("sp", nbvs[b, 448:512, :], kst[0][0:64, :], reads=["sig"])
                            s.dma("sp", nbvs[b, 0:448, :], cbv[b, 64:512, :])
                        bk2 = wbank()
                        for k in range(8):
                            s.pe(lambda e, bk2=bk2, k=k, t=t: e.matmul(
                                banks[bk2][:], lhsT=hT[:, k, t * 128:(t + 1) * 128], rhs=w_in_sb[:, k, 1280:1792],
                                start=(k == 0), stop=(k == 7)), reads=[hkey, "w_in", "w_in_rest"], writes=[bkey[bk2]])
                        s.act(lambda e, bk2=bk2: e.activation(out=kst[1][:], in_=banks[bk2][:], func=AF.Copy),
                              writes=[bkey[bk2], "prod"])
                        if kind == "p":
                            s.dma("sp", nbkp[b, (ti - 12) * 128:(ti - 11) * 128, :], kst[1][:], reads=["prod"])
                        else:
                            s.dma("sp", nbks[b, 448:512, :], kst[1][0:64, :], reads=["prod"])
                            s.dma("sp", nbks[b, 0:448, :], cbk[b, 64:512, :])
                    bk = wbank()
                    for k in range(8):
                        s.pe(lambda e, bk=bk, k=k, t=t: e.matmul(
                            banks[bk][:, 0:128], lhsT=hT[:, k, t * 128:(t + 1) * 128], rhs=w_in_sb[:, k, 640:768],
                            start=(k == 0), stop=(k == 7)), reads=[hkey, "w_in", "w_in_rest"], writes=[bkey[bk]])
                    if outA:
                        for k in range(8):
                            s.pe(lambda e, bk=bk, k=k, t=t: e.matmul(
                                banks[bk][:, 128:256], lhsT=hT[:, k, t * 128:(t + 1) * 128], rhs=w_in_sb[:, k, 512:640],
                                start=(k == 0), stop=(k == 7)), reads=[hkey, "w_in", "w_in_rest"], writes=[bkey[bk]])
                    evac_copy(va[:, sA, :, 0:64], banks[bk][:, 0:128].rearrange("p (h d) -> p h d", d=64),
                              reads=[], writes=[bkey[bk], "va%d" % sA])
                    if outA:
                        s.dve(lambda e, bk=bk: e.tensor_copy(out=tmpf[:, 0:256], in_=banks[bk][:, 0:256]),
                              writes=[bkey[bk], "tmpf"])
                        if kind == "p":
                            s.dma("sp", navp[b, :, :], tmpf[:, 0:128], reads=["tmpf"])
                            s.dma("sp", nakp[b, :, :], tmpf[:, 128:256], reads=["tmpf"])
                        else:
                            s.dma("sp", navs[b, 64:128, :], tmpf[0:64, 0:128], reads=["tmpf"])
                            s.dma("sp", naks[b, 64:128, :], tmpf[0:64, 128:256], reads=["tmpf"])
                            s.dma("sp", navs[b, 0:64, :], cav[b, 64:128, :])
                            s.dma("sp", naks[b, 0:64, :], cak[b, 64:128, :])
                gl = []
                for t in range(nt):
                    gl += attention_groups(t * 128, s0 + t + hA, s0 + t + hB)
                run_attention(gl)
                if nxt_gen is not None:
                    for _ in range(2):
                        next(nxt_gen, None)
                for f in range(8):
                    bz = wbank()
                    bp = wbank()
                    zv = banks[bz][:, 0:2 * W].rearrange("p (j w) -> p j w", j=2)
                    pv = banks[bp][:, 0:2 * W].rearrange("p (j w) -> p j w", j=2)
                    for j in range(2):
                        for k in range(8):
                            s.pe(lambda e, zv=zv, j=j, k=k, f=f: e.matmul(
                                zv[:, j, :], lhsT=wg_sb[:, k, j * 1024 + f * 128:j * 1024 + (f + 1) * 128],
                                rhs=hT[:, k, 0:W], start=(k == 0), stop=(k == 7)),
                                reads=[hkey, "wg"], writes=[bkey[bz]])
                    for j, wsb in enumerate((wpa_sb, wpb_sb)):
                        for k in range(4):
                            s.pe(lambda e, pv=pv, j=j, k=k, f=f, wsb=wsb: e.matmul(
                                pv[:, j, :], lhsT=wsb[:, k, f * 128:(f + 1) * 128], rhs=oT[:, j * 4 + k, 0:W],
                                start=(k == 0), stop=(k == 3)),
                                reads=["oT", "wpa", "wpb"], writes=[bkey[bp]])
                    for j in range(2):
                        s.act(lambda e, zv=zv, j=j, f=f: e.activation(
                            out=sig[:, j * W:(j + 1) * W], in_=zv[:, j, :], func=AF.Sigmoid,
                            bias=bgT[:, j * 8 + f:j * 8 + f + 1]), reads=["bgT"], writes=[bkey[bz], "sig"])
                    s.dve(lambda e, bp=bp: e.tensor_tensor(out=prod[:, 0:2 * W], in0=banks[bp][:, 0:2 * W],
                                                           in1=sig[:, 0:2 * W], op=ALU.mult),
                          reads=["sig"], writes=[bkey[bp], "prod"])
                    s.dve(lambda e, f=f: e.tensor_tensor(out=mT[:, f, 0:W], in0=prod[:, 0:W], in1=prod[:, W:2 * W],
                                                         op=ALU.add), reads=["prod"], writes=["mT"])
                    if nxt_gen is not None and f in (1, 4):
                        next(nxt_gen, None)
                for t in range(nt):
                    ti = s0 + t
                    bks = [wbank(), wbank()]
                    for sl in range(2):
                        for k in range(8):
                            s.pe(lambda e, sl=sl, k=k, t=t, bk=bks[sl]: e.matmul(
                                banks[bk][:], lhsT=mT[:, k, t * 128:(t + 1) * 128], rhs=wo_sb[:, k, sl * 512:(sl + 1) * 512],
                                start=(k == 0), stop=(k == 7)), reads=["mT", "wo"], writes=[bkey[bks[sl]]])
                    for sl in range(2):
                        s.act(lambda e, sl=sl, bk=bks[sl]: e.activation(
                            out=junk[:, 0:512], in_=banks[bk][:], func=AF.Square, scale=1.0 / 32,
                            accum_out=(stt[:, 10:11] if sl == 0 else stt[:, 11:12])),
                            writes=[bkey[bks[sl]], "on", "stt_w%d" % sl])
                    s.dve(lambda e: e.tensor_tensor(out=stt[:, 14:15], in0=stt[:, 10:11], in1=stt[:, 11:12], op=ALU.add),
                          reads=["stt_w0", "stt_w1"], writes=["stt_w"])
                    rstd_from(stt[:, 14:15], stt[:, 14:15], "stt_w")
                    for sl in range(2):
                        s.dve(lambda e, sl=sl, bk=bks[sl]: e.scalar_tensor_tensor(
                            out=tmpf[:], in0=banks[bk][:], scalar=stt[:, 14:15], in1=G1bc[:, sl * 512:(sl + 1) * 512],
                            op0=ALU.mult, op1=ALU.mult), reads=["stt_w", "G1bc"], writes=[bkey[bks[sl]], "tmpf"])
                        s.dve(lambda e, sl=sl, t=t: e.tensor_tensor(
                            out=xs[:, t, sl * 512:(sl + 1) * 512], in0=tmpf[:], in1=xs[:, t, sl * 512:(sl + 1) * 512],
                            op=ALU.add), reads=["tmpf"], writes=[xkey])
                    gt = (b * 16 + ti) if kind == "p" else (64 + b)
                    s.dma("sp", X1d[gt * 128:(gt + 1) * 128, :], xs[:, t, :], reads=[xkey], writes=["X1d"])
                    s.act(lambda e, t=t: e.activation(out=junk, in_=xs[:, t, :], func=AF.Square, scale=1.0 / 32,
                                                      accum_out=stt[:, 16:17]), reads=[xkey], writes=["on", "stt_x1"])
                    s.act(lambda e: e.activation(out=stt[:, 17:18], in_=stt[:, 16:17], func=AF.Ln, bias=1e-6, scale=1.0),
                          reads=["stt_x1"], writes=["stt_x1ln"])
                    s.act(lambda e, gt=gt: e.activation(out=rstd_all[:, gt:gt + 1], in_=stt[:, 17:18], func=AF.Exp, scale=-0.5),
                          reads=["stt_x1ln"], writes=["rstd_all"])

            stiles = [("p", b, s0, 2) for b in range(NB) for s0 in range(0, 16, 2)] + [("s", b, 0, 1) for b in range(NB)]

            def sample_history(b):
                s.dma("pool", ctile, cbk[b].rearrange("(t p) f -> p t f", p=128), writes=["xt1"])
                s.dma("pool", catile, cak[b], writes=["xt1"])
                for t in range(4):
                    s.dma("pool", vb[:, t, :, 0:64], cbv[b, t * 128:(t + 1) * 128, :].rearrange("p (h d) -> p h d", d=64),
                          writes=["vb%d" % t])
                s.dma("pool", va[:, 0, :, 0:64], cav[b].rearrange("p (h d) -> p h d", d=64), writes=["va0"])
                tpv = banks[0][:].bitcast(BF16).rearrange("p (c t) -> p c t", c=8)
                for t in range(4):
                    for c in range(4):
                        s.pe(lambda e, t=t, c=c, tpv=tpv: e.transpose(out=tpv[:, c, :], in_=ctile[:, t, c * 128:(c + 1) * 128],
                                                                      identity=ident[:]),
                             reads=["xt1", "ident"], writes=[bkey[0]])
                    evac_copy(kbT[:, :, t * 128:(t + 1) * 128], tpv[:, 0:4, :], reads=[], writes=[bkey[0], "kbT%d" % t])
                s.pe(lambda e, tpv=tpv: e.transpose(out=tpv[:, 0, :], in_=catile, identity=ident[:]),
                     reads=["xt1", "ident"], writes=[bkey[0]])
                evac_copy(kaT[:, 0:128], tpv[:, 0, :], reads=[], writes=[bkey[0], "kaT0"])

            cur_gen = prep(*stiles[0], 0)
            for _ in cur_gen:
                pass
            for n, (kind, b, s0, nt) in enumerate(stiles):
                if s0 == 0:
                    bb_ = b if kind == "p" else 4 + b
                    s.dma("sp", G1bc[:], AP(Gd, bb_ * 2 * D, [[0, 128], [1, D]]), writes=["G1bc"])
                if kind == "s":
                    sample_history(b)
                nxt = stiles[n + 1] if n + 1 < len(stiles) else None
                inter = nxt is not None and nxt[0] == "p"
                if inter:
                    xload(*nxt, n + 1)
                g = prep(*nxt, n + 1) if nxt is not None else None
                supertile(kind, b, s0, nt, n, g if inter else None)
                if g is not None:
                    for _ in g:
                        pass
            print("phase1 sbuf remaining", nc.sbuf_bytes_remaining)
            s.emit()

        with ExitStack() as e2:
            s = Sched(nc, st)
            h2T = [T(e2, "h2T%d" % i, [128, 8, 1024], BF16) for i in range(2)]
            yacc = T(e2, "yacc", [128, 8, D], F32)
            he = [T(e2, "he%d" % i, [128, 4, 1024], BF16) for i in range(2)]
            sg = [T(e2, "sg%d" % i, [128, 512], BF16) for i in range(2)]
            sgc = [T(e2, "sgc%d" % i, [128, 512], BF16) for i in range(2)]
            cb_sb = [T(e2, "cb_sb%d" % i, [128, 2, 1024], BF16) for i in range(2)]
            combT = T(e2, "combT", [32, 1024], F32)
            chi = [T(e2, "chi%d" % i, [32, 1024], BF16) for i in range(2)]
            wg2 = [T(e2, "wg2_%d" % i, [128, 8, 512], BF16) for i in range(2)]
            wu2 = [T(e2, "wu2_%d" % i, [128, 8, 512], BF16) for i in range(2)]
            wd2 = [T(e2, "wd2_%d" % i, [128, 4, D], BF16) for i in range(2)]
            xin = [T(e2, "xin%d" % i, [128, D], F32) for i in range(2)]
            xfin = [T(e2, "xfin%d" % i, [128, D], F32) for i in range(3)]
            G2t = [T(e2, "G2t%d" % i, [128, D], F32) for i in range(2)]
            xn2 = [T(e2, "xn2_%d" % i, [128, D], BF16) for i in range(2)]
            junk2 = T(e2, "junk2", [128, D], BF16)
            sel = T(e2, "sel", [32, 32, 128], BF16)
            rs = T(e2, "rs", [128, 8, 16], F32)
            LG = T(e2, "LG", [128, 8, 36], F32)
            ohg = T(e2, "ohg", [128, 8, 4], F32)
            rtmp = T(e2, "rtmp", [128, 8, 32], F32)
            r8 = T(e2, "r8", [128, 6, 8, 8], F32)
            comb = T(e2, "comb", [128, 8, 32], F32)
            fs = T(e2, "fs", [128, 24], F32)
            tmp2 = T(e2, "tmp2", [128, D], F32)
            print("phase2 sbuf remaining", nc.sbuf_bytes_remaining)

            s.dve(lambda e: e.tensor_copy(out=sel[:], in_=bcl(identf[0:32, 0:32], 128)), writes=["sel"])

            groups = []
            for b in range(NB):
                for half in range(2):
                    groups.append([("p", b, half * 8 + i) for i in range(8)])
            groups.append([("s", b, 0) for b in range(NB)])

            def gtile(kind, b, ti):
                return (b * 16 + ti) if kind == "p" else (64 + b)

            xic = [0]

            def prologue(gi):
                grp = groups[gi]
                ntl = len(grp)
                G = ntl * 128
                hb = gi % 2
                H = h2T[hb]
                base_x = xic[0]
                xic[0] += ntl

                def x1load(i):
                    kind, b, ti = grp[i]
                    gt = gtile(kind, b, ti)
                    xs_ = (base_x + i) % 2
                    s.dma("sp", xin[xs_][:], X1d[gt * 128:(gt + 1) * 128, :], writes=["xin%d" % xs_])

                x1load(0)
                for i, (kind, b, ti) in enumerate(grp):
                    bb = b if kind == "p" else 4 + b
                    xs_ = (base_x + i) % 2
                    xk = "xin%d" % xs_
                    gt = gtile(kind, b, ti)
                    if i + 1 < ntl:
                        x1load(i + 1)
                    s.dve(lambda e, xs_=xs_, gt=gt: e.tensor_scalar(out=xn2[xs_][:], in0=xin[xs_][:],
                                                                    scalar1=rstd_all[:, gt:gt + 1], scalar2=None, op0=ALU.mult),
                          reads=[xk], writes=["xn2_%d" % xs_])
                    yield
                    tb = xs_
                    tpv = banks[tb][:].bitcast(BF16).rearrange("p (c t) -> p c t", c=8)
                    for c in range(8):
                        s.pe(lambda e, c=c, tpv=tpv, xs_=xs_: e.transpose(out=tpv[:, c, :], in_=xn2[xs_][:, c * 128:(c + 1) * 128],
                                                                          identity=ident[:]),
                             reads=["xn2_%d" % xs_, "ident"], writes=[bkey[tb]])
                    for c in range(8):
                        if c % 2 == 0:
                            s.act(lambda e, c=c, i=i, tpv=tpv, bb=bb: e.activation(
                                out=H[:, c, i * 128:(i + 1) * 128], in_=tpv[:, c, :], func=AF.Identity,
                                scale=A2[:, c, bb:bb + 1], bias=adaT[:, 24 + c, bb:bb + 1]),
                                reads=["A2", "adaT"], writes=[bkey[tb], "h2T%d_%d" % (hb, i)])
                        else:
                            s.dve(lambda e, c=c, i=i, tpv=tpv, bb=bb: e.tensor_scalar(
                                out=H[:, c, i * 128:(i + 1) * 128], in0=tpv[:, c, :],
                                scalar1=A2[:, c, bb:bb + 1], scalar2=adaT[:, 24 + c, bb:bb + 1], op0=ALU.mult, op1=ALU.add),
                                reads=["A2", "adaT"], writes=[bkey[tb], "h2T%d_%d" % (hb, i)])
                    yield
                lgp = banks[1][:, 0:ntl * 36].rearrange("p (t x) -> p t x", x=36)
                for i in range(ntl):
                    for k in range(8):
                        s.pe(lambda e, k=k, i=i: e.matmul(lgp[:, i, :], lhsT=H[:, k, i * 128:(i + 1) * 128],
                                                          rhs=wr_sb[:, k, :], start=(k == 0), stop=(k == 7)),
                             reads=["h2T%d_%d" % (hb, i), "wr"], writes=[bkey[1]])
                    if i % 4 == 3:
                        yield
                R = ["rt"]
                Tn = ntl
                lgv = LG[:, 0:Tn, :]
                rb3 = bass.AP(rbias, rbias[:].offset, [list(rbias[:].ap[0]), [0, Tn], [1, 36]])
                s.dve(lambda e: e.tensor_tensor(out=lgv, in0=lgp, in1=rb3, op=ALU.add), reads=["rbias"], writes=[bkey[1]] + R)
                gmax = rs[:, 0:Tn, 3]
                s.dve(lambda e: e.tensor_reduce(out=gmax, in_=LG[:, 0:Tn, 0:4], axis=AX.X, op=ALU.max), writes=R)
                s.dve(lambda e: e.tensor_tensor(out=rtmp[:, 0:Tn, 0:4], in0=LG[:, 0:Tn, 0:4], in1=bcl(gmax, 4), op=ALU.subtract),
                      writes=R)
                s.act(lambda e: e.activation(out=rtmp[:, 0:Tn, 4:8], in_=rtmp[:, 0:Tn, 0:4], func=AF.Exp), writes=R)
                s.dve(lambda e: e.tensor_reduce(out=rs[:, 0:Tn, 4], in_=rtmp[:, 0:Tn, 4:8], axis=AX.X, op=ALU.add), writes=R)
                s.dve(lambda e: e.reciprocal(out=rs[:, 0:Tn, 5], in_=rs[:, 0:Tn, 4]), writes=R)
                s.dve(lambda e: e.tensor_tensor(out=ohg[:, 0:Tn, :], in0=LG[:, 0:Tn, 0:4], in1=bcl(gmax, 4), op=ALU.is_equal),
                      writes=R)
                yield
                s.dve(lambda e: e.tensor_tensor(out=rtmp[:, 0:Tn, :].rearrange("p t (g x) -> p t g x", g=4),
                                                in0=LG[:, 0:Tn, 4:36].rearrange("p t (g x) -> p t g x", g=4),
                                                in1=bcl(ohg[:, 0:Tn, :], 8), op=ALU.mult), writes=R)
                esel, oh1, msk, oh2, c8, t8 = (r8[:, j, 0:Tn, :] for j in range(6))
                s.dve(lambda e: e.tensor_reduce(out=esel, in_=rtmp[:, 0:Tn, :].rearrange("p t (g x) -> p t x g", g=4),
                                                axis=AX.X, op=ALU.add), writes=R)
                m1 = rs[:, 0:Tn, 6]
                m2 = rs[:, 0:Tn, 7]
                s.dve(lambda e: e.tensor_reduce(out=m1, in_=esel, axis=AX.X, op=ALU.max), writes=R)
                s.dve(lambda e: e.tensor_tensor(out=oh1, in0=esel, in1=bcl(m1, 8), op=ALU.is_equal), writes=R)
                s.dve(lambda e: e.scalar_tensor_tensor(out=msk, in0=oh1, scalar=-1e9, in1=esel, op0=ALU.mult, op1=ALU.add),
                      writes=R)
                s.dve(lambda e: e.tensor_reduce(out=m2, in_=msk, axis=AX.X, op=ALU.max), writes=R)
                s.dve(lambda e: e.tensor_tensor(out=oh2, in0=msk, in1=bcl(m2, 8), op=ALU.is_equal), writes=R)
                yield
                s.dve(lambda e: e.tensor_tensor(out=rs[:, 0:Tn, 8], in0=m2, in1=m1, op=ALU.subtract), writes=R)
                s.act(lambda e: e.activation(out=rs[:, 0:Tn, 9], in_=rs[:, 0:Tn, 8], func=AF.Exp), writes=R)
                s.dve(lambda e: e.tensor_scalar(out=rs[:, 0:Tn, 10], in0=rs[:, 0:Tn, 9], scalar1=1.0, scalar2=None, op0=ALU.add),
                      writes=R)
                s.dve(lambda e: e.reciprocal(out=rs[:, 0:Tn, 11], in_=rs[:, 0:Tn, 10]), writes=R)
                s.dve(lambda e: e.tensor_tensor(out=rs[:, 0:Tn, 12], in0=rs[:, 0:Tn, 11], in1=rs[:, 0:Tn, 5], op=ALU.mult),
                      writes=R)
                s.dve(lambda e: e.tensor_tensor(out=rs[:, 0:Tn, 13], in0=rs[:, 0:Tn, 12], in1=rs[:, 0:Tn, 9], op=ALU.mult),
                      writes=R)
                s.dve(lambda e: e.tensor_tensor(out=c8, in0=oh1, in1=bcl(rs[:, 0:Tn, 12], 8), op=ALU.mult), writes=R)
                s.dve(lambda e: e.tensor_tensor(out=t8, in0=oh2, in1=bcl(rs[:, 0:Tn, 13], 8), op=ALU.mult), writes=R)
                s.dve(lambda e: e.tensor_tensor(out=c8, in0=c8, in1=t8, op=ALU.add), writes=R)
                c8b = bass.AP(r8, r8[:, 4, 0:Tn, :].offset, [list(r8[:].ap[0]), [8, Tn], [0, 4], [1, 8]])
                s.dve(lambda e: e.tensor_tensor(out=comb[:, 0:Tn, :].rearrange("p t (g x) -> p t g x", g=4),
                                                in0=bcl(ohg[:, 0:Tn, :], 8), in1=c8b, op=ALU.mult), writes=R + ["comb"])
                yield
                for i in range(ntl):
                    tb = 1 - (i // 4)
                    s.pe(lambda e, i=i, tb=tb: e.transpose(out=banks[tb][0:32, (i % 4) * 128:(i % 4 + 1) * 128],
                                                           in_=comb[:, i, :], identity=identf[:]),
                         reads=["comb", "identf"], writes=[bkey[tb]])
                for half in range((ntl + 3) // 4):
                    tb = 1 - half
                    s.act(lambda e, half=half, tb=tb: e.activation(out=combT[:, half * 512:(half + 1) * 512],
                                                                   in_=banks[tb][0:32, :], func=AF.Copy),
                          writes=[bkey[tb], "combT"])
                s.dve(lambda e: e.tensor_copy(out=chi[hb][:, 0:G], in_=combT[:, 0:G]), reads=["combT"], writes=["chi%d" % hb])
                yield

            fic = [0]

            def finalize(gi):
                grp = groups[gi]
                ntl = len(grp)
                base_f = fic[0]
                fic[0] += ntl
                prompt = grp[0][0] == "p"

                def xload_f(i):
                    kind, b, ti = grp[i]
                    fsl = (base_f + i) % 3
                    gt = gtile(kind, b, ti)
                    s.dma("sp", xfin[fsl][:], X1d[gt * 128:(gt + 1) * 128, :], writes=["xfin%d" % fsl])

                def gload_f(i):
                    kind, b, ti = grp[i]
                    bb = b if kind == "p" else 4 + b
                    gsl = 0 if prompt else i % 2
                    s.dma("sp", G2t[gsl][:], AP(Gd, (bb * 2 + 1) * D, [[0, 128], [1, D]]), writes=["G2t%d" % gsl])

                gload_f(0)
                xload_f(0)
                if ntl > 1:
                    xload_f(1)
                for i in range(ntl):
                    s.act(lambda e, i=i: e.activation(out=junk2[:], in_=yacc[:, i, :], func=AF.Square, scale=1.0 / 32,
                                                      accum_out=fs[:, i:i + 1]), reads=["yacc%d" % i], writes=["junk2", "fs_ms"])
                    if i % 2 == 1:
                        yield
                s.act(lambda e: e.activation(out=fs[:, 8:8 + ntl], in_=fs[:, 0:ntl], func=AF.Ln, bias=1e-6, scale=1.0),
                      reads=["fs_ms"], writes=["fs_ln"])
                s.act(lambda e: e.activation(out=fs[:, 16:16 + ntl], in_=fs[:, 8:8 + ntl], func=AF.Exp, scale=-0.5),
                      reads=["fs_ln"], writes=["fs_rstd"])
                for i, (kind, b, ti) in enumerate(grp):
                    fsl = (base_f + i) % 3
                    gsl = 0 if prompt else i % 2
                    xk = "xfin%d" % fsl
                    gk = "G2t%d" % gsl
                    yk = "yacc%d" % i
                    if i + 2 < ntl:
                        xload_f(i + 2)
                    if (not prompt) and i + 1 < ntl:
                        gload_f(i + 1)
                    s.dve(lambda e, i=i, fsl=fsl, gsl=gsl: e.scalar_tensor_tensor(
                        out=tmp2[:], in0=yacc[:, i, :], scalar=fs[:, 16 + i:17 + i], in1=G2t[gsl][:],
                        op0=ALU.mult, op1=ALU.mult), reads=[yk, "fs_rstd", gk], writes=["tmp2"])
                    s.dve(lambda e, fsl=fsl: e.tensor_tensor(out=xfin[fsl][:], in0=tmp2[:], in1=xfin[fsl][:], op=ALU.add),
                          reads=["tmp2"], writes=[xk])
                    if kind == "p":
                        s.dma("sp", yp[b, ti * 128:(ti + 1) * 128, :], xfin[fsl][:], reads=[xk])
                    else:
                        s.dma("sp", ys[b, :, :], xfin[fsl][0:64, :], reads=[xk])
                    yield

            ldc = [0]

            def load_gu(slot, eb):
                for j in range(2):
                    e_ = eb * 2 + j
                    s.dma("pool", wg2[slot][:, :, j * 256:(j + 1) * 256], w_eg[e_].rearrange("(c p) n -> p c n", p=128),
                          writes=["wg2_%d" % slot])
                    s.dma("pool", wu2[slot][:, :, j * 256:(j + 1) * 256], w_eu[e_].rearrange("(c p) n -> p c n", p=128),
                          writes=["wu2_%d" % slot])

            def load_d(slot, eb):
                for j in range(2):
                    e_ = eb * 2 + j
                    s.dma("pool", wd2[slot][:, 2 * j:2 * j + 2, :], w_ed[e_].rearrange("(c p) n -> p c n", p=128),
                          writes=["wd2_%d" % slot])

            aux = []
            fin_ids = set()

            def aux_step():
                while aux:
                    try:
                        next(aux[0])
                        return
                    except StopIteration:
                        aux.pop(0)

            def aux_drain(n_keep=0):
                while len(aux) > n_keep:
                    try:
                        next(aux[0])
                    except StopIteration:
                        aux.pop(0)

            for _ in prologue(0):
                pass
            load_gu(0, 0)
            load_d(0, 0)

            def run_group(gi, grp):
                ntl = len(grp)
                G = ntl * 128
                nblk = (G + 511) // 512
                bw = min(512, G)
                hb = gi % 2
                H = h2T[hb]

                def gu(eb):
                    slot = ldc[0] % 2
                    hs = eb % 2
                    for j in range(2):
                        e_ = eb * 2 + j
                        for blk in range(nblk):
                            bk = blk % 2
                            s.pe(lambda e, bk=bk, e_=e_, blk=blk: e.matmul(
                                banks[bk][:, 0:bw], lhsT=sel[:, e_, :], rhs=chi[hb][:, blk * 512:blk * 512 + bw],
                                start=True, stop=True), reads=["sel", "chi%d" % hb], writes=[bkey[bk]])
                            s.act(lambda e, bk=bk, j=j, blk=blk, hs=hs: e.activation(
                                out=cb_sb[hs][:, j, blk * 512:blk * 512 + bw], in_=banks[bk][:, 0:bw], func=AF.Copy),
                                writes=[bkey[bk], "cb%d" % hs])
                    for fc in range(4):
                        j = fc // 2
                        for blk in range(nblk):
                            n_ = fc * nblk + blk
                            bg = 2 + n_ % 2
                            bu = 4 + n_ % 2
                            i2 = n_ % 2
                            hkeys = ["h2T%d_%d" % (hb, i) for i in range(blk * 4, min(ntl, blk * 4 + 4))]
                            for k in range(8):
                                s.pe(lambda e, bg=bg, k=k, fc=fc, blk=blk, slot=slot: e.matmul(
                                    banks[bg][:, 0:bw], lhsT=wg2[slot][:, k, fc * 128:(fc + 1) * 128],
                                    rhs=H[:, k, blk * 512:blk * 512 + bw], start=(k == 0), stop=(k == 7)),
                                    reads=hkeys + ["wg2_%d" % slot], writes=[bkey[bg]])
                            for k in range(8):
                                s.pe(lambda e, bu=bu, k=k, fc=fc, blk=blk, slot=slot: e.matmul(
                                    banks[bu][:, 0:bw], lhsT=wu2[slot][:, k, fc * 128:(fc + 1) * 128],
                                    rhs=H[:, k, blk * 512:blk * 512 + bw], start=(k == 0), stop=(k == 7)),
                                    reads=hkeys + ["wu2_%d" % slot], writes=[bkey[bu]])
                            s.act(lambda e, bg=bg, i2=i2: e.activation(out=sg[i2][:, 0:bw], in_=banks[bg][:, 0:bw], func=AF.Silu),
                                  writes=[bkey[bg], "sg%d" % i2])
                            s.dve(lambda e, i2=i2, j=j, blk=blk, hs=hs: e.tensor_tensor(
                                out=sgc[i2][:, 0:bw], in0=sg[i2][:, 0:bw], in1=cb_sb[hs][:, j, blk * 512:blk * 512 + bw],
                                op=ALU.mult), reads=["sg%d" % i2, "cb%d" % hs], writes=["sgc%d" % i2])
                            s.dve(lambda e, bu=bu, i2=i2, fc=fc, blk=blk, hs=hs: e.tensor_tensor(
                                out=he[hs][:, fc, blk * 512:blk * 512 + bw], in0=banks[bu][:, 0:bw], in1=sgc[i2][:, 0:bw],
                                op=ALU.mult), reads=["sgc%d" % i2], writes=[bkey[bu], "he%d" % hs])
                            aux_step()

                def down(eb, dslot):
                    hs = eb % 2
                    for i in range(ntl):
                        for sl in range(2):
                            n_ = i * 2 + sl
                            bd = 6 + n_ % 2
                            for fc in range(4):
                                s.pe(lambda e, bd=bd, fc=fc, i=i, sl=sl, hs=hs, dslot=dslot: e.matmul(
                                    banks[bd][:], lhsT=he[hs][:, fc, i * 128:(i + 1) * 128],
                                    rhs=wd2[dslot][:, fc, sl * 512:(sl + 1) * 512], start=(fc == 0), stop=(fc == 3)),
                                    reads=["he%d" % hs, "wd2_%d" % dslot], writes=[bkey[bd]])
                            yk = "yacc%d" % i
                            if eb == 0:
                                s.dve(lambda e, bd=bd, i=i, sl=sl: e.tensor_copy(out=yacc[:, i, sl * 512:(sl + 1) * 512],
                                                                                in_=banks[bd][:]), writes=[bkey[bd], yk])
                            else:
                                s.dve(lambda e, bd=bd, i=i, sl=sl: e.tensor_tensor(
                                    out=yacc[:, i, sl * 512:(sl + 1) * 512], in0=banks[bd][:],
                                    in1=yacc[:, i, sl * 512:(sl + 1) * 512], op=ALU.add), writes=[bkey[bd], yk])

                dslots = {}
                for eb in range(17):
                    if eb == 3 and gi + 1 < len(groups):
                        aux.append(prologue(gi + 1))
                    if eb < 16:
                        dslots[eb] = ldc[0] % 2
                        if eb + 1 < 16:
                            load_gu((ldc[0] + 1) % 2, eb + 1)
                        elif gi + 1 < len(groups):
                            load_gu((ldc[0] + 1) % 2, 0)
                        gu(eb)
                    if eb == 1:
                        while aux and id(aux[0]) in fin_ids:
                            for _ in aux[0]:
                                pass
                            aux.pop(0)
                    if eb >= 1:
                        down(eb - 1, dslots[eb - 1])
                    if eb < 16:
                        if eb + 1 < 16:
                            load_d((ldc[0] + 1) % 2, eb + 1)
                        elif gi + 1 < len(groups):
                            load_d((ldc[0] + 1) % 2, 0)
                        ldc[0] += 1
                aux_drain(0)
                fin = finalize(gi)
                aux.append(fin)
                fin_ids.add(id(fin))

            for gi, grp in enumerate(groups):
                run_group(gi, grp)
            aux_drain(0)
            s.emit()
    return nc


_PROG = {}


def kernel(**inputs):
    f = lambda a: np.ascontiguousarray(np.asarray(a, dtype=np.float32))
    if "nc" not in _PROG:
        _PROG["nc"] = build_program()
    nc = _PROG["nc"]
    shared = {
        "w_ada": f(inputs["w_ada"][0]), "b_ada": f(inputs["b_ada"][0]),
        "g_pre_mix": f(inputs["g_pre_mix"][0]), "g_post_mix": f(inputs["g_post_mix"][0]),
        "g_pre_ffn": f(inputs["g_pre_ffn"][0]), "g_post_ffn": f(inputs["g_post_ffn"][0]),
        "w_in": f(inputs["w_in"][0]), "a_sinks": f(inputs["a_sinks"][0]), "t5_table": f(inputs["t5_table"]),
        "b_rel": f(inputs["b_rel_table"][0]), "w_pa": f(inputs["w_proj_a"][0]), "w_pb": f(inputs["w_proj_b"][0]),
        "w_gate": f(inputs["w_gate"][0]), "b_gate": f(inputs["b_gate"][0]), "w_o": f(inputs["w_o"][0]),
        "w_rg": f(inputs["w_route_g"][0]), "b_rg": f(inputs["b_route_g"][0]),
        "w_re": f(inputs["w_route_e"][0]), "b_re": f(inputs["b_route_e"][0]),
        "w_eg": f(inputs["w_e_gate"][0]), "w_eu": f(inputs["w_e_up"][0]), "w_ed": f(inputs["w_e_down"][0]),
    }
    in_maps = []
    for c in range(NCORES):
        sl = slice(c * NB, (c + 1) * NB)
        m = dict(shared)
        m["xp"] = f(inputs["x_prompt"][sl]); m["xs"] = f(inputs["x_sample"][sl])
        m["cp"] = f(inputs["c_prompt"][sl]); m["cs"] = f(inputs["c_sample"][sl])
        m["cak"] = f(inputs["cache_a_k"][0, sl]).reshape(NB, 128, 128)
        m["cav"] = f(inputs["cache_a_v"][0, sl]).reshape(NB, 128, 128)
        m["cbk"] = f(inputs["cache_b_k"][0, sl]).reshape(NB, 512, 512)
        m["cbv"] = f(inputs["cache_b_v"][0, sl]).reshape(NB, 512, 512)
        in_maps.append(m)
    res = run_bass_kernel_spmd(nc, in_maps, core_ids=list(range(NCORES)))
    R = res.results
    cat = lambda k: np.concatenate([np.asarray(r[k], dtype=np.float32) for r in R], axis=0)
    y_p = cat("yp"); y_s = cat("ys")
    nakp = cat("nakp").reshape(1, 32, 128, 2, 64); navp = cat("navp").reshape(1, 32, 128, 2, 64)
    nbkp = cat("nbkp").reshape(1, 32, 512, 8, 64); nbvp = cat("nbvp").reshape(1, 32, 512, 8, 64)
    naks = cat("naks").reshape(1, 32, 128, 2, 64); navs = cat("navs").reshape(1, 32, 128, 2, 64)
    nbks = cat("nbks").reshape(1, 32, 512, 8, 64); nbvs = cat("nbvs").reshape(1, 32, 512, 8, 64)
    return (y_p, y_s, nakp, navp, nbkp, nbvp, naks, navs, nbks, nbvs)
```
